# Optimizing a Trainium2 kernel written in Bass

```python
import jax, jax.numpy as jnp
from jax import lax
import numpy as np

D_MODEL = 1024
BATCH = 8
SEQ = 4096
DEPTH = 2

GRID_W = 64
CTX_LEN = 256
EPS = 1e-6
HEAD_DIM = 64
CONV_WIDTH = 512
CONV_K = 3
NA_HEADS = 8
NA_WIDTH = NA_HEADS * HEAD_DIM
NA_KH = 8
NA_KW = 16
GQA_Q_HEADS = 8
GQA_KV_HEADS = 2
GQA_WIDTH = GQA_Q_HEADS * HEAD_DIM
GQA_KV_WIDTH = GQA_KV_HEADS * HEAD_DIM
ROPE_THETA = 10000.0
Q_BLOCK = 128
N_EXPERTS = 32
TOP_K = 4
D_FF_EXPERT = D_MODEL
SWIGLU_ALPHA = 1.702
SWIGLU_LIMIT = 7.0
MOE_BLOCK = 128
IN_SPLITS = (CONV_WIDTH,) * 3 + (NA_WIDTH,) * 3 + (GQA_WIDTH, GQA_KV_WIDTH, GQA_KV_WIDTH) + (D_MODEL,) * 3
IN_COLS = sum(IN_SPLITS)
IN_OFFSETS = [int(o) for o in np.cumsum(IN_SPLITS)[:-1]]

kernel_name = 'hybrid_conv_na_gqa_moe_dit'


def rmsnorm(x, g):
    xf = x.astype(jnp.float32)
    y = xf * lax.rsqrt(jnp.mean(xf * xf, axis=-1, keepdims=True) + EPS)
    return (y * g.astype(jnp.float32)).astype(x.dtype)


def modulate(h, shift, scale):
    return h * (1 + scale) + shift


def split_in(p):
    return jnp.split(p, IN_OFFSETS, axis=-1)


def heads(t, h):
    return t.reshape(t.shape[:-1] + (h, HEAD_DIM))


def conv3_centered(u, w):
    up = jnp.pad(u, ((0, 0), (1, 1), (0, 0)))
    return up[:, :-2] * w[0] + up[:, 1:-1] * w[1] + up[:, 2:] * w[2]


def axial_rope(x):
    n, dh = x.shape[1], x.shape[-1]
    half = dh // 2
    t = jnp.arange(n)
    inv = ROPE_THETA ** (-jnp.arange(0, half, 2, dtype=jnp.float32) / half)

    def rot(xa, pos):
        ang = pos.astype(jnp.float32)[:, None] * inv
        cos = jnp.cos(ang)[None, :, None, :]
        sin = jnp.sin(ang)[None, :, None, :]
        x1, x2 = jnp.split(xa.astype(jnp.float32), 2, axis=-1)
        return jnp.concatenate([x1 * cos - x2 * sin, x1 * sin + x2 * cos], axis=-1)

    xr, xc = jnp.split(x, 2, axis=-1)
    return jnp.concatenate([rot(xr, t // GRID_W), rot(xc, t % GRID_W)], axis=-1).astype(x.dtype)


def attend(q, k, v):
    b, t, hq, dh = q.shape
    hkv = k.shape[2]
    qg = q.reshape(b, t, hkv, hq // hkv, dh)
    s = jnp.einsum('btkgd,bskd->bkgts', qg, k).astype(jnp.float32) * (dh ** -0.5)
    p = jax.nn.softmax(s, axis=-1).astype(v.dtype)
    o = jnp.einsum('bkgts,bskd->btkgd', p, v)
    return o.reshape(b, t, hq * dh)


def gqa_blocked(q, k, v, kc, vc):
    b, n, hq, dh = q.shape
    k_all = jnp.concatenate([k, kc], axis=1)
    v_all = jnp.concatenate([v, vc], axis=1)
    nb = n // Q_BLOCK
    qb = jnp.moveaxis(q.reshape(b, nb, Q_BLOCK, hq, dh), 1, 0)
    out = lax.map(lambda qi: attend(qi, k_all, v_all), qb)
    return jnp.moveaxis(out, 0, 1).reshape(b, n, hq * dh)


def neighborhood_attention(q, k, v, kc, vc, rpb):
    b, n, h, dh = q.shape
    rows = n // GRID_W
    wh = min(NA_KH, rows)
    qg = q.reshape(b, rows, GRID_W, h, dh)
    kg = k.reshape(b, rows, GRID_W, h, dh)
    vg = v.reshape(b, rows, GRID_W, h, dh)
    cols = jnp.arange(GRID_W)
    col_start = jnp.clip(cols - NA_KW // 2, 0, GRID_W - NA_KW)
    cidx = col_start[:, None] + jnp.arange(NA_KW)
    coff = cidx - cols[:, None] + (NA_KW - 1)
    scale = dh ** -0.5
    n_win = wh * NA_KW

    def row_block(r):
        rs = jnp.clip(r - NA_KH // 2, 0, rows - wh)
        q_r = lax.dynamic_index_in_dim(qg, r, axis=1, keepdims=False)
        k_band = lax.dynamic_slice_in_dim(kg, rs, wh, axis=1)
        v_band = lax.dynamic_slice_in_dim(vg, rs, wh, axis=1)
        k_win = k_band[:, :, cidx]
        v_win = v_band[:, :, cidx]
        roff = rs + jnp.arange(wh) - r + (NA_KH - 1)
        bias = rpb[:, roff[None, :, None], coff[:, None, :]]
        s_win = jnp.einsum('bchd,bicjhd->bhcij', q_r, k_win).astype(jnp.float32) * scale + bias.astype(jnp.float32)[None]
        s_ctx = jnp.einsum('bchd,blhd->bhcl', q_r, kc).astype(jnp.float32) * scale
        s = jnp.concatenate([s_win.reshape(b, h, GRID_W, n_win), s_ctx], axis=-1)
        p = jax.nn.softmax(s, axis=-1).astype(v.dtype)
        p_win = p[..., :n_win].reshape(b, h, GRID_W, wh, NA_KW)
        p_ctx = p[..., n_win:]
        return jnp.einsum('bhcij,bicjhd->bchd', p_win, v_win) + jnp.einsum('bhcl,blhd->bchd', p_ctx, vc)

    out = lax.map(row_block, jnp.arange(rows))
    return jnp.moveaxis(out, 0, 1).reshape(b, n, h * dh)


def branch_merge(y_conv, y_na, y_gqa, g_conv, g_na, g_gqa, w_br_conv, w_br_na, w_br_gqa, w_out):
    m = (jax.nn.sigmoid(g_conv) * (y_conv @ w_br_conv)
         + jax.nn.sigmoid(g_na) * (y_na @ w_br_na)
         + jax.nn.sigmoid(g_gqa) * (y_gqa @ w_br_gqa))
    return m @ w_out


def clamped_swiglu(hgu):
    g = jnp.minimum(hgu[..., ::2], SWIGLU_LIMIT)
    u = jnp.clip(hgu[..., 1::2], -SWIGLU_LIMIT, SWIGLU_LIMIT)
    return (u + 1) * (g * jax.nn.sigmoid(SWIGLU_ALPHA * g))


def moe(h, router_w, router_b, w_gu, b_gu, w_down, b_down):
    n, d = h.shape
    logits = (h @ router_w + router_b).astype(jnp.float32)
    top_val, top_idx = lax.top_k(logits, TOP_K)
    gates = jax.nn.softmax(top_val, axis=-1).astype(h.dtype)
    nk = n * TOP_K
    flat_e = top_idx.reshape(nk)
    flat_tok = jnp.repeat(jnp.arange(n, dtype=jnp.int32), TOP_K)
    flat_g = gates.reshape(nk)
    order = jnp.argsort(flat_e)
    se, stok, sg = flat_e[order], flat_tok[order], flat_g[order]
    counts = jnp.bincount(flat_e, length=N_EXPERTS)
    padded = (counts + MOE_BLOCK - 1) // MOE_BLOCK * MOE_BLOCK
    pend = jnp.cumsum(padded)
    pstart = pend - padded
    cstart = jnp.cumsum(counts) - counts
    dest = pstart[se] + jnp.arange(nk) - cstart[se]
    n_blocks = -(-nk // MOE_BLOCK) + N_EXPERTS
    cap = n_blocks * MOE_BLOCK
    tok_buf = jnp.zeros((cap,), jnp.int32).at[dest].set(stok)
    gate_buf = jnp.zeros((cap,), h.dtype).at[dest].set(sg)
    block_e = jnp.minimum(jnp.searchsorted(pend, jnp.arange(n_blocks) * MOE_BLOCK, side='right'), N_EXPERTS - 1)

    def expert_block(args):
        tok, g, e = args
        xb = h[tok]
        a = clamped_swiglu(xb @ w_gu[e] + b_gu[e])
        return (a @ w_down[e] + b_down[e]) * g[:, None]

    y = lax.map(expert_block, (tok_buf.reshape(n_blocks, MOE_BLOCK), gate_buf.reshape(n_blocks, MOE_BLOCK), block_e))
    return jnp.zeros_like(h).at[tok_buf].add(y.reshape(cap, d))


def setup_inputs(seed: int = 0) -> dict:
    key = jax.random.key(seed)
    ks = jax.random.split(key, 24)

    def nrm(k, shape, scale):
        return jax.random.normal(k, shape, jnp.float32) * scale

    return {
        'x': nrm(ks[0], (BATCH, SEQ, D_MODEL), 1.0),
        'c': nrm(ks[1], (BATCH, D_MODEL), 1.0),
        'ctx': nrm(ks[2], (BATCH, CTX_LEN, D_MODEL), 1.0),
        'c_ctx': nrm(ks[3], (D_MODEL,), 1.0),
        'ada_w': nrm(ks[4], (DEPTH, D_MODEL, 6 * D_MODEL), 0.5 * D_MODEL ** -0.5),
        'ada_b': nrm(ks[5], (DEPTH, 6 * D_MODEL), 0.02),
        'norm1_g': 1.0 + nrm(ks[6], (DEPTH, D_MODEL), 0.02),
        'norm2_g': 1.0 + nrm(ks[7], (DEPTH, D_MODEL), 0.02),
        'w_in': nrm(ks[8], (DEPTH, D_MODEL, IN_COLS), D_MODEL ** -0.5),
        'conv_w': nrm(ks[9], (DEPTH, CONV_K, CONV_WIDTH), CONV_K ** -0.5),
        'na_rpb': nrm(ks[10], (DEPTH, NA_HEADS, 2 * NA_KH - 1, 2 * NA_KW - 1), 0.1),
        'q_norm_g': 1.0 + nrm(ks[11], (DEPTH, HEAD_DIM), 0.02),
        'k_norm_g': 1.0 + nrm(ks[12], (DEPTH, HEAD_DIM), 0.02),
        'w_br_conv': nrm(ks[13], (DEPTH, CONV_WIDTH, D_MODEL), CONV_WIDTH ** -0.5),
        'w_br_na': nrm(ks[14], (DEPTH, NA_WIDTH, D_MODEL), NA_WIDTH ** -0.5),
        'w_br_gqa': nrm(ks[15], (DEPTH, GQA_WIDTH, D_MODEL), GQA_WIDTH ** -0.5),
        'w_out': nrm(ks[16], (DEPTH, D_MODEL, D_MODEL), D_MODEL ** -0.5),
        'router_w': nrm(ks[17], (DEPTH, D_MODEL, N_EXPERTS), D_MODEL ** -0.5),
        'router_b': nrm(ks[18], (DEPTH, N_EXPERTS), 0.01),
        'w_gu': nrm(ks[19], (DEPTH, N_EXPERTS, D_MODEL, 2 * D_FF_EXPERT), D_MODEL ** -0.5),
        'b_gu': nrm(ks[20], (DEPTH, N_EXPERTS, 2 * D_FF_EXPERT), 0.01),
        'w_down': nrm(ks[21], (DEPTH, N_EXPERTS, D_FF_EXPERT, D_MODEL), D_FF_EXPERT ** -0.5),
        'b_down': nrm(ks[22], (DEPTH, N_EXPERTS, D_MODEL), 0.01),
        'final_g': 1.0 + nrm(ks[23], (D_MODEL,), 0.02),
    }


def reference(x, c, ctx, c_ctx, ada_w, ada_b, norm1_g, norm2_g, w_in, conv_w, na_rpb,
              q_norm_g, k_norm_g, w_br_conv, w_br_na, w_br_gqa, w_out,
              router_w, router_b, w_gu, b_gu, w_down, b_down, final_g):
    b, n, d = x.shape
    n_ctx = ctx.shape[1]
    xl, xc = x, ctx
    for l in range(DEPTH):
        last = l == DEPTH - 1
        mod_l = (jax.nn.silu(c) @ ada_w[l] + ada_b[l])[:, None, :]
        mod_c = jax.nn.silu(c_ctx) @ ada_w[l] + ada_b[l]
        sh1_l, sc1_l, gt1_l, sh2_l, sc2_l, gt2_l = jnp.split(mod_l, 6, axis=-1)
        sh1_c, sc1_c, gt1_c, sh2_c, sc2_c, gt2_c = jnp.split(mod_c, 6, axis=-1)

        hl = modulate(rmsnorm(xl, norm1_g[l]), sh1_l, sc1_l)
        hc = modulate(rmsnorm(xc, norm1_g[l]), sh1_c, sc1_c)
        (cb_l, cc_l, cx_l, naq_l, nak_l, nav_l, gq_l, gk_l, gv_l,
         bg_conv_l, bg_na_l, bg_gqa_l) = split_in(hl @ w_in[l])
        (cb_c, cc_c, cx_c, naq_c, nak_c, nav_c, gq_c, gk_c, gv_c,
         bg_conv_c, bg_na_c, bg_gqa_c) = split_in(hc @ w_in[l])
        na_kc = heads(nak_c, NA_HEADS)
        na_vc = heads(nav_c, NA_HEADS)
        gqa_kc = rmsnorm(heads(gk_c, GQA_KV_HEADS), k_norm_g[l])
        gqa_vc = heads(gv_c, GQA_KV_HEADS)

        y_conv = cb_l * conv3_centered(cc_l * cx_l, conv_w[l])
        y_na = neighborhood_attention(heads(naq_l, NA_HEADS), heads(nak_l, NA_HEADS),
                                      heads(nav_l, NA_HEADS), na_kc, na_vc, na_rpb[l])
        q_l = axial_rope(rmsnorm(heads(gq_l, GQA_Q_HEADS), q_norm_g[l]))
        k_l = axial_rope(rmsnorm(heads(gk_l, GQA_KV_HEADS), k_norm_g[l]))
        y_gqa = gqa_blocked(q_l, k_l, heads(gv_l, GQA_KV_HEADS), gqa_kc, gqa_vc)
        xl = xl + gt1_l * branch_merge(y_conv, y_na, y_gqa, bg_conv_l, bg_na_l, bg_gqa_l,
                                       w_br_conv[l], w_br_na[l], w_br_gqa[l], w_out[l])

        if not last:
            yc_conv = cb_c * conv3_centered(cc_c * cx_c, conv_w[l])
            yc_na = attend(heads(naq_c, NA_HEADS), na_kc, na_vc)
            yc_gqa = attend(rmsnorm(heads(gq_c, GQA_Q_HEADS), q_norm_g[l]), gqa_kc, gqa_vc)
            xc = xc + gt1_c * branch_merge(yc_conv, yc_na, yc_gqa, bg_conv_c, bg_na_c, bg_gqa_c,
                                           w_br_conv[l], w_br_na[l], w_br_gqa[l], w_out[l])

        hl2 = modulate(rmsnorm(xl, norm2_g[l]), sh2_l, sc2_l).reshape(b * n, d)
        if not last:
            hc2 = modulate(rmsnorm(xc, norm2_g[l]), sh2_c, sc2_c).reshape(b * n_ctx, d)
            out = moe(jnp.concatenate([hl2, hc2], axis=0), router_w[l], router_b[l],
                      w_gu[l], b_gu[l], w_down[l], b_down[l])
            xl = xl + gt2_l * out[:b * n].reshape(b, n, d)
            xc = xc + gt2_c * out[b * n:].reshape(b, n_ctx, d)
        else:
            out = moe(hl2, router_w[l], router_b[l], w_gu[l], b_gu[l], w_down[l], b_down[l])
            xl = xl + gt2_l * out.reshape(b, n, d)
    return rmsnorm(xl, final_g)
```

```python
import numpy as np
from contextlib import ExitStack
import concourse.bass as bass
import concourse.mybir as mybir
from concourse.bass_utils import run_bass_kernel_spmd

F32 = mybir.dt.float32
BF16 = mybir.dt.bfloat16
I32 = mybir.dt.int32
AF = mybir.ActivationFunctionType
ALU = mybir.AluOpType
AX = mybir.AxisListType

D = 1024
TL = 4096
TC = 256
T = TL + TC
NT = T // 128
DEPTH = 2
NE = 32
CAP = 2048
EPS = 1e-6
NEG = -30000.0
NJ = 22
JOFF = 10
INC = 6912


class Res:
    __slots__ = ("w", "rs")

    def __init__(self):
        self.w = None
        self.rs = {}


class Sched:
    ENGS = ("pe", "act", "dve", "pool", "sp")
    NQ = 20

    def __init__(self, nc, stack, same_engine_sync=True):
        self.nc = nc
        self.eng = {"pe": nc.tensor, "act": nc.scalar, "dve": nc.vector,
                    "pool": nc.gpsimd, "sp": nc.sync}
        self.sem = {}
        self.cnt = {}
        self.seen = {e: {} for e in self.ENGS}
        self.prog = {e: [] for e in self.ENGS}
        self.same_engine_sync = same_engine_sync
        for e in self.ENGS:
            self.sem[e] = stack.enter_context(nc.semaphore("c_" + e))
            self.cnt[e] = 0
        self.dq = {}
        for q in ("sp", "act", "pool"):
            keys = []
            for i in range(self.NQ):
                k = "d_%s_%d" % (q, i)
                self.sem[k] = stack.enter_context(nc.semaphore(k))
                self.cnt[k] = 0
                keys.append(k)
            self.dq[q] = [keys, 0]

    def _deps(self, engine, reads, writes, extra=()):
        need = {}
        for r in reads:
            if r.w is not None:
                k, v = r.w
                if need.get(k, 0) < v:
                    need[k] = v
        for w in writes:
            if w.w is not None:
                k, v = w.w
                if need.get(k, 0) < v:
                    need[k] = v
            for k, v in w.rs.items():
                if need.get(k, 0) < v:
                    need[k] = v
        for k, v in extra:
            if need.get(k, 0) < v:
                need[k] = v
        out = []
        seen = self.seen[engine]
        for k, v in need.items():
            if k == engine and (engine == "pe" or not self.same_engine_sync):
                continue
            if seen.get(k, 0) >= v:
                continue
            seen[k] = v
            out.append((k, v))
        return out

    def _mark(self, ev, reads, writes):
        k, v = ev
        for r in reads:
            if r.rs.get(k, 0) < v:
                r.rs[k] = v
        for w in writes:
            w.w = ev
            w.rs = {}

    def op(self, engine, fn, reads=(), writes=(), signal=True):
        waits = self._deps(engine, reads, writes)
        sem = self.sem[engine]
        if signal:
            self.cnt[engine] += 1
        ev = (engine, self.cnt[engine] if signal else self.cnt[engine] + 1)
        sems = self.sem

        def emit(eng):
            for k, v in waits:
                eng.wait_ge(sems[k], v)
            ins = fn(eng)
            if signal:
                ins.then_inc(sem, 1)

        self.prog[engine].append(emit)
        self._mark(ev, reads, writes)
        return ev

    def dma(self, queue, fn, reads=(), writes=()):
        keys, idx = self.dq[queue]
        k = keys[idx % len(keys)]
        self.dq[queue][1] = idx + 1
        prev = self.cnt[k]
        extra = [(k, prev)] if prev > 0 else []
        waits = self._deps(queue, reads, writes, extra)
        self.cnt[k] = prev + 16
        ev = (k, prev + 16)
        sems = self.sem

        def emit(eng):
            for kk, v in waits:
                eng.wait_ge(sems[kk], v)
            fn(eng).then_inc(sems[k], 16)

        self.prog[queue].append(emit)
        self._mark(ev, reads, writes)
        return ev

    def barrier(self):
        sems = self.sem
        for e in self.ENGS:
            waits = []
            seen = self.seen[e]
            for k, v in self.cnt.items():
                if v > 0 and seen.get(k, 0) < v:
                    seen[k] = v
                    waits.append((k, v))

            def emit(eng, waits=waits):
                for k, v in waits:
                    eng.wait_ge(sems[k], v)

            self.prog[e].append(emit)

    def flush(self):
        nc = self.nc
        prog = self.prog
        with nc.Block() as block:
            @block.sync
            def _(e):
                for f in prog["sp"]:
                    f(e)

            @block.scalar
            def _(e):
                for f in prog["act"]:
                    f(e)

            @block.vector
            def _(e):
                for f in prog["dve"]:
                    f(e)

            @block.gpsimd
            def _(e):
                for f in prog["pool"]:
                    f(e)

            @block.tensor
            def _(e):
                for f in prog["pe"]:
                    f(e)
        self.prog = {e: [] for e in self.ENGS}


class Rot:
    def __init__(self, items):
        self.items = items
        self.i = 0

    def next(self):
        it = self.items[self.i % len(self.items)]
        self.i += 1
        return it


def ntiles512(n_tok):
    out = []
    t = 0
    while t < n_tok:
        w = min(512, n_tok - t)
        out.append((t, w))
        t += w
    return out


class Builder:
    def __init__(self, nc, dbg=None, layers=(0, 1), stop_after=None):
        self.nc = nc
        self.dbg = dbg or []
        self.layers = layers
        self.stop_after = stop_after

    def sb(self, st, name, shape, dt):
        self._uid = getattr(self, "_uid", 0) + 1
        return st.enter_context(self.nc.sbuf_tensor("%s_%d" % (name, self._uid), list(shape), dt))

    def rot_sb(self, st, name, shape, dt, n):
        return Rot([(self.sb(st, "%s%d" % (name, i), shape, dt), Res()) for i in range(n)])

    def end_phase(self):
        self.S.barrier()
        self.S.flush()

    def declare(self):
        nc = self.nc
        di = lambda n, s, dt=F32: nc.dram_tensor(n, list(s), dt, kind="ExternalInput").ap()
        self.xin = di("xin", [T, D])
        self.cvec = di("cvec", [2, D])
        self.ada_w = di("ada_w", [DEPTH, D, 6 * D])
        self.ada_b = di("ada_b", [DEPTH, 6 * D])
        self.norm1_g = di("norm1_g", [DEPTH, D])
        self.norm2_g = di("norm2_g", [DEPTH, D])
        self.w_in = di("w_in", [DEPTH, D, INC])
        self.conv_w = di("conv_w", [DEPTH, 3, 512])
        self.natab = di("natab", [DEPTH, 2, 128, 8, NJ * 64])
        self.qkgain = di("qkgain", [DEPTH, 640])
        self.ropec = di("ropec", [TL, 640])
        self.ropes = di("ropes", [TL, 640])
        self.w_br = di("w_br", [DEPTH, 3, 512, D])
        self.w_out = di("w_out", [DEPTH, D, D])
        self.router_w = di("router_w", [DEPTH, D, NE])
        self.router_b = di("router_b", [DEPTH, NE])
        self.w_gu = di("w_gu", [DEPTH, NE, D, 2 * D])
        self.b_gu = di("b_gu", [DEPTH, NE, 2 * D])
        self.w_down = di("w_down", [DEPTH, NE, D, D])
        self.b_down = di("b_down", [DEPTH, NE, D])
        self.final_g = di("final_g", [1, D])
        self.out = nc.dram_tensor("out", [TL, D], F32, kind="ExternalOutput").ap()

        def scr(n, s, dt):
            kind = "ExternalOutput" if n in self.dbg else "Internal"
            return nc.dram_tensor(n, list(s), dt, kind=kind).ap()
        self.X = scr("X", [T, D], F32)
        self.MOD = scr("MOD", [DEPTH, 2, 6 * D], F32)
        self.FT = scr("FT", [INC, T], BF16)
        self.TM = scr("TM", [T, 1280], BF16)
        self.YT = scr("YT", [1536, T], BF16)
        self.XE = scr("XE", [NE * CAP, D], BF16)
        self.YE = scr("YE", [NE * CAP, D], F32)
        self.rX = [Res() for _ in range(NT)]
        self.rMOD = Res()
        self.rFT = Res()
        self.rTM = Res()
        self.rYT = Res()
        self.rXE = Res()
        self.rYE = Res()
        self.rOUT = Res()

    def build(self):
        nc = self.nc
        self.declare()
        with ExitStack() as gst:
            S = self.S = Sched(nc, gst)
            self.pA = Rot([(gst.enter_context(nc.psum_tensor("pA%d" % i, [128, 512], F32)), Res()) for i in range(4)])
            self.pB = Rot([(gst.enter_context(nc.psum_tensor("pB%d" % i, [128, 512], F32)), Res()) for i in range(2)])
            self.pT = Rot([(gst.enter_context(nc.psum_tensor("pT%d" % i, [128, 1024], BF16)), Res()) for i in range(2)])
            self.identf = self.sb(gst, "identf", [128, 128], F32)
            self.identb = self.sb(gst, "identb", [128, 128], BF16)
            self.r_id = Res()
            idf, idb = self.identf, self.identb
            S.op("pool", lambda e: e.memset(idf[:], 0.0), writes=[self.r_id])
            S.op("pool", lambda e: e.affine_select(out=idf[:], in_=idf[:], pattern=[[-1, 128]],
                                                   compare_op=ALU.not_equal, fill=1.0, base=0, channel_multiplier=1),
                 reads=[self.r_id], writes=[self.r_id])
            S.op("dve", lambda e: e.tensor_copy(out=idb[:], in_=idf[:]), reads=[self.r_id], writes=[self.r_id])
            self.DEST = self.sb(gst, "DEST", [128, NT, 4], I32)
            self.GATES = self.sb(gst, "GATES", [128, NT, 4], F32)
            self.rROUTE = [Res() for _ in range(NT)]
            self.end_phase()

            self.phase_mods()
            if self.stop_after == "mods":
                return self.finish()
            for l in self.layers:
                last = (l == DEPTH - 1)
                self.Xsrc = self.xin if l == 0 else self.X
                self.nt_act = 32 if last else NT
                self.phase_AB(l)
                if self.stop_after == "AB%d" % l:
                    return self.finish()
                self.phase_conv(l)
                self.phase_gqa(l, last)
                self.phase_na(l, last)
                if self.stop_after == "attn%d" % l:
                    return self.finish()
                self.phase_merge(l)
                if self.stop_after == "merge%d" % l:
                    return self.finish()
                self.phase_route(l)
                self.phase_experts(l)
                if self.stop_after == "exp%d" % l:
                    return self.finish()
                self.phase_combine(l, last)
                if self.stop_after == "comb%d" % l:
                    return self.finish()
            return self.finish()

    def finish(self):
        S = self.S
        S.barrier()
        S.flush()

    def phase_mods(self):
        nc, S = self.nc, self.S
        with ExitStack() as st:
            cs = self.sb(st, "cs", [128, 8, 2], F32)
            ca = self.sb(st, "ca", [128, 8, 2], F32)
            r_cs = Res()
            for j in range(2):
                S.dma("sp", lambda e, j=j: e.dma_start(out=cs[:, :, j], in_=self.cvec[j, :].rearrange("(p k) -> p k", k=8), allow_slow_non_contiguous=True),
                      writes=[r_cs])
            S.op("act", lambda e: e.activation(out=ca[:], in_=cs[:], func=AF.Silu), reads=[r_cs], writes=[r_cs])
            wrot = self.rot_sb(st, "adaw", [128, 8, 512], F32, 2)
            bias = self.sb(st, "adab", [2, 6 * D], F32)
            modsb = self.sb(st, "modsb", [2, 6 * D], F32)
            r_b = Res()
            r_m = Res()
            for l in range(DEPTH):
                S.dma("sp", lambda e, l=l: e.dma_start(out=bias[:], in_=self.ada_b[l:l + 1, :].to_broadcast([2, 6 * D])),
                      writes=[r_b])
                wv = self.ada_w[l].rearrange("(p k) f -> p k f", k=8)
                for fb in range(12):
                    wt, r_w = wrot.next()
                    S.dma("sp" if fb % 2 == 0 else "act",
                          lambda e, wt=wt, fb=fb, wv=wv: e.dma_start(out=wt[:], in_=wv[:, :, fb * 512:(fb + 1) * 512]),
                          writes=[r_w])
                    pt, r_p = self.pA.next()
                    for k in range(8):
                        S.op("pe", lambda e, pt=pt, wt=wt, k=k: e.matmul(pt[0:2, :], lhsT=ca[:, k, :], rhs=wt[:, k, :],
                                                                        start=(k == 0), stop=(k == 7)),
                             reads=[r_cs, r_w], writes=[r_p], signal=(k == 7))
                    S.op("dve", lambda e, pt=pt, fb=fb: e.tensor_tensor(out=modsb[:, fb * 512:(fb + 1) * 512], in0=pt[0:2, :],
                                                                        in1=bias[:, fb * 512:(fb + 1) * 512], op=ALU.add),
                         reads=[r_p, r_b], writes=[r_m])
                S.dma("sp", lambda e, l=l: e.dma_start(out=self.MOD[l], in_=modsb[:]), reads=[r_m], writes=[self.rMOD])
            self.end_phase()

    def load_feat(self, queue, tile, res, src_row):
        self.S.dma(queue, lambda e: e.dma_start(out=tile[:], in_=src_row.rearrange("(k p) -> p k", p=128),
                                                allow_slow_non_contiguous=True),
                   reads=[self.rMOD], writes=[res])

    def load_bc(self, queue, tile_ap, res, src_row2d, n=128):
        F = src_row2d.shape[-1]
        self.S.dma(queue, lambda e: e.dma_start(out=tile_ap, in_=src_row2d.to_broadcast([n, F])),
                   reads=[self.rMOD], writes=[res])

    def rms_rstd(self, st_tiles, xt, r_x, width):
        S = self.S
        junk, ss, ms, rstd, r_s = st_tiles
        S.op("act", lambda e: e.activation(out=junk[:, 0:width], in_=xt[:, 0:width], func=AF.Square, accum_out=ss[:, 0:1]),
             reads=[r_x], writes=[r_s])
        S.op("dve", lambda e: e.tensor_scalar(out=ms[:], in0=ss[:], scalar1=1.0 / width, scalar2=EPS, op0=ALU.mult, op1=ALU.add),
             reads=[r_s], writes=[r_s])
        S.op("act", lambda e: e.activation(out=ms[:], in_=ms[:], func=AF.Sqrt), reads=[r_s], writes=[r_s])
        S.op("dve", lambda e: e.reciprocal(out=rstd[:], in_=ms[:]), reads=[r_s], writes=[r_s])
        return rstd, r_s

    def stat_tiles(self, st, name, n=2):
        items = []
        for i in range(n):
            items.append((self.sb(st, "%sj%d" % (name, i), [128, 1024], BF16), self.sb(st, "%ss%d" % (name, i), [128, 1], F32),
                          self.sb(st, "%sm%d" % (name, i), [128, 1], F32), self.sb(st, "%sr%d" % (name, i), [128, 1], F32), Res()))
        return Rot(items)

    def phase_AB(self, l):
        nc, S = self.nc, self.S
        with ExitStack() as st:
            hT = self.sb(st, "hT", [128, 8, T], BF16)
            r_h = [Res() for _ in range(NT)]
            G1 = self.sb(st, "G1", [128, 2, 8], F32)
            SH1 = self.sb(st, "SH1", [128, 2, 8], F32)
            ng = self.sb(st, "ng", [128, 8], F32)
            r_g = Res()
            self.load_feat("sp", ng, r_g, self.norm1_g[l, :])
            for j in range(2):
                S.dma("sp", lambda e, j=j: e.dma_start(out=SH1[:, j, :], in_=self.MOD[l, j, 0:D].rearrange("(k p) -> p k", p=128),
                                                       allow_slow_non_contiguous=True), reads=[self.rMOD], writes=[r_g])
                S.dma("sp", lambda e, j=j: e.dma_start(out=G1[:, j, :], in_=self.MOD[l, j, D:2 * D].rearrange("(k p) -> p k", p=128),
                                                       allow_slow_non_contiguous=True), reads=[self.rMOD], writes=[r_g])
            for j in range(2):
                S.op("dve", lambda e, j=j: e.scalar_tensor_tensor(out=G1[:, j, :], in0=G1[:, j, :], scalar=1.0, in1=ng[:],
                                                                  op0=ALU.add, op1=ALU.mult), reads=[r_g], writes=[r_g])
            xrot = self.rot_sb(st, "xt", [128, D], F32, 2)
            xsrot = self.rot_sb(st, "xs", [128, D], BF16, 2)
            strot = self.stat_tiles(st, "st")
            for i in range(NT):
                xt, r_x = xrot.next()
                S.dma("sp", lambda e, xt=xt, i=i: e.dma_start(out=xt[:], in_=self.Xsrc[i * 128:(i + 1) * 128, :]),
                      reads=[self.rX[i]], writes=[r_x])
                stt = strot.next()
                rstd, r_s = self.rms_rstd(stt, xt, r_x, D)
                xs, r_xs = xsrot.next()
                S.op("act", lambda e, xs=xs, xt=xt, rstd=rstd: e.activation(out=xs[:], in_=xt[:], func=AF.Copy, scale=rstd[:, 0:1]),
                     reads=[r_x, r_s], writes=[r_xs])
                pt, r_p = self.pT.next()
                for k in range(8):
                    S.op("pe", lambda e, pt=pt, xs=xs, k=k: e.transpose(out=pt[:, k * 128:(k + 1) * 128], in_=xs[:, k * 128:(k + 1) * 128],
                                                                        identity=self.identb[:]),
                         reads=[r_xs, self.r_id], writes=[r_p], signal=(k == 7))
                j = 0 if i < 32 else 1
                for k in range(8):
                    S.op("act", lambda e, pt=pt, k=k, i=i, j=j: e.activation(out=hT[:, k, i * 128:(i + 1) * 128], in_=pt[:, k * 128:(k + 1) * 128],
                                                                             func=AF.Identity, scale=G1[:, j, k:k + 1], bias=SH1[:, j, k:k + 1]),
                         reads=[r_p, r_g], writes=[r_h[i]])
            wv = self.w_in[l].rearrange("(k p) c -> p k c", p=128)
            wrot = self.rot_sb(st, "wblk", [128, 8, 512], BF16, 2)
            stg = self.rot_sb(st, "stg", [128, T], BF16, 2)
            tmst = self.rot_sb(st, "tmst", [128, 512], BF16, 3)
            nts = ntiles512(T)
            ev_i = 0
            for cb in range(14):
                c0 = cb * 512
                cw = min(512, INC - c0)
                wt, r_w = wrot.next()
                S.dma("pool", lambda e, wt=wt, c0=c0, cw=cw: e.dma_start(out=wt[:, :, 0:cw], in_=wv[:, :, c0:c0 + cw]), writes=[r_w])
                tm_lo, tm_hi = max(c0, 2560), min(c0 + cw, 3840)
                for cc in range(cw // 128):
                    col = c0 + cc * 128
                    if 2560 <= col < 3840:
                        continue
                    is_gate = col >= 3840
                    sg, r_sg = stg.next()
                    for (t0, tw) in nts:
                        pt, r_p = self.pA.next()
                        rh = r_h[t0 // 128:(t0 + tw) // 128]
                        for k in range(8):
                            S.op("pe", lambda e, pt=pt, wt=wt, k=k, cc=cc, t0=t0, tw=tw: e.matmul(
                                pt[:, 0:tw], lhsT=wt[:, k, cc * 128:(cc + 1) * 128], rhs=hT[:, k, t0:t0 + tw], start=(k == 0), stop=(k == 7)),
                                reads=[r_w] + rh, writes=[r_p], signal=(k == 7))
                        if is_gate:
                            S.op("act", lambda e, pt=pt, sg=sg, t0=t0, tw=tw: e.activation(out=sg[:, t0:t0 + tw], in_=pt[:, 0:tw], func=AF.Sigmoid),
                                 reads=[r_p], writes=[r_sg])
                        elif ev_i % 2 == 0:
                            S.op("act", lambda e, pt=pt, sg=sg, t0=t0, tw=tw: e.activation(out=sg[:, t0:t0 + tw], in_=pt[:, 0:tw], func=AF.Copy),
                                 reads=[r_p], writes=[r_sg])
                        else:
                            S.op("dve", lambda e, pt=pt, sg=sg, t0=t0, tw=tw: e.tensor_copy(out=sg[:, t0:t0 + tw], in_=pt[:, 0:tw]),
                                 reads=[r_p], writes=[r_sg])
                        ev_i += 1
                    S.dma("sp", lambda e, sg=sg, col=col: e.dma_start(out=self.FT[col:col + 128, :], in_=sg[:]), reads=[r_sg], writes=[self.rFT])
                if tm_lo < tm_hi:
                    w0, wn = tm_lo - c0, tm_hi - tm_lo
                    for i in range(NT):
                        pt, r_p = self.pA.next()
                        for k in range(8):
                            S.op("pe", lambda e, pt=pt, wt=wt, k=k, i=i, w0=w0, wn=wn: e.matmul(
                                pt[:, 0:wn], lhsT=hT[:, k, i * 128:(i + 1) * 128], rhs=wt[:, k, w0:w0 + wn], start=(k == 0), stop=(k == 7)),
                                reads=[r_w, r_h[i]], writes=[r_p], signal=(k == 7))
                        ts, r_ts = tmst.next()
                        if i % 2 == 0:
                            S.op("act", lambda e, pt=pt, ts=ts, wn=wn: e.activation(out=ts[:, 0:wn], in_=pt[:, 0:wn], func=AF.Copy),
                                 reads=[r_p], writes=[r_ts])
                        else:
                            S.op("dve", lambda e, pt=pt, ts=ts, wn=wn: e.tensor_copy(out=ts[:, 0:wn], in_=pt[:, 0:wn]),
                                 reads=[r_p], writes=[r_ts])
                        S.dma("sp", lambda e, ts=ts, i=i, wn=wn, tm_lo=tm_lo: e.dma_start(
                            out=self.TM[i * 128:(i + 1) * 128, tm_lo - 2560:tm_lo - 2560 + wn], in_=ts[:, 0:wn]), reads=[r_ts], writes=[self.rTM])
            self.end_phase()

    def phase_conv(self, l):
        nc, S = self.nc, self.S
        with ExitStack() as st:
            cw = self.sb(st, "cw", [128, 4, 3], F32)
            r_cw = Res()
            for kk in range(3):
                S.dma("sp", lambda e, kk=kk: e.dma_start(out=cw[:, :, kk], in_=self.conv_w[l, kk, :].rearrange("(j p) -> p j", p=128),
                                                         allow_slow_non_contiguous=True), writes=[r_cw])
            inrot = self.rot_sb(st, "cin", [128, 3, T], BF16, 2)
            u = self.sb(st, "cu", [128, T], F32)
            acc = self.sb(st, "cacc", [128, T], F32)
            yrot = self.rot_sb(st, "cy", [128, T], BF16, 2)
            r_u, r_a = Res(), Res()
            for j in range(4):
                ci, r_ci = inrot.next()
                for b in range(3):
                    S.dma("sp" if b != 1 else "act", lambda e, ci=ci, b=b, j=j: e.dma_start(
                        out=ci[:, b, :], in_=self.FT[b * 512 + j * 128:b * 512 + (j + 1) * 128, :]), reads=[self.rFT], writes=[r_ci])
                S.op("pool", lambda e, ci=ci: e.tensor_tensor(out=u[:], in0=ci[:, 1, :], in1=ci[:, 2, :], op=ALU.mult),
                     reads=[r_ci], writes=[r_u])
                S.op("dve", lambda e, j=j: e.tensor_scalar(out=acc[:], in0=u[:], scalar1=cw[:, j, 1:2], scalar2=None, op0=ALU.mult),
                     reads=[r_u, r_cw], writes=[r_a])
                for (a, b) in ((0, TL), (TL, T)):
                    S.op("dve", lambda e, j=j, a=a, b=b: e.scalar_tensor_tensor(out=acc[:, a + 1:b], in0=u[:, a:b - 1], scalar=cw[:, j, 0:1],
                                                                              in1=acc[:, a + 1:b], op0=ALU.mult, op1=ALU.add),
                         reads=[r_u, r_cw, r_a], writes=[r_a])
                    S.op("dve", lambda e, j=j, a=a, b=b: e.scalar_tensor_tensor(out=acc[:, a:b - 1], in0=u[:, a + 1:b], scalar=cw[:, j, 2:3],
                                                                              in1=acc[:, a:b - 1], op0=ALU.mult, op1=ALU.add),
                         reads=[r_u, r_cw, r_a], writes=[r_a])
                y, r_y = yrot.next()
                S.op("pool", lambda e, y=y, ci=ci: e.tensor_tensor(out=y[:], in0=ci[:, 0, :], in1=acc[:], op=ALU.mult),
                     reads=[r_ci, r_a], writes=[r_y])
                S.dma("sp", lambda e, y=y, j=j: e.dma_start(out=self.YT[j * 128:(j + 1) * 128, :], in_=y[:]), reads=[r_y], writes=[self.rYT])
            self.end_phase()

    def attn_block(self, kt_ap_fn, q_ap, va_ap_fn, chunks, N, acc, r_acc, prot, reads, tab_fn=None, addrot=None, scale=0.125):
        S = self.S
        n = len(chunks)
        pend = []

        def qk(si):
            s = chunks[si]
            ps, r_ps = self.pA.next()
            kt = kt_ap_fn(s)
            S.op("pe", lambda e: e.matmul(ps[:, 0:N], lhsT=kt, rhs=q_ap, start=True, stop=True), reads=reads, writes=[r_ps])
            pe_t, r_pe = prot.next()
            tb = tab_fn(s) if tab_fn is not None else None
            if tb is not None:
                ad, r_ad = addrot.next()
                S.op("dve", lambda e: e.scalar_tensor_tensor(out=ad[:, 0:N], in0=ps[:, 0:N], scalar=scale, in1=tb, op0=ALU.mult, op1=ALU.add),
                     reads=[r_ps] + reads, writes=[r_ad])
                S.op("act", lambda e: e.activation(out=pe_t[:, 0:N], in_=ad[:, 0:N], func=AF.Exp), reads=[r_ad], writes=[r_pe])
            else:
                S.op("act", lambda e: e.activation(out=pe_t[:, 0:N], in_=ps[:, 0:N], func=AF.Exp, scale=scale), reads=[r_ps], writes=[r_pe])
            return (s, pe_t, r_pe)

        pend.append(qk(0))
        for si in range(n):
            if si + 1 < n:
                pend.append(qk(si + 1))
            s, pe_t, r_pe = pend.pop(0)
            va_ = va_ap_fn(s)
            S.op("pe", lambda e, va_=va_, pe_t=pe_t, si=si: e.matmul(acc[:, 0:N], lhsT=va_, rhs=pe_t[:, 0:N], start=(si == 0), stop=(si == n - 1)),
                 reads=[r_pe] + reads, writes=[r_acc], signal=(si == n - 1))

    def attn_finish(self, acc, r_acc, N, out_ap, r_out, recrot):
        S = self.S
        rec, r_rec = recrot.next()
        S.op("act", lambda e: e.activation(out=rec[0:64, 0:N], in_=acc[64:128, 0:N], func=AF.Copy), reads=[r_acc], writes=[r_rec])
        S.op("dve", lambda e: e.reciprocal(out=rec[0:64, 0:N], in_=rec[0:64, 0:N]), reads=[r_rec], writes=[r_rec])
        S.op("dve", lambda e: e.tensor_tensor(out=out_ap, in0=acc[0:64, 0:N], in1=rec[0:64, 0:N], op=ALU.mult),
             reads=[r_acc, r_rec], writes=[r_out])

    def phase_gqa(self, l, last):
        nc, S = self.nc, self.S
        with ExitStack() as st:
            QT = self.sb(st, "QT", [128, 4, T], BF16)
            KT2 = self.sb(st, "KT2", [128, 2, T], BF16)
            VA = self.sb(st, "VA", [128, NT, 2, 128], BF16)
            r_q = Res()
            r_vat = [Res() for _ in range(NT)]
            S.op("pool", lambda e: e.memset(VA[:, :, :, 64:128], 1.0), writes=r_vat)
            for i in range(NT):
                S.dma("sp" if i % 2 == 0 else "act", lambda e, i=i: e.dma_start(
                    out=VA[:, i, :, 0:64], in_=self.TM[i * 128:(i + 1) * 128, 1152:1280].rearrange("p (g d) -> p g d", g=2)),
                    reads=[self.rTM], writes=[r_vat[i]])
            gain = self.sb(st, "gain", [128, 640], F32)
            r_gn = Res()
            S.dma("sp", lambda e: e.dma_start(out=gain[:], in_=self.qkgain[l:l + 1, :].to_broadcast([128, 640])), writes=[r_gn])
            with ExitStack() as st2:
                inrot = self.rot_sb(st2, "gin", [128, 640], BF16, 2)
                sqrot = self.rot_sb(st2, "gsq", [128, 640], F32, 2)
                xnrot = self.rot_sb(st2, "gxn", [128, 640], F32, 2)
                swrot = self.rot_sb(st2, "gsw", [128, 640], F32, 2)
                cosrot = self.rot_sb(st2, "gcos", [128, 640], F32, 2)
                sinrot = self.rot_sb(st2, "gsin", [128, 640], F32, 2)
                qbrot = self.rot_sb(st2, "gqb", [128, 640], BF16, 2)
                ssrot = Rot([(self.sb(st2, "gss%d" % i, [128, 10], F32), Res()) for i in range(2)])
                for i in range(NT):
                    xi, r_xi = inrot.next()
                    S.dma("sp", lambda e, xi=xi, i=i: e.dma_start(out=xi[:], in_=self.TM[i * 128:(i + 1) * 128, 512:1152]), reads=[self.rTM], writes=[r_xi])
                    sq, r_sq = sqrot.next()
                    S.op("pool", lambda e, sq=sq, xi=xi: e.tensor_tensor(out=sq[:], in0=xi[:], in1=xi[:], op=ALU.mult), reads=[r_xi], writes=[r_sq])
                    ss, r_ss = ssrot.next()
                    S.op("dve", lambda e, ss=ss, sq=sq: e.tensor_reduce(out=ss[:], in_=sq[:].rearrange("p (h d) -> p h d", d=64), axis=AX.X, op=ALU.add),
                         reads=[r_sq], writes=[r_ss])
                    S.op("dve", lambda e, ss=ss: e.tensor_scalar(out=ss[:], in0=ss[:], scalar1=1.0 / 64, scalar2=EPS, op0=ALU.mult, op1=ALU.add),
                         reads=[r_ss], writes=[r_ss])
                    S.op("act", lambda e, ss=ss: e.activation(out=ss[:], in_=ss[:], func=AF.Sqrt), reads=[r_ss], writes=[r_ss])
                    S.op("dve", lambda e, ss=ss: e.reciprocal(out=ss[:], in_=ss[:]), reads=[r_ss], writes=[r_ss])
                    xn, r_xn = xnrot.next()
                    r_xh = [Res() for _ in range(10)]
                    for h in range(10):
                        S.op("dve", lambda e, xn=xn, xi=xi, ss=ss, h=h: e.tensor_scalar(
                            out=xn[:, h * 64:(h + 1) * 64], in0=xi[:, h * 64:(h + 1) * 64], scalar1=ss[:, h:h + 1], scalar2=None, op0=ALU.mult),
                            reads=[r_xi, r_ss, r_xn], writes=[r_xh[h]])
                    S.op("dve", lambda e, xn=xn: e.tensor_tensor(out=xn[:], in0=xn[:], in1=gain[:], op=ALU.mult), reads=r_xh + [r_gn], writes=[r_xn])
                    qb, r_qb = qbrot.next()
                    if i < 32:
                        co, r_co = cosrot.next()
                        si_, r_si = sinrot.next()
                        S.dma("act", lambda e, co=co, i=i: e.dma_start(out=co[:], in_=self.ropec[i * 128:(i + 1) * 128, :]), writes=[r_co])
                        S.dma("act", lambda e, si_=si_, i=i: e.dma_start(out=si_[:], in_=self.ropes[i * 128:(i + 1) * 128, :]), writes=[r_si])
                        sw, r_sw = swrot.next()
                        xv = xn[:].rearrange("p (g two d) -> p g two d", two=2, d=16)
                        swv = sw[:].rearrange("p (g two d) -> p g two d", two=2, d=16)
                        S.op("pool", lambda e, swv=swv, xv=xv: e.tensor_copy(out=swv[:, :, 0, :], in_=xv[:, :, 1, :]), reads=[r_xn], writes=[r_sw])
                        S.op("pool", lambda e, swv=swv, xv=xv: e.tensor_copy(out=swv[:, :, 1, :], in_=xv[:, :, 0, :]), reads=[r_xn, r_sw], writes=[r_sw])
                        S.op("pool", lambda e, sw=sw, si_=si_: e.tensor_tensor(out=sw[:], in0=sw[:], in1=si_[:], op=ALU.mult), reads=[r_sw, r_si], writes=[r_sw])
                        S.op("dve", lambda e, xn=xn, co=co: e.tensor_tensor(out=xn[:], in0=xn[:], in1=co[:], op=ALU.mult), reads=[r_xn, r_co], writes=[r_xn])
                        S.op("dve", lambda e, qb=qb, xn=xn, sw=sw: e.tensor_tensor(out=qb[:], in0=xn[:], in1=sw[:], op=ALU.add), reads=[r_xn, r_sw], writes=[r_qb])
                    else:
                        S.op("dve", lambda e, qb=qb, xn=xn: e.tensor_copy(out=qb[:], in_=xn[:]), reads=[r_xn], writes=[r_qb])
                    pt, r_p = self.pT.next()
                    for c in range(5):
                        S.op("pe", lambda e, pt=pt, qb=qb, c=c: e.transpose(out=pt[:, c * 128:(c + 1) * 128], in_=qb[:, c * 128:(c + 1) * 128], identity=self.identb[:]),
                             reads=[r_qb, self.r_id], writes=[r_p], signal=(c == 4))
                    tsl = slice(i * 128, (i + 1) * 128)
                    S.op("act", lambda e, pt=pt, tsl=tsl: e.activation(out=QT[:, :, tsl], in_=pt[:, 0:512].rearrange("p (c t) -> p c t", c=4), func=AF.Copy),
                         reads=[r_p], writes=[r_q])
                    S.op("dve", lambda e, pt=pt, tsl=tsl: e.tensor_copy(out=KT2[0:64, 0, tsl], in_=pt[0:64, 512:640]), reads=[r_p, r_q], writes=[r_q])
                    S.op("dve", lambda e, pt=pt, tsl=tsl: e.tensor_copy(out=KT2[64:128, 1, tsl], in_=pt[64:128, 512:640]), reads=[r_p, r_q], writes=[r_q])
                    S.op("act", lambda e, pt=pt, tsl=tsl: e.activation(out=KT2[64:128, 0, tsl], in_=pt[0:64, 512:640], func=AF.Copy), reads=[r_p, r_q], writes=[r_q])
                    S.op("act", lambda e, pt=pt, tsl=tsl: e.activation(out=KT2[0:64, 1, tsl], in_=pt[64:128, 512:640], func=AF.Copy), reads=[r_p, r_q], writes=[r_q])
                self.S.barrier()
            prot = self.rot_sb(st, "gpe", [128, 512], BF16, 3)
            recrot = self.rot_sb(st, "grec", [64, 512], F32, 2)
            ysrot = self.rot_sb(st, "gys", [64, T], BF16, 2)
            for h in range(8):
                g, c, hh = h // 4, h // 2, h % 2
                ps_ = slice(hh * 64, (hh + 1) * 64)
                ys, r_ys = ysrot.next()
                blocks = [(n * 512, 512, list(range(NT))) for n in range(8)]
                if not last:
                    blocks.append((TL, TC, [32, 33]))
                for (q0, N, chunks) in blocks:
                    acc, r_acc = self.pB.next()
                    self.attn_block(lambda s: KT2[ps_, g, s * 128:(s + 1) * 128], QT[ps_, c, q0:q0 + N],
                                    lambda s: VA[:, s, g, :], chunks, N, acc, r_acc, prot, [r_q] + r_vat)
                    self.attn_finish(acc, r_acc, N, ys[:, q0:q0 + N], r_ys, recrot)
                ncol = T if not last else TL
                S.dma("sp", lambda e, ys=ys, h=h, ncol=ncol: e.dma_start(out=self.YT[1024 + h * 64:1024 + (h + 1) * 64, 0:ncol], in_=ys[:, 0:ncol]),
                      reads=[r_ys], writes=[self.rYT])
            self.end_phase()

    def phase_na(self, l, last):
        nc, S = self.nc, self.S
        qblocks = [(0, 4, 1, [0, 2, 4, 6])]
        for r0 in range(4, 60, 8):
            qblocks.append((r0, 8, 0, list(range(r0 - 4, r0 + 12, 2))))
        qblocks.append((60, 1, 0, [56, 58, 60, 62]))
        qblocks.append((61, 3, 1, [56, 58, 60, 62]))
        with ExitStack() as st:
            qkrot = self.rot_sb(st, "nqk", [128, 2, T], BF16, 2)
            varot = self.rot_sb(st, "nva", [128, NT, 2, 128], BF16, 2)
            tabrot = self.rot_sb(st, "ntab", [128, 2, 2, NJ * 64], BF16, 2)
            prot = self.rot_sb(st, "npe", [128, 512], BF16, 3)
            addrot = self.rot_sb(st, "nad", [128, 512], F32, 2)
            recrot = self.rot_sb(st, "nrec", [64, 512], F32, 2)
            ysrot = self.rot_sb(st, "nys", [64, T], BF16, 2)
            for c in range(4):
                qk, r_qk = qkrot.next()
                va, r_va = varot.next()
                tab, r_tab = tabrot.next()
                S.dma("sp", lambda e, qk=qk, c=c: e.dma_start(out=qk[:, 0, :], in_=self.FT[1536 + c * 128:1536 + (c + 1) * 128, :]), reads=[self.rFT], writes=[r_qk])
                S.dma("act", lambda e, qk=qk, c=c: e.dma_start(out=qk[:, 1, :], in_=self.FT[2048 + c * 128:2048 + (c + 1) * 128, :]), reads=[self.rFT], writes=[r_qk])
                S.op("pool", lambda e, va=va: e.memset(va[:, :, :, 64:128], 1.0), writes=[r_va])
                r_vt = [Res() for _ in range(NT)]
                for i in range(NT):
                    S.dma("sp" if i % 2 == 0 else "act", lambda e, va=va, i=i, c=c: e.dma_start(
                        out=va[:, i, :, 0:64], in_=self.TM[i * 128:(i + 1) * 128, c * 128:(c + 1) * 128].rearrange("p (g d) -> p g d", g=2)),
                        reads=[self.rTM, r_va], writes=[r_vt[i]])
                for v in range(2):
                    S.dma("pool", lambda e, tab=tab, v=v, c=c: e.dma_start(out=tab[:, v, :, :], in_=self.natab[l, v, :, 2 * c:2 * c + 2, :]), writes=[r_tab])
                for hh in range(2):
                    h = 2 * c + hh
                    ps_ = slice(hh * 64, (hh + 1) * 64)
                    ys, r_ys = ysrot.next()
                    for (r0, R, v, krows) in qblocks:
                        N = 64 * R
                        q0 = r0 * 64
                        chunks = [kr // 2 for kr in krows] + [32, 33]

                        def tab_fn(s, r0=r0, v=v, N=N, tab=tab, hh=hh):
                            if s >= 32:
                                return None
                            j0 = r0 - 2 * s + JOFF
                            return tab[:, v, hh, j0 * 64:j0 * 64 + N]
                        acc, r_acc = self.pB.next()
                        self.attn_block(lambda s, qk=qk: qk[ps_, 1, s * 128:(s + 1) * 128], qk[ps_, 0, q0:q0 + N],
                                        lambda s, va=va, hh=hh: va[:, s, hh, :], chunks, N, acc, r_acc, prot, [r_qk, r_va, r_tab] + r_vt,
                                        tab_fn=tab_fn, addrot=addrot)
                        self.attn_finish(acc, r_acc, N, ys[:, q0:q0 + N], r_ys, recrot)
                    if not last:
                        acc, r_acc = self.pB.next()
                        self.attn_block(lambda s, qk=qk: qk[ps_, 1, s * 128:(s + 1) * 128], qk[ps_, 0, TL:T],
                                        lambda s, va=va, hh=hh: va[:, s, hh, :], [32, 33], TC, acc, r_acc, prot, [r_qk, r_va] + r_vt)
                        self.attn_finish(acc, r_acc, TC, ys[:, TL:T], r_ys, recrot)
                    ncol = T if not last else TL
                    S.dma("sp", lambda e, ys=ys, h=h, ncol=ncol: e.dma_start(out=self.YT[512 + h * 64:512 + (h + 1) * 64, 0:ncol], in_=ys[:, 0:ncol]),
                          reads=[r_ys], writes=[self.rYT])
            self.end_phase()

    def phase_merge(self, l):
        nc, S = self.nc, self.S
        ntok = self.nt_act * 128
        with ExitStack() as st:
            WBR = self.sb(st, "WBR", [128, 12, D], BF16)
            WO = self.sb(st, "WO", [128, 8, D], BF16)
            r_w = Res()
            for b in range(3):
                S.dma("pool", lambda e, b=b: e.dma_start(out=WBR[:, b * 4:(b + 1) * 4, :], in_=self.w_br[l, b].rearrange("(k p) f -> p k f", p=128)), writes=[r_w])
            S.dma("pool", lambda e: e.dma_start(out=WO[:], in_=self.w_out[l].rearrange("(k p) f -> p k f", p=128)), writes=[r_w])
            GT = self.sb(st, "GT1", [128, 2, D], F32)
            r_gt = Res()
            for j in range(2):
                self.load_bc("sp", GT[:, j, :], r_gt, self.MOD[l, j:j + 1, 2 * D:3 * D])
            ytrot = self.rot_sb(st, "mYT", [128, 12, 512], BF16, 2)
            sgrot = self.rot_sb(st, "mSG", [128, 24, 512], BF16, 2)
            mrot = self.rot_sb(st, "mT", [128, 8, 512], BF16, 2)
            m0rot = self.rot_sb(st, "m0", [128, 512], F32, 2)
            m1rot = self.rot_sb(st, "m1", [128, 512], F32, 2)
            m2rot = self.rot_sb(st, "m2", [128, 512], F32, 2)
            xrot = self.rot_sb(st, "mx", [128, D], F32, 2)
            xorot = self.rot_sb(st, "mxo", [128, D], F32, 2)
            ytv = self.YT.rearrange("(j p) t -> p j t", p=128)
            sgv = self.FT[3840:INC, :].rearrange("(j p) t -> p j t", p=128)
            for (t0, tw) in ntiles512(ntok):
                yt, r_yt = ytrot.next()
                sg, r_sg = sgrot.next()
                S.dma("sp", lambda e, yt=yt, t0=t0, tw=tw: e.dma_start(out=yt[:, :, 0:tw], in_=ytv[:, :, t0:t0 + tw]), reads=[self.rYT], writes=[r_yt])
                S.dma("act", lambda e, sg=sg, t0=t0, tw=tw: e.dma_start(out=sg[:, :, 0:tw], in_=sgv[:, :, t0:t0 + tw]), reads=[self.rFT], writes=[r_sg])
                mT, r_m = mrot.next()
                for f in range(8):
                    pb = []
                    for b in range(3):
                        pt, r_p = self.pA.next()
                        for kc in range(4):
                            S.op("pe", lambda e, pt=pt, b=b, kc=kc, f=f, yt=yt, tw=tw: e.matmul(
                                pt[:, 0:tw], lhsT=WBR[:, b * 4 + kc, f * 128:(f + 1) * 128], rhs=yt[:, b * 4 + kc, 0:tw], start=(kc == 0), stop=(kc == 3)),
                                reads=[r_w, r_yt], writes=[r_p], signal=(kc == 3))
                        pb.append((pt, r_p))
                    a0, r_a0 = m0rot.next()
                    a1, r_a1 = m1rot.next()
                    a2, r_a2 = m2rot.next()
                    for b, (a, r_a) in enumerate(((a0, r_a0), (a1, r_a1), (a2, r_a2))):
                        pt, r_p = pb[b]
                        S.op("dve", lambda e, a=a, pt=pt, sg=sg, b=b, f=f, tw=tw: e.tensor_tensor(out=a[:, 0:tw], in0=pt[:, 0:tw], in1=sg[:, b * 8 + f, 0:tw], op=ALU.mult),
                             reads=[r_p, r_sg], writes=[r_a])
                    S.op("pool", lambda e, a0=a0, a1=a1, tw=tw: e.tensor_tensor(out=a0[:, 0:tw], in0=a0[:, 0:tw], in1=a1[:, 0:tw], op=ALU.add),
                         reads=[r_a0, r_a1], writes=[r_a0])
                    S.op("pool", lambda e, a0=a0, a2=a2, mT=mT, f=f, tw=tw: e.tensor_tensor(out=mT[:, f, 0:tw], in0=a0[:, 0:tw], in1=a2[:, 0:tw], op=ALU.add),
                         reads=[r_a0, r_a2], writes=[r_m])
                for ts in range(tw // 128):
                    i = t0 // 128 + ts
                    j = 0 if i < 32 else 1
                    xt, r_x = xrot.next()
                    S.dma("sp", lambda e, xt=xt, i=i: e.dma_start(out=xt[:], in_=self.Xsrc[i * 128:(i + 1) * 128, :]), reads=[self.rX[i]], writes=[r_x])
                    xo, r_xo = xorot.next()
                    for half in range(2):
                        pt, r_p = self.pA.next()
                        for f in range(8):
                            S.op("pe", lambda e, pt=pt, f=f, mT=mT, ts=ts, half=half: e.matmul(
                                pt[:, :], lhsT=mT[:, f, ts * 128:(ts + 1) * 128], rhs=WO[:, f, half * 512:(half + 1) * 512], start=(f == 0), stop=(f == 7)),
                                reads=[r_w, r_m], writes=[r_p], signal=(f == 7))
                        hs = slice(half * 512, (half + 1) * 512)
                        S.op("dve", lambda e, pt=pt, xo=xo, hs=hs, j=j: e.tensor_tensor(out=xo[:, hs], in0=pt[:, :], in1=GT[:, j, hs], op=ALU.mult),
                             reads=[r_p, r_gt], writes=[r_xo])
                    S.op("pool", lambda e, xo=xo, xt=xt: e.tensor_tensor(out=xo[:], in0=xo[:], in1=xt[:], op=ALU.add), reads=[r_xo, r_x], writes=[r_xo])
                    S.dma("sp", lambda e, xo=xo, i=i: e.dma_start(out=self.X[i * 128:(i + 1) * 128, :], in_=xo[:]), reads=[r_xo], writes=[self.rX[i]])
            self.end_phase()
            self.Xsrc = self.X

    def phase_route(self, l):
        nc, S = self.nc, self.S
        with ExitStack() as st:
            G2 = self.sb(st, "G2", [128, 2, D], F32)
            SH2 = self.sb(st, "SH2", [128, 2, D], F32)
            NG = self.sb(st, "NG2", [128, D], F32)
            r_g = Res()
            self.load_bc("sp", NG[:], r_g, self.norm2_g[l:l + 1, :])
            for j in range(2):
                self.load_bc("sp", SH2[:, j, :], r_g, self.MOD[l, j:j + 1, 3 * D:4 * D])
                self.load_bc("act", G2[:, j, :], r_g, self.MOD[l, j:j + 1, 4 * D:5 * D])
            for j in range(2):
                S.op("dve", lambda e, j=j: e.scalar_tensor_tensor(out=G2[:, j, :], in0=G2[:, j, :], scalar=1.0, in1=NG[:], op0=ALU.add, op1=ALU.mult),
                     reads=[r_g], writes=[r_g])
            RW = self.sb(st, "RW", [128, 8, NE], F32)
            RB = self.sb(st, "RB", [128, NE], F32)
            S.dma("sp", lambda e: e.dma_start(out=RW[:], in_=self.router_w[l].rearrange("(k p) e -> p k e", p=128)), writes=[r_g])
            self.load_bc("sp", RB[:], r_g, self.router_b[l:l + 1, :])
            UTf = self.sb(st, "UTf", [128, 128], F32)
            UT = self.sb(st, "UT", [128, 128], BF16)
            ONES = self.sb(st, "ONES", [128, 128], BF16)
            EB = self.sb(st, "EB", [128, NE], F32)
            CNT = self.sb(st, "CNT", [128, NE], F32)
            r_c = Res()
            r_cnt = Res()
            S.op("pool", lambda e: e.memset(UTf[:], 1.0), writes=[r_c])
            S.op("pool", lambda e: e.affine_select(out=UTf[:], in_=UTf[:], pattern=[[1, 128]], compare_op=ALU.is_gt, fill=0.0, base=0, channel_multiplier=-1),
                 reads=[r_c], writes=[r_c])
            S.op("dve", lambda e: e.tensor_copy(out=UT[:], in_=UTf[:]), reads=[r_c], writes=[r_c])
            S.op("pool", lambda e: e.memset(ONES[:], 1.0), writes=[r_c])
            S.op("pool", lambda e: e.iota(EB[:], pattern=[[CAP, NE]], base=0, channel_multiplier=0, allow_small_or_imprecise_dtypes=True), writes=[r_c])
            S.op("pool", lambda e: e.memset(CNT[:], 0.0), writes=[r_cnt])
            xrot = self.rot_sb(st, "rx", [128, D], F32, 2)
            hrot = self.rot_sb(st, "rh", [128, D], F32, 2)
            hbrot = self.rot_sb(st, "rhb", [128, D], BF16, 6)
            htrot = self.rot_sb(st, "rht", [128, 8, 128], F32, 2)
            strot = self.stat_tiles(st, "rst")
            smrot = Rot([({n: self.sb(st, "rs%s%d" % (n, i), [128, w], dt) for n, w, dt in (
                ("lg", NE, F32), ("t8", 8, F32), ("nm", 1, F32), ("e4", 4, F32), ("sm", 1, F32), ("mk", NE, BF16),
                ("pos", NE, F32), ("oh", NE, F32), ("df", 4, F32))}, Res()) for i in range(2)])
            for i in range(self.nt_act):
                j = 0 if i < 32 else 1
                xt, r_x = xrot.next()
                S.dma("sp", lambda e, xt=xt, i=i: e.dma_start(out=xt[:], in_=self.X[i * 128:(i + 1) * 128, :]), reads=[self.rX[i]], writes=[r_x])
                stt = strot.next()
                rstd, r_s = self.rms_rstd(stt, xt, r_x, D)
                h2, r_h = hrot.next()
                S.op("dve", lambda e, h2=h2, xt=xt, rstd=rstd, j=j: e.scalar_tensor_tensor(out=h2[:], in0=xt[:], scalar=rstd[:, 0:1], in1=G2[:, j, :], op0=ALU.mult, op1=ALU.mult),
                     reads=[r_x, r_s, r_g], writes=[r_h])
                S.op("pool", lambda e, h2=h2, j=j: e.tensor_tensor(out=h2[:], in0=h2[:], in1=SH2[:, j, :], op=ALU.add), reads=[r_h, r_g], writes=[r_h])
                hb, r_hb = hbrot.next()
                S.op("act", lambda e, hb=hb, h2=h2: e.activation(out=hb[:], in_=h2[:], func=AF.Copy), reads=[r_h], writes=[r_hb])
                ht, r_ht = htrot.next()
                for half in range(2):
                    pt, r_p = self.pA.next()
                    for kk in range(4):
                        k = half * 4 + kk
                        S.op("pe", lambda e, pt=pt, h2=h2, k=k, kk=kk: e.transpose(out=pt[:, kk * 128:(kk + 1) * 128], in_=h2[:, k * 128:(k + 1) * 128], identity=self.identf[:]),
                             reads=[r_h, self.r_id], writes=[r_p], signal=(kk == 3))
                    if half == 0:
                        S.op("act", lambda e, pt=pt, ht=ht: e.activation(out=ht[:, 0:4, :], in_=pt[:, :].rearrange("p (k t) -> p k t", k=4), func=AF.Copy), reads=[r_p], writes=[r_ht])
                    else:
                        S.op("dve", lambda e, pt=pt, ht=ht: e.tensor_copy(out=ht[:, 4:8, :], in_=pt[:, :].rearrange("p (k t) -> p k t", k=4)), reads=[r_p, r_ht], writes=[r_ht])
                pl, r_pl = self.pB.next()
                for k in range(8):
                    S.op("pe", lambda e, pl=pl, ht=ht, k=k: e.matmul(pl[:, 0:NE], lhsT=ht[:, k, :], rhs=RW[:, k, :], start=(k == 0), stop=(k == 7)),
                         reads=[r_ht, r_g], writes=[r_pl], signal=(k == 7))
                sm, r_sm = smrot.next()
                S.op("dve", lambda e, sm=sm, pl=pl: e.tensor_tensor(out=sm["lg"][:], in0=pl[:, 0:NE], in1=RB[:], op=ALU.add), reads=[r_pl, r_g], writes=[r_sm])
                S.op("dve", lambda e, sm=sm: e.max(out=sm["t8"][:], in_=sm["lg"][:]), reads=[r_sm], writes=[r_sm])
                S.op("dve", lambda e, sm=sm: e.tensor_scalar(out=sm["nm"][:], in0=sm["t8"][:, 0:1], scalar1=-1.0, scalar2=None, op0=ALU.mult), reads=[r_sm], writes=[r_sm])
                S.op("act", lambda e, sm=sm: e.activation(out=sm["e4"][:], in_=sm["t8"][:, 0:4], func=AF.Exp, bias=sm["nm"][:, 0:1], accum_out=sm["sm"][:, 0:1]), reads=[r_sm], writes=[r_sm])
                S.op("dve", lambda e, sm=sm: e.reciprocal(out=sm["sm"][:], in_=sm["sm"][:]), reads=[r_sm], writes=[r_sm])
                S.op("dve", lambda e, sm=sm, i=i: e.tensor_scalar(out=self.GATES[:, i, :], in0=sm["e4"][:], scalar1=sm["sm"][:, 0:1], scalar2=None, op0=ALU.mult),
                     reads=[r_sm], writes=[self.rROUTE[i]])
                S.op("dve", lambda e, sm=sm: e.tensor_scalar(out=sm["mk"][:], in0=sm["lg"][:], scalar1=sm["t8"][:, 3:4], scalar2=None, op0=ALU.is_ge), reads=[r_sm], writes=[r_sm])
                pp, r_pp = self.pB.next()
                S.op("pe", lambda e, pp=pp, sm=sm: e.matmul(pp[:, 0:NE], lhsT=UT[:], rhs=sm["mk"][:], start=True, stop=True), reads=[r_sm, r_c], writes=[r_pp], signal=False)
                S.op("pe", lambda e, pp=pp, sm=sm: e.matmul(pp[:, 64:64 + NE], lhsT=ONES[:], rhs=sm["mk"][:], start=True, stop=True), reads=[r_sm, r_c], writes=[r_pp])
                S.op("dve", lambda e, sm=sm, pp=pp: e.tensor_tensor(out=sm["pos"][:], in0=pp[:, 0:NE], in1=CNT[:], op=ALU.add), reads=[r_pp, r_cnt, r_sm], writes=[r_sm])
                S.op("dve", lambda e, pp=pp: e.tensor_tensor(out=CNT[:], in0=pp[:, 64:64 + NE], in1=CNT[:], op=ALU.add), reads=[r_pp, r_cnt], writes=[r_cnt])
                S.op("dve", lambda e, sm=sm: e.scalar_tensor_tensor(out=sm["pos"][:], in0=sm["pos"][:], scalar=float(CAP - 1), in1=EB[:], op0=ALU.min, op1=ALU.add),
                     reads=[r_sm, r_c], writes=[r_sm])
                for k in range(4):
                    S.op("dve", lambda e, sm=sm, k=k: e.scalar_tensor_tensor(out=sm["oh"][:], in0=sm["lg"][:], scalar=sm["t8"][:, k:k + 1], in1=sm["pos"][:], op0=ALU.is_equal, op1=ALU.mult),
                         reads=[r_sm], writes=[r_sm])
                    S.op("dve", lambda e, sm=sm, k=k: e.reduce_sum(out=sm["df"][:, k:k + 1], in_=sm["oh"][:], axis=AX.X), reads=[r_sm], writes=[r_sm])
                S.op("dve", lambda e, sm=sm, i=i: e.tensor_copy(out=self.DEST[:, i, :], in_=sm["df"][:]), reads=[r_sm, self.rROUTE[i]], writes=[self.rROUTE[i]])
                for k in range(4):
                    S.dma("pool", lambda e, hb=hb, i=i, k=k: e.indirect_dma_start(
                        out=self.XE[:, :], out_offset=bass.IndirectOffsetOnAxis(ap=self.DEST[:, i, k:k + 1], axis=0), in_=hb[:], in_offset=None),
                        reads=[r_hb, self.rROUTE[i]], writes=[self.rXE])
            self.end_phase()

    def phase_experts(self, l):
        nc, S = self.nc, self.S
        NTE = CAP // 512
        with ExitStack() as st:
            xerot = self.rot_sb(st, "xe", [128, 4, D], BF16, 2)
            xtrot = self.rot_sb(st, "xeT", [128, 8, 512], BF16, 2)
            wgrot = self.rot_sb(st, "wg", [128, 8, D], BF16, 2)
            wurot = self.rot_sb(st, "wu", [128, 8, D], BF16, 2)
            wdrot = self.rot_sb(st, "wd", [128, 8, D], BF16, 1)
            stgrot = self.rot_sb(st, "wstg", [128, 4, 512], F32, 3)
            bgrot = self.rot_sb(st, "bgu", [128, 8, 2], F32, 2)
            bdrot = self.rot_sb(st, "bd", [128, D], F32, 2)
            atrot = self.rot_sb(st, "aT", [128, 8, 512], BF16, 2)
            gsrot = self.rot_sb(st, "gs", [128, 512], F32, 2)
            sgrot = self.rot_sb(st, "sg", [128, 512], F32, 2)
            usrot = self.rot_sb(st, "us", [128, 512], F32, 2)
            yrot = self.rot_sb(st, "ye", [128, D], F32, 2)
            for e_ in range(NE):
                wg, r_wg = wgrot.next()
                wu, r_wu = wurot.next()
                wd, r_wd = wdrot.next()
                wguv = self.w_gu[l, e_].rearrange("(k p) c -> p k c", p=128)
                for kh in range(2):
                    for q in range(4):
                        sgt, r_st = stgrot.next()
                        S.dma("sp" if (kh * 4 + q) % 2 == 0 else "act", lambda e, sgt=sgt, q=q, kh=kh, wguv=wguv: e.dma_start(
                            out=sgt[:], in_=wguv[:, kh * 4:(kh + 1) * 4, q * 512:(q + 1) * 512]), writes=[r_st])
                        sv = sgt[:].rearrange("p k (c two) -> p k c two", two=2)
                        S.op("pool", lambda e, wg=wg, sv=sv, q=q, kh=kh: e.tensor_copy(out=wg[:, kh * 4:(kh + 1) * 4, q * 256:(q + 1) * 256], in_=sv[:, :, :, 0]),
                             reads=[r_st], writes=[r_wg])
                        S.op("act", lambda e, wu=wu, sv=sv, q=q, kh=kh: e.activation(out=wu[:, kh * 4:(kh + 1) * 4, q * 256:(q + 1) * 256], in_=sv[:, :, :, 1], func=AF.Copy),
                             reads=[r_st], writes=[r_wu])
                S.dma("pool", lambda e, wd=wd, e_=e_: e.dma_start(out=wd[:], in_=self.w_down[l, e_].rearrange("(k p) c -> p k c", p=128)), writes=[r_wd])
                bg, r_bg = bgrot.next()
                S.dma("sp", lambda e, bg=bg, e_=e_: e.dma_start(out=bg[:], in_=self.b_gu[l, e_, :].rearrange("(k p two) -> p k two", p=128, two=2),
                                                               allow_slow_non_contiguous=True), writes=[r_bg])
                bd, r_bd = bdrot.next()
                S.dma("act", lambda e, bd=bd, e_=e_: e.dma_start(out=bd[:], in_=self.b_down[l, e_:e_ + 1, :].to_broadcast([128, D])), writes=[r_bd])
                for nt in range(NTE):
                    row0 = e_ * CAP + nt * 512
                    self.expert_ntile(row0, (wg, r_wg), (wu, r_wu), (wd, r_wd), (bg, r_bg), (bd, r_bd),
                                      xerot, xtrot, atrot, gsrot, sgrot, usrot, yrot)
            self.end_phase()

    def expert_ntile(self, row0, wg_, wu_, wd_, bg_, bd_, xerot, xtrot, atrot, gsrot, sgrot, usrot, yrot):
        S = self.S
        wg, r_wg = wg_
        wu, r_wu = wu_
        wd, r_wd = wd_
        bg, r_bg = bg_
        bd, r_bd = bd_
        xe, r_xe = xerot.next()
        S.dma("sp", lambda e: e.dma_start(out=xe[:], in_=self.XE[row0:row0 + 512, :].rearrange("(j p) d -> p j d", p=128)),
              reads=[self.rXE], writes=[r_xe])
        xT, r_xT = xtrot.next()
        for jt in range(4):
            pt, r_p = self.pT.next()
            for k in range(8):
                S.op("pe", lambda e, pt=pt, jt=jt, k=k: e.transpose(out=pt[:, k * 128:(k + 1) * 128], in_=xe[:, jt, k * 128:(k + 1) * 128], identity=self.identb[:]),
                     reads=[r_xe, self.r_id], writes=[r_p], signal=(k == 7))
            S.op("dve", lambda e, pt=pt, jt=jt: e.tensor_copy(out=xT[:, :, jt * 128:(jt + 1) * 128], in_=pt[:, :].rearrange("p (k t) -> p k t", k=8)),
                 reads=[r_p], writes=[r_xT])
        aT, r_aT = atrot.next()
        for fk in range(8):
            pg, r_pg = self.pA.next()
            pu, r_pu = self.pA.next()
            for k in range(8):
                S.op("pe", lambda e, pg=pg, k=k, fk=fk: e.matmul(pg[:, :], lhsT=wg[:, k, fk * 128:(fk + 1) * 128], rhs=xT[:, k, :], start=(k == 0), stop=(k == 7)),
                     reads=[r_wg, r_xT], writes=[r_pg], signal=(k == 7))
            for k in range(8):
                S.op("pe", lambda e, pu=pu, k=k, fk=fk: e.matmul(pu[:, :], lhsT=wu[:, k, fk * 128:(fk + 1) * 128], rhs=xT[:, k, :], start=(k == 0), stop=(k == 7)),
                     reads=[r_wu, r_xT], writes=[r_pu], signal=(k == 7))
            gs, r_gs = gsrot.next()
            sg, r_sg = sgrot.next()
            us, r_us = usrot.next()
            S.op("dve", lambda e, gs=gs, pg=pg, fk=fk: e.tensor_scalar(out=gs[:], in0=pg[:, :], scalar1=bg[:, fk, 0:1], scalar2=7.0, op0=ALU.add, op1=ALU.min),
                 reads=[r_pg, r_bg], writes=[r_gs])
            S.op("act", lambda e, sg=sg, gs=gs: e.activation(out=sg[:], in_=gs[:], func=AF.Sigmoid, scale=1.702), reads=[r_gs], writes=[r_sg])
            S.op("dve", lambda e, us=us, pu=pu, fk=fk: e.tensor_scalar(out=us[:], in0=pu[:, :], scalar1=bg[:, fk, 1:2], scalar2=7.0, op0=ALU.add, op1=ALU.min),
                 reads=[r_pu, r_bg], writes=[r_us])
            S.op("pool", lambda e, us=us: e.tensor_scalar(out=us[:], in0=us[:], scalar1=-7.0, scalar2=1.0, op0=ALU.max, op1=ALU.add),
                 reads=[r_us], writes=[r_us])
            S.op("pool", lambda e, gs=gs, sg=sg: e.tensor_tensor(out=gs[:], in0=gs[:], in1=sg[:], op=ALU.mult), reads=[r_gs, r_sg], writes=[r_gs])
            S.op("dve", lambda e, us=us, gs=gs, fk=fk: e.tensor_tensor(out=aT[:, fk, :], in0=us[:], in1=gs[:], op=ALU.mult),
                 reads=[r_us, r_gs], writes=[r_aT])
        for jt in range(4):
            ye, r_ye = yrot.next()
            for half in range(2):
                pt, r_p = self.pA.next()
                for fk in range(8):
                    S.op("pe", lambda e, pt=pt, fk=fk, jt=jt, half=half: e.matmul(
                        pt[:, :], lhsT=aT[:, fk, jt * 128:(jt + 1) * 128], rhs=wd[:, fk, half * 512:(half + 1) * 512], start=(fk == 0), stop=(fk == 7)),
                        reads=[r_aT, r_wd], writes=[r_p], signal=(fk == 7))
                hs = slice(half * 512, (half + 1) * 512)
                S.op("dve", lambda e, ye=ye, pt=pt, hs=hs: e.tensor_tensor(out=ye[:, hs], in0=pt[:, :], in1=bd[:, hs], op=ALU.add),
                     reads=[r_p, r_bd], writes=[r_ye])
            S.dma("sp", lambda e, ye=ye, jt=jt: e.dma_start(out=self.YE[row0 + jt * 128:row0 + (jt + 1) * 128, :], in_=ye[:]),
                  reads=[r_ye], writes=[self.rYE])

    def phase_combine(self, l, last):
        nc, S = self.nc, self.S
        with ExitStack() as st:
            GT = self.sb(st, "GT2", [128, 2, D], F32)
            r_gt = Res()
            for j in range(2):
                self.load_bc("sp", GT[:, j, :], r_gt, self.MOD[l, j:j + 1, 5 * D:6 * D])
            if last:
                FG = self.sb(st, "FG", [128, D], F32)
                self.load_bc("sp", FG[:], r_gt, self.final_g[0:1, :])
            ykrot = self.rot_sb(st, "yk", [128, 4, D], F32, 2)
            accrot = self.rot_sb(st, "kacc", [128, D], F32, 2)
            xrot = self.rot_sb(st, "kx", [128, D], F32, 2)
            orot = self.rot_sb(st, "ko", [128, D], F32, 2)
            strot = self.stat_tiles(st, "kst")
            for i in range(self.nt_act):
                j = 0 if i < 32 else 1
                yk, r_yk = ykrot.next()
                for k in range(4):
                    S.dma("pool", lambda e, yk=yk, i=i, k=k: e.indirect_dma_start(
                        out=yk[:, k, :], out_offset=None, in_=self.YE[:, :], in_offset=bass.IndirectOffsetOnAxis(ap=self.DEST[:, i, k:k + 1], axis=0)),
                        reads=[self.rYE, self.rROUTE[i]], writes=[r_yk])
                xt, r_x = xrot.next()
                S.dma("sp", lambda e, xt=xt, i=i: e.dma_start(out=xt[:], in_=self.X[i * 128:(i + 1) * 128, :]), reads=[self.rX[i]], writes=[r_x])
                acc, r_a = accrot.next()
                S.op("dve", lambda e, acc=acc, yk=yk, i=i: e.tensor_scalar(out=acc[:], in0=yk[:, 0, :], scalar1=self.GATES[:, i, 0:1], scalar2=None, op0=ALU.mult),
                     reads=[r_yk, self.rROUTE[i]], writes=[r_a])
                for k in range(1, 4):
                    S.op("dve", lambda e, acc=acc, yk=yk, i=i, k=k: e.scalar_tensor_tensor(out=acc[:], in0=yk[:, k, :], scalar=self.GATES[:, i, k:k + 1], in1=acc[:], op0=ALU.mult, op1=ALU.add),
                         reads=[r_yk, self.rROUTE[i], r_a], writes=[r_a])
                S.op("pool", lambda e, acc=acc, j=j: e.tensor_tensor(out=acc[:], in0=acc[:], in1=GT[:, j, :], op=ALU.mult), reads=[r_a, r_gt], writes=[r_a])
                xo, r_xo = orot.next()
                S.op("pool", lambda e, xo=xo, acc=acc, xt=xt: e.tensor_tensor(out=xo[:], in0=acc[:], in1=xt[:], op=ALU.add), reads=[r_a, r_x], writes=[r_xo])
                if not last:
                    S.dma("sp", lambda e, xo=xo, i=i: e.dma_start(out=self.X[i * 128:(i + 1) * 128, :], in_=xo[:]), reads=[r_xo], writes=[self.rX[i]])
                else:
                    stt = strot.next()
                    rstd, r_s = self.rms_rstd(stt, xo, r_xo, D)
                    S.op("dve", lambda e, xo=xo, rstd=rstd: e.scalar_tensor_tensor(out=xo[:], in0=xo[:], scalar=rstd[:, 0:1], in1=FG[:], op0=ALU.mult, op1=ALU.mult),
                         reads=[r_xo, r_s, r_gt], writes=[r_xo])
                    S.dma("sp", lambda e, xo=xo, i=i: e.dma_start(out=self.out[i * 128:(i + 1) * 128, :], in_=xo[:]), reads=[r_xo], writes=[self.rOUT])
            self.end_phase()


def _na_table(rpb):
    a = np.arange(2)[:, None, None, None]
    kc = np.arange(64)[None, :, None, None]
    j = np.arange(NJ)[None, None, :, None]
    qc = np.arange(64)[None, None, None, :]
    dr = a - (j - JOFF) + 0 * kc + 0 * qc
    cs = np.clip(qc - 8, 0, 48)
    colv = (kc >= cs) & (kc < cs + 16)
    dc = kc - qc + 0 * a + 0 * j
    out = np.empty((2, 128, 8, NJ * 64), np.float32)
    for v in range(2):
        rowv = ((dr >= -4) & (dr <= 3)) if v == 0 else ((dr >= -7) & (dr <= 7))
        valid = np.broadcast_to(rowv & colv, (2, 64, NJ, 64))
        ri = np.clip(dr + 7, 0, 14)
        ci = np.clip(dc + 15, 0, 30)
        ri = np.broadcast_to(ri, (2, 64, NJ, 64))
        ci = np.broadcast_to(ci, (2, 64, NJ, 64))
        for h in range(8):
            vals = rpb[h][ri, ci]
            tbl = np.where(valid, vals, np.float32(NEG)).astype(np.float32)
            out[v, :, h, :] = tbl.reshape(128, NJ * 64)
    return out


def _rope_tables():
    half = 32
    inv = (10000.0 ** (-np.arange(0, half, 2, dtype=np.float32) / half)).astype(np.float32)
    t = np.arange(TL)
    ang_r = (t // 64).astype(np.float32)[:, None] * inv
    ang_c = (t % 64).astype(np.float32)[:, None] * inv
    cos = np.zeros((TL, 64), np.float32)
    sin = np.zeros((TL, 64), np.float32)
    for base, ang in ((0, ang_r), (32, ang_c)):
        c, s = np.cos(ang).astype(np.float32), np.sin(ang).astype(np.float32)
        cos[:, base:base + 16] = c
        cos[:, base + 16:base + 32] = c
        sin[:, base:base + 16] = -s
        sin[:, base + 16:base + 32] = s
    return np.tile(cos, (1, 10)), np.tile(sin, (1, 10))


def make_in_maps(inputs, n_cores=8):
    f = lambda a: np.ascontiguousarray(np.asarray(a, dtype=np.float32))
    x, c, ctx, c_ctx = f(inputs["x"]), f(inputs["c"]), f(inputs["ctx"]), f(inputs["c_ctx"])
    natab = np.stack([_na_table(f(inputs["na_rpb"])[l]) for l in range(DEPTH)])
    qg, kg = f(inputs["q_norm_g"]), f(inputs["k_norm_g"])
    qkgain = np.concatenate([np.tile(qg, (1, 8)), np.tile(kg, (1, 2))], axis=1)
    ropec, ropes = _rope_tables()
    w_br = np.stack([f(inputs["w_br_conv"]), f(inputs["w_br_na"]), f(inputs["w_br_gqa"])], axis=1)
    shared = dict(
        ada_w=f(inputs["ada_w"]), ada_b=f(inputs["ada_b"]), norm1_g=f(inputs["norm1_g"]), norm2_g=f(inputs["norm2_g"]),
        w_in=f(inputs["w_in"]), conv_w=f(inputs["conv_w"]), natab=natab, qkgain=np.ascontiguousarray(qkgain),
        ropec=ropec, ropes=ropes, w_br=np.ascontiguousarray(w_br), w_out=f(inputs["w_out"]),
        router_w=f(inputs["router_w"]), router_b=f(inputs["router_b"]), w_gu=f(inputs["w_gu"]), b_gu=f(inputs["b_gu"]),
        w_down=f(inputs["w_down"]), b_down=f(inputs["b_down"]), final_g=f(inputs["final_g"]).reshape(1, D))
    maps = []
    for b in range(n_cores):
        m = dict(shared)
        m["xin"] = np.ascontiguousarray(np.concatenate([x[b], ctx[b]], axis=0))
        m["cvec"] = np.ascontiguousarray(np.stack([c[b], c_ctx], axis=0))
        maps.append(m)
    return maps


def kernel(**inputs):
    nc = bass.Bass("TRN2", target_bir_lowering=False)
    Builder(nc).build()
    maps = make_in_maps(inputs)
    res = run_bass_kernel_spmd(nc, maps, core_ids=list(range(8)))
    return np.stack([np.asarray(r["out"], dtype=np.float32) for r in res.results], axis=0)
```

```python
import numpy as np
from contextlib import ExitStack
import concourse.bass as bass
import concourse.mybir as mybir
from concourse.bass_utils import run_bass_kernel_spmd

F32 = mybir.dt.float32
BF16 = mybir.dt.bfloat16
I32 = mybir.dt.int32
AF = mybir.ActivationFunctionType
ALU = mybir.AluOpType
AX = mybir.AxisListType

D = 1024
TL = 4096
TC = 256
T = TL + TC
NT = T // 128
DEPTH = 2
NE = 32
CAP = 2048
EPS = 1e-6
NEG = -30000.0
NJ = 22
JOFF = 10
INC = 6912


class Res:
    __slots__ = ("w", "rs")

    def __init__(self):
        self.w = None
        self.rs = {}


class Sched:
    ENGS = ("pe", "act", "dve", "pool", "sp")
    NQ = 20

    def __init__(self, nc, stack, same_engine_sync=True):
        self.nc = nc
        self.eng = {"pe": nc.tensor, "act": nc.scalar, "dve": nc.vector,
                    "pool": nc.gpsimd, "sp": nc.sync}
        self.sem = {}
        self.cnt = {}
        self.seen = {e: {} for e in self.ENGS}
        self.prog = {e: [] for e in self.ENGS}
        self.same_engine_sync = same_engine_sync
        for e in self.ENGS:
            self.sem[e] = stack.enter_context(nc.semaphore("c_" + e))
            self.cnt[e] = 0
        self.dq = {}
        for q in ("sp", "act", "pool"):
            keys = []
            for i in range(self.NQ):
                k = "d_%s_%d" % (q, i)
                self.sem[k] = stack.enter_context(nc.semaphore(k))
                self.cnt[k] = 0
                keys.append(k)
            self.dq[q] = [keys, 0]

    def _deps(self, engine, reads, writes, extra=()):
        need = {}
        for r in reads:
            if r.w is not None:
                k, v = r.w
                if need.get(k, 0) < v:
                    need[k] = v
        for w in writes:
            if w.w is not None:
                k, v = w.w
                if need.get(k, 0) < v:
                    need[k] = v
            for k, v in w.rs.items():
                if need.get(k, 0) < v:
                    need[k] = v
        for k, v in extra:
            if need.get(k, 0) < v:
                need[k] = v
        out = []
        seen = self.seen[engine]
        for k, v in need.items():
            if k == engine and (engine == "pe" or not self.same_engine_sync):
                continue
            if seen.get(k, 0) >= v:
                continue
            seen[k] = v
            out.append((k, v))
        return out

    def _mark(self, ev, reads, writes):
        k, v = ev
        for r in reads:
            if r.rs.get(k, 0) < v:
                r.rs[k] = v
        for w in writes:
            w.w = ev
            w.rs = {}

    def op(self, engine, fn, reads=(), writes=(), signal=True):
        waits = self._deps(engine, reads, writes)
        sem = self.sem[engine]
        if signal:
            self.cnt[engine] += 1
        ev = (engine, self.cnt[engine] if signal else self.cnt[engine] + 1)
        sems = self.sem

        def emit(eng):
            for k, v in waits:
                eng.wait_ge(sems[k], v)
            ins = fn(eng)
            if signal:
                ins.then_inc(sem, 1)

        self.prog[engine].append(emit)
        self._mark(ev, reads, writes)
        return ev

    def dma(self, queue, fn, reads=(), writes=()):
        keys, idx = self.dq[queue]
        k = keys[idx % len(keys)]
        self.dq[queue][1] = idx + 1
        prev = self.cnt[k]
        extra = [(k, prev)] if prev > 0 else []
        waits = self._deps(queue, reads, writes, extra)
        self.cnt[k] = prev + 16
        ev = (k, prev + 16)
        sems = self.sem

        def emit(eng):
            for kk, v in waits:
                eng.wait_ge(sems[kk], v)
            fn(eng).then_inc(sems[k], 16)

        self.prog[queue].append(emit)
        self._mark(ev, reads, writes)
        return ev

    def barrier(self):
        sems = self.sem
        for e in self.ENGS:
            waits = []
            seen = self.seen[e]
            for k, v in self.cnt.items():
                if v > 0 and seen.get(k, 0) < v:
                    seen[k] = v
                    waits.append((k, v))

            def emit(eng, waits=waits):
                for k, v in waits:
                    eng.wait_ge(sems[k], v)

            self.prog[e].append(emit)

    def flush(self):
        nc = self.nc
        prog = self.prog
        with nc.Block() as block:
            @block.sync
            def _(e):
                for f in prog["sp"]:
                    f(e)

            @block.scalar
            def _(e):
                for f in prog["act"]:
                    f(e)

            @block.vector
            def _(e):
                for f in prog["dve"]:
                    f(e)

            @block.gpsimd
            def _(e):
                for f in prog["pool"]:
                    f(e)

            @block.tensor
            def _(e):
                for f in prog["pe"]:
                    f(e)
        self.prog = {e: [] for e in self.ENGS}


class Rot:
    def __init__(self, items):
        self.items = items
        self.i = 0

    def next(self):
        it = self.items[self.i % len(self.items)]
        self.i += 1
        return it


def ntiles512(n_tok):
    out = []
    t = 0
    while t < n_tok:
        w = min(512, n_tok - t)
        out.append((t, w))
        t += w
    return out


class Builder:
    def __init__(self, nc, dbg=None, layers=(0, 1), stop_after=None):
        self.nc = nc
        self.dbg = dbg or []
        self.layers = layers
        self.stop_after = stop_after

    def sb(self, st, name, shape, dt):
        self._uid = getattr(self, "_uid", 0) + 1
        return st.enter_context(self.nc.sbuf_tensor("%s_%d" % (name, self._uid), list(shape), dt))

    def rot_sb(self, st, name, shape, dt, n):
        return Rot([(self.sb(st, "%s%d" % (name, i), shape, dt), Res()) for i in range(n)])

    def end_phase(self):
        self.S.barrier()
        self.S.flush()

    def declare(self):
        nc = self.nc
        di = lambda n, s, dt=F32: nc.dram_tensor(n, list(s), dt, kind="ExternalInput").ap()
        self.xin = di("xin", [T, D])
        self.cvec = di("cvec", [2, D])
        self.ada_w = di("ada_w", [DEPTH, D, 6 * D])
        self.ada_b = di("ada_b", [DEPTH, 6 * D])
        self.norm1_g = di("norm1_g", [DEPTH, D])
        self.norm2_g = di("norm2_g", [DEPTH, D])
        self.w_in = di("w_in", [DEPTH, D, INC])
        self.conv_w = di("conv_w", [DEPTH, 3, 512])
        self.natab = di("natab", [DEPTH, 2, 128, 8, NJ * 64])
        self.qkgain = di("qkgain", [DEPTH, 640])
        self.ropec = di("ropec", [TL, 640])
        self.ropes = di("ropes", [TL, 640])
        self.w_br = di("w_br", [DEPTH, 3, 512, D])
        self.w_out = di("w_out", [DEPTH, D, D])
        self.router_w = di("router_w", [DEPTH, D, NE])
        self.router_b = di("router_b", [DEPTH, NE])
        self.w_gu = di("w_gu", [DEPTH, NE, D, 2 * D])
        self.b_gu = di("b_gu", [DEPTH, NE, 2 * D])
        self.w_down = di("w_down", [DEPTH, NE, D, D])
        self.b_down = di("b_down", [DEPTH, NE, D])
        self.final_g = di("final_g", [1, D])
        self.out = nc.dram_tensor("out", [TL, D], F32, kind="ExternalOutput").ap()

        def scr(n, s, dt):
            kind = "ExternalOutput" if n in self.dbg else "Internal"
            return nc.dram_tensor(n, list(s), dt, kind=kind).ap()
        self.X = scr("X", [T, D], F32)
        self.MOD = scr("MOD", [DEPTH, 2, 6 * D], F32)
        self.FT = scr("FT", [INC, T], BF16)
        self.TM = scr("TM", [T, 1280], BF16)
        self.YT = scr("YT", [1536, T], BF16)
        self.XE = scr("XE", [NE * CAP, D], BF16)
        self.YE = scr("YE", [NE * CAP, D], F32)
        self.rX = [Res() for _ in range(NT)]
        self.rMOD = Res()
        self.rFT = Res()
        self.rTM = Res()
        self.rYT = Res()
        self.rXE = Res()
        self.rYE = Res()
        self.rOUT = Res()

    def build(self):
        nc = self.nc
        self.declare()
        with ExitStack() as gst:
            S = self.S = Sched(nc, gst)
            self.pA = Rot([(gst.enter_context(nc.psum_tensor("pA%d" % i, [128, 512], F32)), Res()) for i in range(4)])
            self.pB = Rot([(gst.enter_context(nc.psum_tensor("pB%d" % i, [128, 512], F32)), Res()) for i in range(2)])
            self.pT = Rot([(gst.enter_context(nc.psum_tensor("pT%d" % i, [128, 1024], BF16)), Res()) for i in range(2)])
            self.identf = self.sb(gst, "identf", [128, 128], F32)
            self.identb = self.sb(gst, "identb", [128, 128], BF16)
            self.r_id = Res()
            idf, idb = self.identf, self.identb
            S.op("pool", lambda e: e.memset(idf[:], 0.0), writes=[self.r_id])
            S.op("pool", lambda e: e.affine_select(out=idf[:], in_=idf[:], pattern=[[-1, 128]],
                                                   compare_op=ALU.not_equal, fill=1.0, base=0, channel_multiplier=1),
                 reads=[self.r_id], writes=[self.r_id])
            S.op("dve", lambda e: e.tensor_copy(out=idb[:], in_=idf[:]), reads=[self.r_id], writes=[self.r_id])
            self.DEST = self.sb(gst, "DEST", [128, NT, 4], I32)
            self.GATES = self.sb(gst, "GATES", [128, NT, 4], F32)
            self.rROUTE = [Res() for _ in range(NT)]
            self.end_phase()

            self.phase_mods()
            if self.stop_after == "mods":
                return self.finish()
            for l in self.layers:
                last = (l == DEPTH - 1)
                self.Xsrc = self.xin if l == 0 else self.X
                self.nt_act = 32 if last else NT
                self.phase_AB(l)
                if self.stop_after == "AB%d" % l:
                    return self.finish()
                self.phase_conv(l)
                self.phase_gqa(l, last)
                self.phase_na(l, last)
                if self.stop_after == "attn%d" % l:
                    return self.finish()
                self.phase_merge(l)
                if self.stop_after == "merge%d" % l:
                    return self.finish()
                self.phase_route(l)
                self.phase_experts(l)
                if self.stop_after == "exp%d" % l:
                    return self.finish()
                self.phase_combine(l, last)
                if self.stop_after == "comb%d" % l:
                    return self.finish()
            return self.finish()

    def finish(self):
        S = self.S
        S.barrier()
        S.flush()

    def phase_mods(self):
        nc, S = self.nc, self.S
        with ExitStack() as st:
            cs = self.sb(st, "cs", [128, 8, 2], F32)
            ca = self.sb(st, "ca", [128, 8, 2], F32)
            r_cs = Res()
            for j in range(2):
                S.dma("sp", lambda e, j=j: e.dma_start(out=cs[:, :, j], in_=self.cvec[j, :].rearrange("(p k) -> p k", k=8), allow_slow_non_contiguous=True),
                      writes=[r_cs])
            S.op("act", lambda e: e.activation(out=ca[:], in_=cs[:], func=AF.Silu), reads=[r_cs], writes=[r_cs])
            wrot = self.rot_sb(st, "adaw", [128, 8, 512], F32, 2)
            bias = self.sb(st, "adab", [2, 6 * D], F32)
            modsb = self.sb(st, "modsb", [2, 6 * D], F32)
            r_b = Res()
            r_m = Res()
            for l in range(DEPTH):
                S.dma("sp", lambda e, l=l: e.dma_start(out=bias[:], in_=self.ada_b[l:l + 1, :].to_broadcast([2, 6 * D])),
                      writes=[r_b])
                wv = self.ada_w[l].rearrange("(p k) f -> p k f", k=8)
                for fb in range(12):
                    wt, r_w = wrot.next()
                    S.dma("sp" if fb % 2 == 0 else "act",
                          lambda e, wt=wt, fb=fb, wv=wv: e.dma_start(out=wt[:], in_=wv[:, :, fb * 512:(fb + 1) * 512]),
                          writes=[r_w])
                    pt, r_p = self.pA.next()
                    for k in range(8):
                        S.op("pe", lambda e, pt=pt, wt=wt, k=k: e.matmul(pt[0:2, :], lhsT=ca[:, k, :], rhs=wt[:, k, :],
                                                                        start=(k == 0), stop=(k == 7)),
                             reads=[r_cs, r_w], writes=[r_p], signal=(k == 7))
                    S.op("dve", lambda e, pt=pt, fb=fb: e.tensor_tensor(out=modsb[:, fb * 512:(fb + 1) * 512], in0=pt[0:2, :],
                                                                        in1=bias[:, fb * 512:(fb + 1) * 512], op=ALU.add),
                         reads=[r_p, r_b], writes=[r_m])
                S.dma("sp", lambda e, l=l: e.dma_start(out=self.MOD[l], in_=modsb[:]), reads=[r_m], writes=[self.rMOD])
            self.end_phase()

    def load_feat(self, queue, tile, res, src_row):
        self.S.dma(queue, lambda e: e.dma_start(out=tile[:], in_=src_row.rearrange("(k p) -> p k", p=128),
                                                allow_slow_non_contiguous=True),
                   reads=[self.rMOD], writes=[res])

    def load_bc(self, queue, tile_ap, res, src_row2d, n=128):
        F = src_row2d.shape[-1]
        self.S.dma(queue, lambda e: e.dma_start(out=tile_ap, in_=src_row2d.to_broadcast([n, F])),
                   reads=[self.rMOD], writes=[res])

    def rms_rstd(self, st_tiles, xt, r_x, width):
        S = self.S
        junk, ss, ms, rstd, r_s = st_tiles
        S.op("act", lambda e: e.activation(out=junk[:, 0:width], in_=xt[:, 0:width], func=AF.Square, accum_out=ss[:, 0:1]),
             reads=[r_x], writes=[r_s])
        S.op("dve", lambda e: e.tensor_scalar(out=ms[:], in0=ss[:], scalar1=1.0 / width, scalar2=EPS, op0=ALU.mult, op1=ALU.add),
             reads=[r_s], writes=[r_s])
        S.op("act", lambda e: e.activation(out=ms[:], in_=ms[:], func=AF.Sqrt), reads=[r_s], writes=[r_s])
        S.op("dve", lambda e: e.reciprocal(out=rstd[:], in_=ms[:]), reads=[r_s], writes=[r_s])
        return rstd, r_s

    def stat_tiles(self, st, name, n=2):
        items = []
        for i in range(n):
            items.append((self.sb(st, "%sj%d" % (name, i), [128, 1024], BF16), self.sb(st, "%ss%d" % (name, i), [128, 1], F32),
                          self.sb(st, "%sm%d" % (name, i), [128, 1], F32), self.sb(st, "%sr%d" % (name, i), [128, 1], F32), Res()))
        return Rot(items)

    def phase_AB(self, l):
        nc, S = self.nc, self.S
        with ExitStack() as st:
            hT = self.sb(st, "hT", [128, 8, T], BF16)
            r_h = [Res() for _ in range(NT)]
            G1 = self.sb(st, "G1", [128, 2, 8], F32)
            SH1 = self.sb(st, "SH1", [128, 2, 8], F32)
            ng = self.sb(st, "ng", [128, 8], F32)
            r_g = Res()
            self.load_feat("sp", ng, r_g, self.norm1_g[l, :])
            for j in range(2):
                S.dma("sp", lambda e, j=j: e.dma_start(out=SH1[:, j, :], in_=self.MOD[l, j, 0:D].rearrange("(k p) -> p k", p=128),
                                                       allow_slow_non_contiguous=True), reads=[self.rMOD], writes=[r_g])
                S.dma("sp", lambda e, j=j: e.dma_start(out=G1[:, j, :], in_=self.MOD[l, j, D:2 * D].rearrange("(k p) -> p k", p=128),
                                                       allow_slow_non_contiguous=True), reads=[self.rMOD], writes=[r_g])
            for j in range(2):
                S.op("dve", lambda e, j=j: e.scalar_tensor_tensor(out=G1[:, j, :], in0=G1[:, j, :], scalar=1.0, in1=ng[:],
                                                                  op0=ALU.add, op1=ALU.mult), reads=[r_g], writes=[r_g])
            xrot = self.rot_sb(st, "xt", [128, D], F32, 2)
            xsrot = self.rot_sb(st, "xs", [128, D], BF16, 2)
            strot = self.stat_tiles(st, "st")
            for i in range(NT):
                xt, r_x = xrot.next()
                S.dma("sp", lambda e, xt=xt, i=i: e.dma_start(out=xt[:], in_=self.Xsrc[i * 128:(i + 1) * 128, :]),
                      reads=[self.rX[i]], writes=[r_x])
                stt = strot.next()
                rstd, r_s = self.rms_rstd(stt, xt, r_x, D)
                xs, r_xs = xsrot.next()
                S.op("act", lambda e, xs=xs, xt=xt, rstd=rstd: e.activation(out=xs[:], in_=xt[:], func=AF.Copy, scale=rstd[:, 0:1]),
                     reads=[r_x, r_s], writes=[r_xs])
                pt, r_p = self.pT.next()
                for k in range(8):
                    S.op("pe", lambda e, pt=pt, xs=xs, k=k: e.transpose(out=pt[:, k * 128:(k + 1) * 128], in_=xs[:, k * 128:(k + 1) * 128],
                                                                        identity=self.identb[:]),
                         reads=[r_xs, self.r_id], writes=[r_p], signal=(k == 7))
                j = 0 if i < 32 else 1
                for k in range(8):
                    S.op("act", lambda e, pt=pt, k=k, i=i, j=j: e.activation(out=hT[:, k, i * 128:(i + 1) * 128], in_=pt[:, k * 128:(k + 1) * 128],
                                                                             func=AF.Identity, scale=G1[:, j, k:k + 1], bias=SH1[:, j, k:k + 1]),
                         reads=[r_p, r_g], writes=[r_h[i]])
            wv = self.w_in[l].rearrange("(k p) c -> p k c", p=128)
            wrot = self.rot_sb(st, "wblk", [128, 8, 512], BF16, 2)
            stg = self.rot_sb(st, "stg", [128, T], BF16, 2)
            tmst = self.rot_sb(st, "tmst", [128, 512], BF16, 3)
            nts = ntiles512(T)
            ev_i = 0
            for cb in range(14):
                c0 = cb * 512
                cw = min(512, INC - c0)
                wt, r_w = wrot.next()
                S.dma("pool", lambda e, wt=wt, c0=c0, cw=cw: e.dma_start(out=wt[:, :, 0:cw], in_=wv[:, :, c0:c0 + cw]), writes=[r_w])
                tm_lo, tm_hi = max(c0, 2560), min(c0 + cw, 3840)
                for cc in range(cw // 128):
                    col = c0 + cc * 128
                    if 2560 <= col < 3840:
                        continue
                    is_gate = col >= 3840
                    sg, r_sg = stg.next()
                    for (t0, tw) in nts:
                        pt, r_p = self.pA.next()
                        rh = r_h[t0 // 128:(t0 + tw) // 128]
                        for k in range(8):
                            S.op("pe", lambda e, pt=pt, wt=wt, k=k, cc=cc, t0=t0, tw=tw: e.matmul(
                                pt[:, 0:tw], lhsT=wt[:, k, cc * 128:(cc + 1) * 128], rhs=hT[:, k, t0:t0 + tw], start=(k == 0), stop=(k == 7)),
                                reads=[r_w] + rh, writes=[r_p], signal=(k == 7))
                        if is_gate:
                            S.op("act", lambda e, pt=pt, sg=sg, t0=t0, tw=tw: e.activation(out=sg[:, t0:t0 + tw], in_=pt[:, 0:tw], func=AF.Sigmoid),
                                 reads=[r_p], writes=[r_sg])
                        elif ev_i % 2 == 0:
                            S.op("act", lambda e, pt=pt, sg=sg, t0=t0, tw=tw: e.activation(out=sg[:, t0:t0 + tw], in_=pt[:, 0:tw], func=AF.Copy),
                                 reads=[r_p], writes=[r_sg])
                        else:
                            S.op("dve", lambda e, pt=pt, sg=sg, t0=t0, tw=tw: e.tensor_copy(out=sg[:, t0:t0 + tw], in_=pt[:, 0:tw]),
                                 reads=[r_p], writes=[r_sg])
                        ev_i += 1
                    S.dma("sp", lambda e, sg=sg, col=col: e.dma_start(out=self.FT[col:col + 128, :], in_=sg[:]), reads=[r_sg], writes=[self.rFT])
                if tm_lo < tm_hi:
                    w0, wn = tm_lo - c0, tm_hi - tm_lo
                    for i in range(NT):
                        pt, r_p = self.pA.next()
                        for k in range(8):
                            S.op("pe", lambda e, pt=pt, wt=wt, k=k, i=i, w0=w0, wn=wn: e.matmul(
                                pt[:, 0:wn], lhsT=hT[:, k, i * 128:(i + 1) * 128], rhs=wt[:, k, w0:w0 + wn], start=(k == 0), stop=(k == 7)),
                                reads=[r_w, r_h[i]], writes=[r_p], signal=(k == 7))
                        ts, r_ts = tmst.next()
                        if i % 2 == 0:
                            S.op("act", lambda e, pt=pt, ts=ts, wn=wn: e.activation(out=ts[:, 0:wn], in_=pt[:, 0:wn], func=AF.Copy),
                                 reads=[r_p], writes=[r_ts])
                        else:
                            S.op("dve", lambda e, pt=pt, ts=ts, wn=wn: e.tensor_copy(out=ts[:, 0:wn], in_=pt[:, 0:wn]),
                                 reads=[r_p], writes=[r_ts])
                        S.dma("sp", lambda e, ts=ts, i=i, wn=wn, tm_lo=tm_lo: e.dma_start(
                            out=self.TM[i * 128:(i + 1) * 128, tm_lo - 2560:tm_lo - 2560 + wn], in_=ts[:, 0:wn]), reads=[r_ts], writes=[self.rTM])
            self.end_phase()

    def phase_conv(self, l):
        nc, S = self.nc, self.S
        with ExitStack() as st:
            cw = self.sb(st, "cw", [128, 4, 3], F32)
            r_cw = Res()
            for kk in range(3):
                S.dma("sp", lambda e, kk=kk: e.dma_start(out=cw[:, :, kk], in_=self.conv_w[l, kk, :].rearrange("(j p) -> p j", p=128),
                                                         allow_slow_non_contiguous=True), writes=[r_cw])
            inrot = self.rot_sb(st, "cin", [128, 3, T], BF16, 2)
            u = self.sb(st, "cu", [128, T], F32)
            acc = self.sb(st, "cacc", [128, T], F32)
            yrot = self.rot_sb(st, "cy", [128, T], BF16, 2)
            r_u, r_a = Res(), Res()
            for j in range(4):
                ci, r_ci = inrot.next()
                for b in range(3):
                    S.dma("sp" if b != 1 else "act", lambda e, ci=ci, b=b, j=j: e.dma_start(
                        out=ci[:, b, :], in_=self.FT[b * 512 + j * 128:b * 512 + (j + 1) * 128, :]), reads=[self.rFT], writes=[r_ci])
                S.op("pool", lambda e, ci=ci: e.tensor_tensor(out=u[:], in0=ci[:, 1, :], in1=ci[:, 2, :], op=ALU.mult),
                     reads=[r_ci], writes=[r_u])
                S.op("dve", lambda e, j=j: e.tensor_scalar(out=acc[:], in0=u[:], scalar1=cw[:, j, 1:2], scalar2=None, op0=ALU.mult),
                     reads=[r_u, r_cw], writes=[r_a])
                for (a, b) in ((0, TL), (TL, T)):
                    S.op("dve", lambda e, j=j, a=a, b=b: e.scalar_tensor_tensor(out=acc[:, a + 1:b], in0=u[:, a:b - 1], scalar=cw[:, j, 0:1],
                                                                              in1=acc[:, a + 1:b], op0=ALU.mult, op1=ALU.add),
                         reads=[r_u, r_cw, r_a], writes=[r_a])
                    S.op("dve", lambda e, j=j, a=a, b=b: e.scalar_tensor_tensor(out=acc[:, a:b - 1], in0=u[:, a + 1:b], scalar=cw[:, j, 2:3],
                                                                              in1=acc[:, a:b - 1], op0=ALU.mult, op1=ALU.add),
                         reads=[r_u, r_cw, r_a], writes=[r_a])
                y, r_y = yrot.next()
                S.op("pool", lambda e, y=y, ci=ci: e.tensor_tensor(out=y[:], in0=ci[:, 0, :], in1=acc[:], op=ALU.mult),
                     reads=[r_ci, r_a], writes=[r_y])
                S.dma("sp", lambda e, y=y, j=j: e.dma_start(out=self.YT[j * 128:(j + 1) * 128, :], in_=y[:]), reads=[r_y], writes=[self.rYT])
            self.end_phase()

    def attn_block(self, kt_ap_fn, q_ap, va_ap_fn, chunks, N, acc, r_acc, prot, reads, tab_fn=None, addrot=None, scale=0.125):
        S = self.S
        n = len(chunks)
        pend = []

        def qk(si):
            s = chunks[si]
            ps, r_ps = self.pA.next()
            kt = kt_ap_fn(s)
            S.op("pe", lambda e: e.matmul(ps[:, 0:N], lhsT=kt, rhs=q_ap, start=True, stop=True), reads=reads, writes=[r_ps])
            pe_t, r_pe = prot.next()
            tb = tab_fn(s) if tab_fn is not None else None
            if tb is not None:
                ad, r_ad = addrot.next()
                S.op("dve", lambda e: e.scalar_tensor_tensor(out=ad[:, 0:N], in0=ps[:, 0:N], scalar=scale, in1=tb, op0=ALU.mult, op1=ALU.add),
                     reads=[r_ps] + reads, writes=[r_ad])
                S.op("act", lambda e: e.activation(out=pe_t[:, 0:N], in_=ad[:, 0:N], func=AF.Exp), reads=[r_ad], writes=[r_pe])
            else:
                S.op("act", lambda e: e.activation(out=pe_t[:, 0:N], in_=ps[:, 0:N], func=AF.Exp, scale=scale), reads=[r_ps], writes=[r_pe])
            return (s, pe_t, r_pe)

        pend.append(qk(0))
        for si in range(n):
            if si + 1 < n:
                pend.append(qk(si + 1))
            s, pe_t, r_pe = pend.pop(0)
            va_ = va_ap_fn(s)
            S.op("pe", lambda e, va_=va_, pe_t=pe_t, si=si: e.matmul(acc[:, 0:N], lhsT=va_, rhs=pe_t[:, 0:N], start=(si == 0), stop=(si == n - 1)),
                 reads=[r_pe] + reads, writes=[r_acc], signal=(si == n - 1))

    def attn_finish(self, acc, r_acc, N, out_ap, r_out, recrot):
        S = self.S
        rec, r_rec = recrot.next()
        S.op("act", lambda e: e.activation(out=rec[0:64, 0:N], in_=acc[64:128, 0:N], func=AF.Copy), reads=[r_acc], writes=[r_rec])
        S.op("dve", lambda e: e.reciprocal(out=rec[0:64, 0:N], in_=rec[0:64, 0:N]), reads=[r_rec], writes=[r_rec])
        S.op("dve", lambda e: e.tensor_tensor(out=out_ap, in0=acc[0:64, 0:N], in1=rec[0:64, 0:N], op=ALU.mult),
             reads=[r_acc, r_rec], writes=[r_out])

    def phase_gqa(self, l, last):
        nc, S = self.nc, self.S
        with ExitStack() as st:
            QT = self.sb(st, "QT", [128, 8, T], BF16)
            KT2 = self.sb(st, "KT2", [128, 2, T], BF16)
            VA = self.sb(st, "VA", [128, NT, 2, 128], BF16)
            r_q = Res()
            r_vat = [Res() for _ in range(NT)]
            S.op("pool", lambda e: e.memset(VA[:, :, :, 64:128], 1.0), writes=r_vat)
            S.op("pool", lambda e: e.memset(QT[:], 0.0), writes=[r_q])
            for i in range(NT):
                S.dma("sp" if i % 2 == 0 else "act", lambda e, i=i: e.dma_start(
                    out=VA[:, i, :, 0:64], in_=self.TM[i * 128:(i + 1) * 128, 1152:1280].rearrange("p (g d) -> p g d", g=2)),
                    reads=[self.rTM], writes=[r_vat[i]])
            gain = self.sb(st, "gain", [128, 640], F32)
            r_gn = Res()
            S.dma("sp", lambda e: e.dma_start(out=gain[:], in_=self.qkgain[l:l + 1, :].to_broadcast([128, 640])), writes=[r_gn])
            with ExitStack() as st2:
                inrot = self.rot_sb(st2, "gin", [128, 640], BF16, 2)
                sqrot = self.rot_sb(st2, "gsq", [128, 640], F32, 2)
                xnrot = self.rot_sb(st2, "gxn", [128, 640], F32, 2)
                swrot = self.rot_sb(st2, "gsw", [128, 640], F32, 2)
                cosrot = self.rot_sb(st2, "gcos", [128, 640], F32, 2)
                sinrot = self.rot_sb(st2, "gsin", [128, 640], F32, 2)
                qbrot = self.rot_sb(st2, "gqb", [128, 640], BF16, 2)
                ssrot = Rot([(self.sb(st2, "gss%d" % i, [128, 10], F32), Res()) for i in range(2)])
                for i in range(NT):
                    xi, r_xi = inrot.next()
                    S.dma("sp", lambda e, xi=xi, i=i: e.dma_start(out=xi[:], in_=self.TM[i * 128:(i + 1) * 128, 512:1152]), reads=[self.rTM], writes=[r_xi])
                    sq, r_sq = sqrot.next()
                    S.op("pool", lambda e, sq=sq, xi=xi: e.tensor_tensor(out=sq[:], in0=xi[:], in1=xi[:], op=ALU.mult), reads=[r_xi], writes=[r_sq])
                    ss, r_ss = ssrot.next()
                    S.op("dve", lambda e, ss=ss, sq=sq: e.tensor_reduce(out=ss[:], in_=sq[:].rearrange("p (h d) -> p h d", d=64), axis=AX.X, op=ALU.add),
                         reads=[r_sq], writes=[r_ss])
                    S.op("dve", lambda e, ss=ss: e.tensor_scalar(out=ss[:], in0=ss[:], scalar1=1.0 / 64, scalar2=EPS, op0=ALU.mult, op1=ALU.add),
                         reads=[r_ss], writes=[r_ss])
                    S.op("act", lambda e, ss=ss: e.activation(out=ss[:], in_=ss[:], func=AF.Sqrt), reads=[r_ss], writes=[r_ss])
                    S.op("dve", lambda e, ss=ss: e.reciprocal(out=ss[:], in_=ss[:]), reads=[r_ss], writes=[r_ss])
                    xn, r_xn = xnrot.next()
                    r_xh = [Res() for _ in range(10)]
                    for h in range(10):
                        S.op("dve", lambda e, xn=xn, xi=xi, ss=ss, h=h: e.tensor_scalar(
                            out=xn[:, h * 64:(h + 1) * 64], in0=xi[:, h * 64:(h + 1) * 64], scalar1=ss[:, h:h + 1], scalar2=None, op0=ALU.mult),
                            reads=[r_xi, r_ss, r_xn], writes=[r_xh[h]])
                    S.op("dve", lambda e, xn=xn: e.tensor_tensor(out=xn[:], in0=xn[:], in1=gain[:], op=ALU.mult), reads=r_xh + [r_gn], writes=[r_xn])
                    qb, r_qb = qbrot.next()
                    if i < 32:
                        co, r_co = cosrot.next()
                        si_, r_si = sinrot.next()
                        S.dma("act", lambda e, co=co, i=i: e.dma_start(out=co[:], in_=self.ropec[i * 128:(i + 1) * 128, :]), writes=[r_co])
                        S.dma("act", lambda e, si_=si_, i=i: e.dma_start(out=si_[:], in_=self.ropes[i * 128:(i + 1) * 128, :]), writes=[r_si])
                        sw, r_sw = swrot.next()
                        xv = xn[:].rearrange("p (g two d) -> p g two d", two=2, d=16)
                        swv = sw[:].rearrange("p (g two d) -> p g two d", two=2, d=16)
                        S.op("pool", lambda e, swv=swv, xv=xv: e.tensor_copy(out=swv[:, :, 0, :], in_=xv[:, :, 1, :]), reads=[r_xn], writes=[r_sw])
                        S.op("pool", lambda e, swv=swv, xv=xv: e.tensor_copy(out=swv[:, :, 1, :], in_=xv[:, :, 0, :]), reads=[r_xn, r_sw], writes=[r_sw])
                        S.op("pool", lambda e, sw=sw, si_=si_: e.tensor_tensor(out=sw[:], in0=sw[:], in1=si_[:], op=ALU.mult), reads=[r_sw, r_si], writes=[r_sw])
                        S.op("dve", lambda e, xn=xn, co=co: e.tensor_tensor(out=xn[:], in0=xn[:], in1=co[:], op=ALU.mult), reads=[r_xn, r_co], writes=[r_xn])
                        S.op("dve", lambda e, qb=qb, xn=xn, sw=sw: e.tensor_tensor(out=qb[:], in0=xn[:], in1=sw[:], op=ALU.add), reads=[r_xn, r_sw], writes=[r_qb])
                    else:
                        S.op("dve", lambda e, qb=qb, xn=xn: e.tensor_copy(out=qb[:], in_=xn[:]), reads=[r_xn], writes=[r_qb])
                    pt, r_p = self.pT.next()
                    for c in range(5):
                        S.op("pe", lambda e, pt=pt, qb=qb, c=c: e.transpose(out=pt[:, c * 128:(c + 1) * 128], in_=qb[:, c * 128:(c + 1) * 128], identity=self.identb[:]),
                             reads=[r_qb, self.r_id], writes=[r_p], signal=(c == 4))
                    tsl = slice(i * 128, (i + 1) * 128)
                    QTv = QT[:].rearrange("p (c two) t -> p c two t", two=2)
                    S.op("act", lambda e, pt=pt, tsl=tsl, QTv=QTv: e.activation(out=QTv[0:64, :, 0, tsl], in_=pt[0:64, 0:512].rearrange("p (c t) -> p c t", c=4), func=AF.Copy),
                         reads=[r_p, r_q], writes=[r_q])
                    S.op("act", lambda e, pt=pt, tsl=tsl, QTv=QTv: e.activation(out=QTv[64:128, :, 1, tsl], in_=pt[64:128, 0:512].rearrange("p (c t) -> p c t", c=4), func=AF.Copy),
                         reads=[r_p, r_q], writes=[r_q])
                    S.op("dve", lambda e, pt=pt, tsl=tsl: e.tensor_copy(out=KT2[0:64, 0, tsl], in_=pt[0:64, 512:640]), reads=[r_p, r_q], writes=[r_q])
                    S.op("dve", lambda e, pt=pt, tsl=tsl: e.tensor_copy(out=KT2[64:128, 1, tsl], in_=pt[64:128, 512:640]), reads=[r_p, r_q], writes=[r_q])
                    S.op("act", lambda e, pt=pt, tsl=tsl: e.activation(out=KT2[64:128, 0, tsl], in_=pt[0:64, 512:640], func=AF.Copy), reads=[r_p, r_q], writes=[r_q])
                    S.op("act", lambda e, pt=pt, tsl=tsl: e.activation(out=KT2[0:64, 1, tsl], in_=pt[64:128, 512:640], func=AF.Copy), reads=[r_p, r_q], writes=[r_q])
                self.S.barrier()
            prot = self.rot_sb(st, "gpe", [128, 512], BF16, 3)
            recrot = self.rot_sb(st, "grec", [64, 512], F32, 2)
            ysrot = self.rot_sb(st, "gys", [64, T], BF16, 2)
            for h in range(8):
                g, c, hh = h // 4, h // 2, h % 2
                ps_ = slice(hh * 64, (hh + 1) * 64)
                ys, r_ys = ysrot.next()
                blocks = [(n * 512, 512, list(range(NT))) for n in range(8)]
                if not last:
                    blocks.append((TL, TC, [32, 33]))
                for (q0, N, chunks) in blocks:
                    acc, r_acc = self.pB.next()
                    self.attn_block(lambda s: KT2[:, g, s * 128:(s + 1) * 128], QT[:, h, q0:q0 + N],
                                    lambda s: VA[:, s, g, :], chunks, N, acc, r_acc, prot, [r_q] + r_vat)
                    self.attn_finish(acc, r_acc, N, ys[:, q0:q0 + N], r_ys, recrot)
                ncol = T if not last else TL
                S.dma("sp", lambda e, ys=ys, h=h, ncol=ncol: e.dma_start(out=self.YT[1024 + h * 64:1024 + (h + 1) * 64, 0:ncol], in_=ys[:, 0:ncol]),
                      reads=[r_ys], writes=[self.rYT])
            self.end_phase()

    def phase_na(self, l, last):
        nc, S = self.nc, self.S
        qblocks = [(0, 4, 1, [0, 2, 4, 6])]
        for r0 in range(4, 60, 8):
            qblocks.append((r0, 8, 0, list(range(r0 - 4, r0 + 12, 2))))
        qblocks.append((60, 1, 0, [56, 58, 60, 62]))
        qblocks.append((61, 3, 1, [56, 58, 60, 62]))
        with ExitStack() as st:
            qkrot = self.rot_sb(st, "nqk", [128, 2, T], BF16, 2)
            qzrot = self.rot_sb(st, "nqz", [128, 2, T], BF16, 2)
            varot = self.rot_sb(st, "nva", [128, NT, 2, 128], BF16, 2)
            tabrot = self.rot_sb(st, "ntab", [128, 2, 2, NJ * 64], BF16, 2)
            prot = self.rot_sb(st, "npe", [128, 512], BF16, 3)
            addrot = self.rot_sb(st, "nad", [128, 512], F32, 2)
            recrot = self.rot_sb(st, "nrec", [64, 512], F32, 2)
            ysrot = self.rot_sb(st, "nys", [64, T], BF16, 2)
            for c in range(4):
                qk, r_qk = qkrot.next()
                va, r_va = varot.next()
                tab, r_tab = tabrot.next()
                S.dma("sp", lambda e, qk=qk, c=c: e.dma_start(out=qk[:, 0, :], in_=self.FT[1536 + c * 128:1536 + (c + 1) * 128, :]), reads=[self.rFT], writes=[r_qk])
                S.dma("act", lambda e, qk=qk, c=c: e.dma_start(out=qk[:, 1, :], in_=self.FT[2048 + c * 128:2048 + (c + 1) * 128, :]), reads=[self.rFT], writes=[r_qk])
                S.op("pool", lambda e, va=va: e.memset(va[:, :, :, 64:128], 1.0), writes=[r_va])
                qz, r_qz = qzrot.next()
                S.op("pool", lambda e, qz=qz: e.memset(qz[:], 0.0), writes=[r_qz])
                S.op("dve", lambda e, qz=qz, qk=qk: e.tensor_copy(out=qz[0:64, 0, :], in_=qk[0:64, 0, :]), reads=[r_qk, r_qz], writes=[r_qz])
                S.op("pool", lambda e, qz=qz, qk=qk: e.tensor_copy(out=qz[64:128, 1, :], in_=qk[64:128, 0, :]), reads=[r_qk, r_qz], writes=[r_qz])
                r_vt = [Res() for _ in range(NT)]
                for i in range(NT):
                    S.dma("sp" if i % 2 == 0 else "act", lambda e, va=va, i=i, c=c: e.dma_start(
                        out=va[:, i, :, 0:64], in_=self.TM[i * 128:(i + 1) * 128, c * 128:(c + 1) * 128].rearrange("p (g d) -> p g d", g=2)),
                        reads=[self.rTM, r_va], writes=[r_vt[i]])
                for v in range(2):
                    S.dma("pool", lambda e, tab=tab, v=v, c=c: e.dma_start(out=tab[:, v, :, :], in_=self.natab[l, v, :, 2 * c:2 * c + 2, :]), writes=[r_tab])
                for hh in range(2):
                    h = 2 * c + hh
                    ps_ = slice(hh * 64, (hh + 1) * 64)
                    ys, r_ys = ysrot.next()
                    for (r0, R, v, krows) in qblocks:
                        N = 64 * R
                        q0 = r0 * 64
                        chunks = [kr // 2 for kr in krows] + [32, 33]

                        def tab_fn(s, r0=r0, v=v, N=N, tab=tab, hh=hh):
                            if s >= 32:
                                return None
                            j0 = r0 - 2 * s + JOFF
                            return tab[:, v, hh, j0 * 64:j0 * 64 + N]
                        acc, r_acc = self.pB.next()
                        self.attn_block(lambda s, qk=qk: qk[:, 1, s * 128:(s + 1) * 128], qz[:, hh, q0:q0 + N],
                                        lambda s, va=va, hh=hh: va[:, s, hh, :], chunks, N, acc, r_acc, prot, [r_qk, r_qz, r_va, r_tab] + r_vt,
                                        tab_fn=tab_fn, addrot=addrot)
                        self.attn_finish(acc, r_acc, N, ys[:, q0:q0 + N], r_ys, recrot)
                    if not last:
                        acc, r_acc = self.pB.next()
                        self.attn_block(lambda s, qk=qk: qk[:, 1, s * 128:(s + 1) * 128], qz[:, hh, TL:T],
                                        lambda s, va=va, hh=hh: va[:, s, hh, :], [32, 33], TC, acc, r_acc, prot, [r_qk, r_qz, r_va] + r_vt)
                        self.attn_finish(acc, r_acc, TC, ys[:, TL:T], r_ys, recrot)
                    ncol = T if not last else TL
                    S.dma("sp", lambda e, ys=ys, h=h, ncol=ncol: e.dma_start(out=self.YT[512 + h * 64:512 + (h + 1) * 64, 0:ncol], in_=ys[:, 0:ncol]),
                          reads=[r_ys], writes=[self.rYT])
            self.end_phase()

    def phase_merge(self, l):
        nc, S = self.nc, self.S
        ntok = self.nt_act * 128
        with ExitStack() as st:
            WBR = self.sb(st, "WBR", [128, 12, D], BF16)
            WO = self.sb(st, "WO", [128, 8, D], BF16)
            r_w = Res()
            for b in range(3):
                S.dma("pool", lambda e, b=b: e.dma_start(out=WBR[:, b * 4:(b + 1) * 4, :], in_=self.w_br[l, b].rearrange("(k p) f -> p k f", p=128)), writes=[r_w])
            S.dma("pool", lambda e: e.dma_start(out=WO[:], in_=self.w_out[l].rearrange("(k p) f -> p k f", p=128)), writes=[r_w])
            GT = self.sb(st, "GT1", [128, 2, D], F32)
            r_gt = Res()
            for j in range(2):
                self.load_bc("sp", GT[:, j, :], r_gt, self.MOD[l, j:j + 1, 2 * D:3 * D])
            ytrot = self.rot_sb(st, "mYT", [128, 12, 512], BF16, 2)
            sgrot = self.rot_sb(st, "mSG", [128, 24, 512], BF16, 2)
            mrot = self.rot_sb(st, "mT", [128, 8, 512], BF16, 2)
            m0rot = self.rot_sb(st, "m0", [128, 512], F32, 2)
            m1rot = self.rot_sb(st, "m1", [128, 512], F32, 2)
            m2rot = self.rot_sb(st, "m2", [128, 512], F32, 2)
            xrot = self.rot_sb(st, "mx", [128, D], F32, 2)
            xorot = self.rot_sb(st, "mxo", [128, D], F32, 2)
            ytv = self.YT.rearrange("(j p) t -> p j t", p=128)
            sgv = self.FT[3840:INC, :].rearrange("(j p) t -> p j t", p=128)
            for (t0, tw) in ntiles512(ntok):
                yt, r_yt = ytrot.next()
                sg, r_sg = sgrot.next()
                S.dma("sp", lambda e, yt=yt, t0=t0, tw=tw: e.dma_start(out=yt[:, :, 0:tw], in_=ytv[:, :, t0:t0 + tw]), reads=[self.rYT], writes=[r_yt])
                S.dma("act", lambda e, sg=sg, t0=t0, tw=tw: e.dma_start(out=sg[:, :, 0:tw], in_=sgv[:, :, t0:t0 + tw]), reads=[self.rFT], writes=[r_sg])
                mT, r_m = mrot.next()
                for f in range(8):
                    pb = []
                    for b in range(3):
                        pt, r_p = self.pA.next()
                        for kc in range(4):
                            S.op("pe", lambda e, pt=pt, b=b, kc=kc, f=f, yt=yt, tw=tw: e.matmul(
                                pt[:, 0:tw], lhsT=WBR[:, b * 4 + kc, f * 128:(f + 1) * 128], rhs=yt[:, b * 4 + kc, 0:tw], start=(kc == 0), stop=(kc == 3)),
                                reads=[r_w, r_yt], writes=[r_p], signal=(kc == 3))
                        pb.append((pt, r_p))
                    a0, r_a0 = m0rot.next()
                    a1, r_a1 = m1rot.next()
                    a2, r_a2 = m2rot.next()
                    for b, (a, r_a) in enumerate(((a0, r_a0), (a1, r_a1), (a2, r_a2))):
                        pt, r_p = pb[b]
                        S.op("dve", lambda e, a=a, pt=pt, sg=sg, b=b, f=f, tw=tw: e.tensor_tensor(out=a[:, 0:tw], in0=pt[:, 0:tw], in1=sg[:, b * 8 + f, 0:tw], op=ALU.mult),
                             reads=[r_p, r_sg], writes=[r_a])
                    S.op("pool", lambda e, a0=a0, a1=a1, tw=tw: e.tensor_tensor(out=a0[:, 0:tw], in0=a0[:, 0:tw], in1=a1[:, 0:tw], op=ALU.add),
                         reads=[r_a0, r_a1], writes=[r_a0])
                    S.op("pool", lambda e, a0=a0, a2=a2, mT=mT, f=f, tw=tw: e.tensor_tensor(out=mT[:, f, 0:tw], in0=a0[:, 0:tw], in1=a2[:, 0:tw], op=ALU.add),
                         reads=[r_a0, r_a2], writes=[r_m])
                for ts in range(tw // 128):
                    i = t0 // 128 + ts
                    j = 0 if i < 32 else 1
                    xt, r_x = xrot.next()
                    S.dma("sp", lambda e, xt=xt, i=i: e.dma_start(out=xt[:], in_=self.Xsrc[i * 128:(i + 1) * 128, :]), reads=[self.rX[i]], writes=[r_x])
                    xo, r_xo = xorot.next()
                    for half in range(2):
                        pt, r_p = self.pA.next()
                        for f in range(8):
                            S.op("pe", lambda e, pt=pt, f=f, mT=mT, ts=ts, half=half: e.matmul(
                                pt[:, :], lhsT=mT[:, f, ts * 128:(ts + 1) * 128], rhs=WO[:, f, half * 512:(half + 1) * 512], start=(f == 0), stop=(f == 7)),
                                reads=[r_w, r_m], writes=[r_p], signal=(f == 7))
                        hs = slice(half * 512, (half + 1) * 512)
                        S.op("dve", lambda e, pt=pt, xo=xo, hs=hs, j=j: e.tensor_tensor(out=xo[:, hs], in0=pt[:, :], in1=GT[:, j, hs], op=ALU.mult),
                             reads=[r_p, r_gt], writes=[r_xo])
                    S.op("pool", lambda e, xo=xo, xt=xt: e.tensor_tensor(out=xo[:], in0=xo[:], in1=xt[:], op=ALU.add), reads=[r_xo, r_x], writes=[r_xo])
                    S.dma("sp", lambda e, xo=xo, i=i: e.dma_start(out=self.X[i * 128:(i + 1) * 128, :], in_=xo[:]), reads=[r_xo], writes=[self.rX[i]])
            self.end_phase()
            self.Xsrc = self.X

    def phase_route(self, l):
        nc, S = self.nc, self.S
        with ExitStack() as st:
            G2 = self.sb(st, "G2", [128, 2, D], F32)
            SH2 = self.sb(st, "SH2", [128, 2, D], F32)
            NG = self.sb(st, "NG2", [128, D], F32)
            r_g = Res()
            self.load_bc("sp", NG[:], r_g, self.norm2_g[l:l + 1, :])
            for j in range(2):
                self.load_bc("sp", SH2[:, j, :], r_g, self.MOD[l, j:j + 1, 3 * D:4 * D])
                self.load_bc("act", G2[:, j, :], r_g, self.MOD[l, j:j + 1, 4 * D:5 * D])
            for j in range(2):
                S.op("dve", lambda e, j=j: e.scalar_tensor_tensor(out=G2[:, j, :], in0=G2[:, j, :], scalar=1.0, in1=NG[:], op0=ALU.add, op1=ALU.mult),
                     reads=[r_g], writes=[r_g])
            RW = self.sb(st, "RW", [128, 8, NE], F32)
            RB = self.sb(st, "RB", [128, NE], F32)
            S.dma("sp", lambda e: e.dma_start(out=RW[:], in_=self.router_w[l].rearrange("(k p) e -> p k e", p=128)), writes=[r_g])
            self.load_bc("sp", RB[:], r_g, self.router_b[l:l + 1, :])
            UTf = self.sb(st, "UTf", [128, 128], F32)
            UT = self.sb(st, "UT", [128, 128], BF16)
            ONES = self.sb(st, "ONES", [128, 128], BF16)
            EB = self.sb(st, "EB", [128, NE], F32)
            CNT = self.sb(st, "CNT", [128, NE], F32)
            r_c = Res()
            r_cnt = Res()
            S.op("pool", lambda e: e.memset(UTf[:], 1.0), writes=[r_c])
            S.op("pool", lambda e: e.affine_select(out=UTf[:], in_=UTf[:], pattern=[[1, 128]], compare_op=ALU.is_gt, fill=0.0, base=0, channel_multiplier=-1),
                 reads=[r_c], writes=[r_c])
            S.op("dve", lambda e: e.tensor_copy(out=UT[:], in_=UTf[:]), reads=[r_c], writes=[r_c])
            S.op("pool", lambda e: e.memset(ONES[:], 1.0), writes=[r_c])
            S.op("pool", lambda e: e.iota(EB[:], pattern=[[CAP, NE]], base=0, channel_multiplier=0, allow_small_or_imprecise_dtypes=True), writes=[r_c])
            S.op("pool", lambda e: e.memset(CNT[:], 0.0), writes=[r_cnt])
            xrot = self.rot_sb(st, "rx", [128, D], F32, 2)
            hrot = self.rot_sb(st, "rh", [128, D], F32, 2)
            hbrot = self.rot_sb(st, "rhb", [128, D], BF16, 6)
            htrot = self.rot_sb(st, "rht", [128, 8, 128], F32, 2)
            strot = self.stat_tiles(st, "rst")
            smrot = Rot([({n: self.sb(st, "rs%s%d" % (n, i), [128, w], dt) for n, w, dt in (
                ("lg", NE, F32), ("t8", 8, F32), ("nm", 1, F32), ("e4", 4, F32), ("sm", 1, F32), ("mk", NE, BF16),
                ("pos", NE, F32), ("oh", NE, F32), ("df", 4, F32))}, Res()) for i in range(2)])
            for i in range(self.nt_act):
                j = 0 if i < 32 else 1
                xt, r_x = xrot.next()
                S.dma("sp", lambda e, xt=xt, i=i: e.dma_start(out=xt[:], in_=self.X[i * 128:(i + 1) * 128, :]), reads=[self.rX[i]], writes=[r_x])
                stt = strot.next()
                rstd, r_s = self.rms_rstd(stt, xt, r_x, D)
                h2, r_h = hrot.next()
                S.op("dve", lambda e, h2=h2, xt=xt, rstd=rstd, j=j: e.scalar_tensor_tensor(out=h2[:], in0=xt[:], scalar=rstd[:, 0:1], in1=G2[:, j, :], op0=ALU.mult, op1=ALU.mult),
                     reads=[r_x, r_s, r_g], writes=[r_h])
                S.op("pool", lambda e, h2=h2, j=j: e.tensor_tensor(out=h2[:], in0=h2[:], in1=SH2[:, j, :], op=ALU.add), reads=[r_h, r_g], writes=[r_h])
                hb, r_hb = hbrot.next()
                S.op("act", lambda e, hb=hb, h2=h2: e.activation(out=hb[:], in_=h2[:], func=AF.Copy), reads=[r_h], writes=[r_hb])
                ht, r_ht = htrot.next()
                for half in range(2):
                    pt, r_p = self.pA.next()
                    for kk in range(4):
                        k = half * 4 + kk
                        S.op("pe", lambda e, pt=pt, h2=h2, k=k, kk=kk: e.transpose(out=pt[:, kk * 128:(kk + 1) * 128], in_=h2[:, k * 128:(k + 1) * 128], identity=self.identf[:]),
                             reads=[r_h, self.r_id], writes=[r_p], signal=(kk == 3))
                    if half == 0:
                        S.op("act", lambda e, pt=pt, ht=ht: e.activation(out=ht[:, 0:4, :], in_=pt[:, :].rearrange("p (k t) -> p k t", k=4), func=AF.Copy), reads=[r_p], writes=[r_ht])
                    else:
                        S.op("dve", lambda e, pt=pt, ht=ht: e.tensor_copy(out=ht[:, 4:8, :], in_=pt[:, :].rearrange("p (k t) -> p k t", k=4)), reads=[r_p, r_ht], writes=[r_ht])
                pl, r_pl = self.pB.next()
                for k in range(8):
                    S.op("pe", lambda e, pl=pl, ht=ht, k=k: e.matmul(pl[:, 0:NE], lhsT=ht[:, k, :], rhs=RW[:, k, :], start=(k == 0), stop=(k == 7)),
                         reads=[r_ht, r_g], writes=[r_pl], signal=(k == 7))
                sm, r_sm = smrot.next()
                S.op("dve", lambda e, sm=sm, pl=pl: e.tensor_tensor(out=sm["lg"][:], in0=pl[:, 0:NE], in1=RB[:], op=ALU.add), reads=[r_pl, r_g], writes=[r_sm])
                S.op("dve", lambda e, sm=sm: e.max(out=sm["t8"][:], in_=sm["lg"][:]), reads=[r_sm], writes=[r_sm])
                S.op("dve", lambda e, sm=sm: e.tensor_scalar(out=sm["nm"][:], in0=sm["t8"][:, 0:1], scalar1=-1.0, scalar2=None, op0=ALU.mult), reads=[r_sm], writes=[r_sm])
                S.op("act", lambda e, sm=sm: e.activation(out=sm["e4"][:], in_=sm["t8"][:, 0:4], func=AF.Exp, bias=sm["nm"][:, 0:1], accum_out=sm["sm"][:, 0:1]), reads=[r_sm], writes=[r_sm])
                S.op("dve", lambda e, sm=sm: e.reciprocal(out=sm["sm"][:], in_=sm["sm"][:]), reads=[r_sm], writes=[r_sm])
                S.op("dve", lambda e, sm=sm, i=i: e.tensor_scalar(out=self.GATES[:, i, :], in0=sm["e4"][:], scalar1=sm["sm"][:, 0:1], scalar2=None, op0=ALU.mult),
                     reads=[r_sm], writes=[self.rROUTE[i]])
                S.op("dve", lambda e, sm=sm: e.tensor_scalar(out=sm["mk"][:], in0=sm["lg"][:], scalar1=sm["t8"][:, 3:4], scalar2=None, op0=ALU.is_ge), reads=[r_sm], writes=[r_sm])
                pp, r_pp = self.pB.next()
                S.op("pe", lambda e, pp=pp, sm=sm: e.matmul(pp[:, 0:NE], lhsT=UT[:], rhs=sm["mk"][:], start=True, stop=True), reads=[r_sm, r_c], writes=[r_pp], signal=False)
                S.op("pe", lambda e, pp=pp, sm=sm: e.matmul(pp[:, 64:64 + NE], lhsT=ONES[:], rhs=sm["mk"][:], start=True, stop=True), reads=[r_sm, r_c], writes=[r_pp])
                S.op("dve", lambda e, sm=sm, pp=pp: e.tensor_tensor(out=sm["pos"][:], in0=pp[:, 0:NE], in1=CNT[:], op=ALU.add), reads=[r_pp, r_cnt, r_sm], writes=[r_sm])
                S.op("dve", lambda e, pp=pp: e.tensor_tensor(out=CNT[:], in0=pp[:, 64:64 + NE], in1=CNT[:], op=ALU.add), reads=[r_pp, r_cnt], writes=[r_cnt])
                S.op("dve", lambda e, sm=sm: e.scalar_tensor_tensor(out=sm["pos"][:], in0=sm["pos"][:], scalar=float(CAP - 1), in1=EB[:], op0=ALU.min, op1=ALU.add),
                     reads=[r_sm, r_c], writes=[r_sm])
                for k in range(4):
                    S.op("dve", lambda e, sm=sm, k=k: e.scalar_tensor_tensor(out=sm["oh"][:], in0=sm["lg"][:], scalar=sm["t8"][:, k:k + 1], in1=sm["pos"][:], op0=ALU.is_equal, op1=ALU.mult),
                         reads=[r_sm], writes=[r_sm])
                    S.op("dve", lambda e, sm=sm, k=k: e.reduce_sum(out=sm["df"][:, k:k + 1], in_=sm["oh"][:], axis=AX.X), reads=[r_sm], writes=[r_sm])
                S.op("dve", lambda e, sm=sm, i=i: e.tensor_copy(out=self.DEST[:, i, :], in_=sm["df"][:]), reads=[r_sm, self.rROUTE[i]], writes=[self.rROUTE[i]])
                for k in range(4):
                    S.dma("pool", lambda e, hb=hb, i=i, k=k: e.indirect_dma_start(
                        out=self.XE[:, :], out_offset=bass.IndirectOffsetOnAxis(ap=self.DEST[:, i, k:k + 1], axis=0), in_=hb[:], in_offset=None),
                        reads=[r_hb, self.rROUTE[i]], writes=[self.rXE])
            self.end_phase()

    def phase_experts(self, l):
        nc, S = self.nc, self.S
        NTE = CAP // 512
        with ExitStack() as st:
            xerot = self.rot_sb(st, "xe", [128, 4, D], BF16, 2)
            xtrot = self.rot_sb(st, "xeT", [128, 8, 512], BF16, 2)
            wgrot = self.rot_sb(st, "wg", [128, 8, D], BF16, 2)
            wurot = self.rot_sb(st, "wu", [128, 8, D], BF16, 2)
            wdrot = self.rot_sb(st, "wd", [128, 8, D], BF16, 1)
            stgrot = self.rot_sb(st, "wstg", [128, 4, 512], F32, 3)
            bgrot = self.rot_sb(st, "bgu", [128, 8, 2], F32, 2)
            bdrot = self.rot_sb(st, "bd", [128, D], F32, 2)
            atrot = self.rot_sb(st, "aT", [128, 8, 512], BF16, 2)
            gsrot = self.rot_sb(st, "gs", [128, 512], F32, 2)
            sgrot = self.rot_sb(st, "sg", [128, 512], F32, 2)
            usrot = self.rot_sb(st, "us", [128, 512], F32, 2)
            yrot = self.rot_sb(st, "ye", [128, D], F32, 2)
            for e_ in range(NE):
                wg, r_wg = wgrot.next()
                wu, r_wu = wurot.next()
                wd, r_wd = wdrot.next()
                wguv = self.w_gu[l, e_].rearrange("(k p) c -> p k c", p=128)
                for kh in range(2):
                    for q in range(4):
                        sgt, r_st = stgrot.next()
                        S.dma("sp" if (kh * 4 + q) % 2 == 0 else "act", lambda e, sgt=sgt, q=q, kh=kh, wguv=wguv: e.dma_start(
                            out=sgt[:], in_=wguv[:, kh * 4:(kh + 1) * 4, q * 512:(q + 1) * 512]), writes=[r_st])
                        sv = sgt[:].rearrange("p k (c two) -> p k c two", two=2)
                        S.op("pool", lambda e, wg=wg, sv=sv, q=q, kh=kh: e.tensor_copy(out=wg[:, kh * 4:(kh + 1) * 4, q * 256:(q + 1) * 256], in_=sv[:, :, :, 0]),
                             reads=[r_st], writes=[r_wg])
                        S.op("act", lambda e, wu=wu, sv=sv, q=q, kh=kh: e.activation(out=wu[:, kh * 4:(kh + 1) * 4, q * 256:(q + 1) * 256], in_=sv[:, :, :, 1], func=AF.Copy),
                             reads=[r_st], writes=[r_wu])
                S.dma("pool", lambda e, wd=wd, e_=e_: e.dma_start(out=wd[:], in_=self.w_down[l, e_].rearrange("(k p) c -> p k c", p=128)), writes=[r_wd])
                bg, r_bg = bgrot.next()
                S.dma("sp", lambda e, bg=bg, e_=e_: e.dma_start(out=bg[:], in_=self.b_gu[l, e_, :].rearrange("(k p two) -> p k two", p=128, two=2),
                                                               allow_slow_non_contiguous=True), writes=[r_bg])
                bd, r_bd = bdrot.next()
                S.dma("act", lambda e, bd=bd, e_=e_: e.dma_start(out=bd[:], in_=self.b_down[l, e_:e_ + 1, :].to_broadcast([128, D])), writes=[r_bd])
                for nt in range(NTE):
                    row0 = e_ * CAP + nt * 512
                    self.expert_ntile(row0, (wg, r_wg), (wu, r_wu), (wd, r_wd), (bg, r_bg), (bd, r_bd),
                                      xerot, xtrot, atrot, gsrot, sgrot, usrot, yrot)
            self.end_phase()

    def expert_ntile(self, row0, wg_, wu_, wd_, bg_, bd_, xerot, xtrot, atrot, gsrot, sgrot, usrot, yrot):
        S = self.S
        wg, r_wg = wg_
        wu, r_wu = wu_
        wd, r_wd = wd_
        bg, r_bg = bg_
        bd, r_bd = bd_
        xe, r_xe = xerot.next()
        S.dma("sp", lambda e: e.dma_start(out=xe[:], in_=self.XE[row0:row0 + 512, :].rearrange("(j p) d -> p j d", p=128)),
              reads=[self.rXE], writes=[r_xe])
        xT, r_xT = xtrot.next()
        for jt in range(4):
            pt, r_p = self.pT.next()
            for k in range(8):
                S.op("pe", lambda e, pt=pt, jt=jt, k=k: e.transpose(out=pt[:, k * 128:(k + 1) * 128], in_=xe[:, jt, k * 128:(k + 1) * 128], identity=self.identb[:]),
                     reads=[r_xe, self.r_id], writes=[r_p], signal=(k == 7))
            S.op("dve", lambda e, pt=pt, jt=jt: e.tensor_copy(out=xT[:, :, jt * 128:(jt + 1) * 128], in_=pt[:, :].rearrange("p (k t) -> p k t", k=8)),
                 reads=[r_p], writes=[r_xT])
        aT, r_aT = atrot.next()
        for fk in range(8):
            pg, r_pg = self.pA.next()
            pu, r_pu = self.pA.next()
            for k in range(8):
                S.op("pe", lambda e, pg=pg, k=k, fk=fk: e.matmul(pg[:, :], lhsT=wg[:, k, fk * 128:(fk + 1) * 128], rhs=xT[:, k, :], start=(k == 0), stop=(k == 7)),
                     reads=[r_wg, r_xT], writes=[r_pg], signal=(k == 7))
            for k in range(8):
                S.op("pe", lambda e, pu=pu, k=k, fk=fk: e.matmul(pu[:, :], lhsT=wu[:, k, fk * 128:(fk + 1) * 128], rhs=xT[:, k, :], start=(k == 0), stop=(k == 7)),
                     reads=[r_wu, r_xT], writes=[r_pu], signal=(k == 7))
            gs, r_gs = gsrot.next()
            sg, r_sg = sgrot.next()
            us, r_us = usrot.next()
            S.op("dve", lambda e, gs=gs, pg=pg, fk=fk: e.tensor_scalar(out=gs[:], in0=pg[:, :], scalar1=bg[:, fk, 0:1], scalar2=7.0, op0=ALU.add, op1=ALU.min),
                 reads=[r_pg, r_bg], writes=[r_gs])
            S.op("act", lambda e, sg=sg, gs=gs: e.activation(out=sg[:], in_=gs[:], func=AF.Sigmoid, scale=1.702), reads=[r_gs], writes=[r_sg])
            S.op("dve", lambda e, us=us, pu=pu, fk=fk: e.tensor_scalar(out=us[:], in0=pu[:, :], scalar1=bg[:, fk, 1:2], scalar2=7.0, op0=ALU.add, op1=ALU.min),
                 reads=[r_pu, r_bg], writes=[r_us])
            S.op("dve", lambda e, us=us: e.tensor_scalar(out=us[:], in0=us[:], scalar1=-7.0, scalar2=1.0, op0=ALU.max, op1=ALU.add),
                 reads=[r_us], writes=[r_us])
            S.op("pool", lambda e, gs=gs, sg=sg: e.tensor_tensor(out=gs[:], in0=gs[:], in1=sg[:], op=ALU.mult), reads=[r_gs, r_sg], writes=[r_gs])
            S.op("dve", lambda e, us=us, gs=gs, fk=fk: e.tensor_tensor(out=aT[:, fk, :], in0=us[:], in1=gs[:], op=ALU.mult),
                 reads=[r_us, r_gs], writes=[r_aT])
        for jt in range(4):
            ye, r_ye = yrot.next()
            for half in range(2):
                pt, r_p = self.pA.next()
                for fk in range(8):
                    S.op("pe", lambda e, pt=pt, fk=fk, jt=jt, half=half: e.matmul(
                        pt[:, :], lhsT=aT[:, fk, jt * 128:(jt + 1) * 128], rhs=wd[:, fk, half * 512:(half + 1) * 512], start=(fk == 0), stop=(fk == 7)),
                        reads=[r_aT, r_wd], writes=[r_p], signal=(fk == 7))
                hs = slice(half * 512, (half + 1) * 512)
                S.op("dve", lambda e, ye=ye, pt=pt, hs=hs: e.tensor_tensor(out=ye[:, hs], in0=pt[:, :], in1=bd[:, hs], op=ALU.add),
                     reads=[r_p, r_bd], writes=[r_ye])
            S.dma("sp", lambda e, ye=ye, jt=jt: e.dma_start(out=self.YE[row0 + jt * 128:row0 + (jt + 1) * 128, :], in_=ye[:]),
                  reads=[r_ye], writes=[self.rYE])

    def phase_combine(self, l, last):
        nc, S = self.nc, self.S
        with ExitStack() as st:
            GT = self.sb(st, "GT2", [128, 2, D], F32)
            r_gt = Res()
            for j in range(2):
                self.load_bc("sp", GT[:, j, :], r_gt, self.MOD[l, j:j + 1, 5 * D:6 * D])
            if last:
                FG = self.sb(st, "FG", [128, D], F32)
                self.load_bc("sp", FG[:], r_gt, self.final_g[0:1, :])
            ykrot = self.rot_sb(st, "yk", [128, 4, D], F32, 2)
            accrot = self.rot_sb(st, "kacc", [128, D], F32, 2)
            xrot = self.rot_sb(st, "kx", [128, D], F32, 2)
            orot = self.rot_sb(st, "ko", [128, D], F32, 2)
            strot = self.stat_tiles(st, "kst")
            for i in range(self.nt_act):
                j = 0 if i < 32 else 1
                yk, r_yk = ykrot.next()
                for k in range(4):
                    S.dma("pool", lambda e, yk=yk, i=i, k=k: e.indirect_dma_start(
                        out=yk[:, k, :], out_offset=None, in_=self.YE[:, :], in_offset=bass.IndirectOffsetOnAxis(ap=self.DEST[:, i, k:k + 1], axis=0)),
                        reads=[self.rYE, self.rROUTE[i]], writes=[r_yk])
                xt, r_x = xrot.next()
                S.dma("sp", lambda e, xt=xt, i=i: e.dma_start(out=xt[:], in_=self.X[i * 128:(i + 1) * 128, :]), reads=[self.rX[i]], writes=[r_x])
                acc, r_a = accrot.next()
                S.op("dve", lambda e, acc=acc, yk=yk, i=i: e.tensor_scalar(out=acc[:], in0=yk[:, 0, :], scalar1=self.GATES[:, i, 0:1], scalar2=None, op0=ALU.mult),
                     reads=[r_yk, self.rROUTE[i]], writes=[r_a])
                for k in range(1, 4):
                    S.op("dve", lambda e, acc=acc, yk=yk, i=i, k=k: e.scalar_tensor_tensor(out=acc[:], in0=yk[:, k, :], scalar=self.GATES[:, i, k:k + 1], in1=acc[:], op0=ALU.mult, op1=ALU.add),
                         reads=[r_yk, self.rROUTE[i], r_a], writes=[r_a])
                S.op("pool", lambda e, acc=acc, j=j: e.tensor_tensor(out=acc[:], in0=acc[:], in1=GT[:, j, :], op=ALU.mult), reads=[r_a, r_gt], writes=[r_a])
                xo, r_xo = orot.next()
                S.op("pool", lambda e, xo=xo, acc=acc, xt=xt: e.tensor_tensor(out=xo[:], in0=acc[:], in1=xt[:], op=ALU.add), reads=[r_a, r_x], writes=[r_xo])
                if not last:
                    S.dma("sp", lambda e, xo=xo, i=i: e.dma_start(out=self.X[i * 128:(i + 1) * 128, :], in_=xo[:]), reads=[r_xo], writes=[self.rX[i]])
                else:
                    stt = strot.next()
                    rstd, r_s = self.rms_rstd(stt, xo, r_xo, D)
                    S.op("dve", lambda e, xo=xo, rstd=rstd: e.scalar_tensor_tensor(out=xo[:], in0=xo[:], scalar=rstd[:, 0:1], in1=FG[:], op0=ALU.mult, op1=ALU.mult),
                         reads=[r_xo, r_s, r_gt], writes=[r_xo])
                    S.dma("sp", lambda e, xo=xo, i=i: e.dma_start(out=self.out[i * 128:(i + 1) * 128, :], in_=xo[:]), reads=[r_xo], writes=[self.rOUT])
            self.end_phase()


def _na_table(rpb):
    a = np.arange(2)[:, None, None, None]
    kc = np.arange(64)[None, :, None, None]
    j = np.arange(NJ)[None, None, :, None]
    qc = np.arange(64)[None, None, None, :]
    dr = a - (j - JOFF) + 0 * kc + 0 * qc
    cs = np.clip(qc - 8, 0, 48)
    colv = (kc >= cs) & (kc < cs + 16)
    dc = kc - qc + 0 * a + 0 * j
    out = np.empty((2, 128, 8, NJ * 64), np.float32)
    for v in range(2):
        rowv = ((dr >= -4) & (dr <= 3)) if v == 0 else ((dr >= -7) & (dr <= 7))
        valid = np.broadcast_to(rowv & colv, (2, 64, NJ, 64))
        ri = np.clip(dr + 7, 0, 14)
        ci = np.clip(dc + 15, 0, 30)
        ri = np.broadcast_to(ri, (2, 64, NJ, 64))
        ci = np.broadcast_to(ci, (2, 64, NJ, 64))
        for h in range(8):
            vals = rpb[h][ri, ci]
            tbl = np.where(valid, vals, np.float32(NEG)).astype(np.float32)
            out[v, :, h, :] = tbl.reshape(128, NJ * 64)
    return out


def _rope_tables():
    half = 32
    inv = (10000.0 ** (-np.arange(0, half, 2, dtype=np.float32) / half)).astype(np.float32)
    t = np.arange(TL)
    ang_r = (t // 64).astype(np.float32)[:, None] * inv
    ang_c = (t % 64).astype(np.float32)[:, None] * inv
    cos = np.zeros((TL, 64), np.float32)
    sin = np.zeros((TL, 64), np.float32)
    for base, ang in ((0, ang_r), (32, ang_c)):
        c, s = np.cos(ang).astype(np.float32), np.sin(ang).astype(np.float32)
        cos[:, base:base + 16] = c
        cos[:, base + 16:base + 32] = c
        sin[:, base:base + 16] = -s
        sin[:, base + 16:base + 32] = s
    return np.tile(cos, (1, 10)), np.tile(sin, (1, 10))


def make_in_maps(inputs, n_cores=8):
    f = lambda a: np.ascontiguousarray(np.asarray(a, dtype=np.float32))
    x, c, ctx, c_ctx = f(inputs["x"]), f(inputs["c"]), f(inputs["ctx"]), f(inputs["c_ctx"])
    natab = np.stack([_na_table(f(inputs["na_rpb"])[l]) for l in range(DEPTH)])
    qg, kg = f(inputs["q_norm_g"]), f(inputs["k_norm_g"])
    qkgain = np.concatenate([np.tile(qg, (1, 8)), np.tile(kg, (1, 2))], axis=1)
    ropec, ropes = _rope_tables()
    w_br = np.stack([f(inputs["w_br_conv"]), f(inputs["w_br_na"]), f(inputs["w_br_gqa"])], axis=1)
    shared = dict(
        ada_w=f(inputs["ada_w"]), ada_b=f(inputs["ada_b"]), norm1_g=f(inputs["norm1_g"]), norm2_g=f(inputs["norm2_g"]),
        w_in=f(inputs["w_in"]), conv_w=f(inputs["conv_w"]), natab=natab, qkgain=np.ascontiguousarray(qkgain),
        ropec=ropec, ropes=ropes, w_br=np.ascontiguousarray(w_br), w_out=f(inputs["w_out"]),
        router_w=f(inputs["router_w"]), router_b=f(inputs["router_b"]), w_gu=f(inputs["w_gu"]), b_gu=f(inputs["b_gu"]),
        w_down=f(inputs["w_down"]), b_down=f(inputs["b_down"]), final_g=f(inputs["final_g"]).reshape(1, D))
    maps = []
    for b in range(n_cores):
        m = dict(shared)
        m["xin"] = np.ascontiguousarray(np.concatenate([x[b], ctx[b]], axis=0))
        m["cvec"] = np.ascontiguousarray(np.stack([c[b], c_ctx], axis=0))
        maps.append(m)
    return maps


def kernel(**inputs):
    nc = bass.Bass("TRN2", target_bir_lowering=False)
    Builder(nc).build()
    maps = make_in_maps(inputs)
    res = run_bass_kernel_spmd(nc, maps, core_ids=list(range(8)))
    return np.stack([np.asarray(r["out"], dtype=np.float32) for r in res.results], axis=0)
```

```python
import numpy as np
from contextlib import ExitStack
import concourse.bass as bass
import concourse.mybir as mybir
from concourse.bass_utils import run_bass_kernel_spmd

F32 = mybir.dt.float32
BF16 = mybir.dt.bfloat16
I32 = mybir.dt.int32
AF = mybir.ActivationFunctionType
ALU = mybir.AluOpType
AX = mybir.AxisListType

D = 1024
TL = 4096
TC = 256
T = TL + TC
NT = T // 128
DEPTH = 2
NE = 32
CAP = 2048
EPS = 1e-6
NEG = -30000.0
NJ = 22
JOFF = 10
INC = 6912
DYNAMIC_SKIP = True


class Res:
    __slots__ = ("w", "rs")

    def __init__(self):
        self.w = None
        self.rs = {}


class Sched:
    ENGS = ("pe", "act", "dve", "pool", "sp")
    NQ = 20

    def __init__(self, nc, stack, same_engine_sync=True):
        self.nc = nc
        self.eng = {"pe": nc.tensor, "act": nc.scalar, "dve": nc.vector,
                    "pool": nc.gpsimd, "sp": nc.sync}
        self.sem = {}
        self.cnt = {}
        self.seen = {e: {} for e in self.ENGS}
        self.prog = {e: [] for e in self.ENGS}
        self.same_engine_sync = same_engine_sync
        for e in self.ENGS:
            self.sem[e] = stack.enter_context(nc.semaphore("c_" + e))
            self.cnt[e] = 0
        self.dq = {}
        for q in ("sp", "act", "pool"):
            keys = []
            for i in range(self.NQ):
                k = "d_%s_%d" % (q, i)
                self.sem[k] = stack.enter_context(nc.semaphore(k))
                self.cnt[k] = 0
                keys.append(k)
            self.dq[q] = [keys, 0]

    def _deps(self, engine, reads, writes, extra=()):
        need = {}
        for r in reads:
            if r.w is not None:
                k, v = r.w
                if need.get(k, 0) < v:
                    need[k] = v
        for w in writes:
            if w.w is not None:
                k, v = w.w
                if need.get(k, 0) < v:
                    need[k] = v
            for k, v in w.rs.items():
                if need.get(k, 0) < v:
                    need[k] = v
        for k, v in extra:
            if need.get(k, 0) < v:
                need[k] = v
        out = []
        seen = self.seen[engine]
        for k, v in need.items():
            if k == engine and (engine == "pe" or not self.same_engine_sync):
                continue
            if seen.get(k, 0) >= v:
                continue
            seen[k] = v
            out.append((k, v))
        return out

    def _mark(self, ev, reads, writes):
        k, v = ev
        for r in reads:
            if r.rs.get(k, 0) < v:
                r.rs[k] = v
        for w in writes:
            w.w = ev
            w.rs = {}

    def op(self, engine, fn, reads=(), writes=(), signal=True):
        waits = self._deps(engine, reads, writes)
        sem = self.sem[engine]
        if signal:
            self.cnt[engine] += 1
        ev = (engine, self.cnt[engine] if signal else self.cnt[engine] + 1)
        sems = self.sem

        def emit(eng):
            for k, v in waits:
                eng.wait_ge(sems[k], v)
            ins = fn(eng)
            if signal:
                ins.then_inc(sem, 1)

        self.prog[engine].append(emit)
        self._mark(ev, reads, writes)
        return ev

    def dma(self, queue, fn, reads=(), writes=()):
        keys, idx = self.dq[queue]
        k = keys[idx % len(keys)]
        self.dq[queue][1] = idx + 1
        prev = self.cnt[k]
        extra = [(k, prev)] if prev > 0 else []
        waits = self._deps(queue, reads, writes, extra)
        self.cnt[k] = prev + 16
        ev = (k, prev + 16)
        sems = self.sem

        def emit(eng):
            for kk, v in waits:
                eng.wait_ge(sems[kk], v)
            fn(eng).then_inc(sems[k], 16)

        self.prog[queue].append(emit)
        self._mark(ev, reads, writes)
        return ev

    def load_count(self, ap):
        for e in self.ENGS:
            self.prog[e].append(("ldreg", ap))

    def cond_begin(self, thr):
        self._cond = dict(thr=thr, cnt=dict(self.cnt), seen={e: dict(v) for e, v in self.seen.items()},
                          dq={q: self.dq[q][1] for q in self.dq})
        for e in self.ENGS:
            self.prog[e].append(("if", thr))

    def cond_end(self):
        c = self._cond
        sems = self.sem
        for e in self.ENGS:
            delta = self.cnt[e] - c["cnt"][e]
            dl = []
            if e in self.dq:
                keys = self.dq[e][0]
                run = dict()
                for idx in range(c["dq"][e], self.dq[e][1]):
                    k = keys[idx % len(keys)]
                    prev = run.get(k, c["cnt"][k])
                    dl.append((k, prev))
                    run[k] = prev + 16

            def comp(eng, e=e, delta=delta, dl=dl):
                if e != "sp":
                    eng.drain()
                if delta > 0:
                    eng.sem_inc(sems[e], delta)
                for k, prev in dl:
                    if prev > 0:
                        eng.wait_ge(sems[k], prev)
                    eng.sem_inc(sems[k], 16)

            self.prog[e].append(("endif", comp))
        self.seen = c["seen"]
        self._cond = None

    def barrier(self):
        sems = self.sem
        for e in self.ENGS:
            waits = []
            seen = self.seen[e]
            for k, v in self.cnt.items():
                if v > 0 and seen.get(k, 0) < v:
                    seen[k] = v
                    waits.append((k, v))

            def emit(eng, waits=waits):
                for k, v in waits:
                    eng.wait_ge(sems[k], v)

            self.prog[e].append(emit)

    def _run(self, name, eng):
        items = self.prog[name]
        if not hasattr(self, "regs"):
            self.regs = {}
        i = 0
        n = len(items)
        while i < n:
            it = items[i]
            if callable(it):
                it(eng)
                i += 1
                continue
            kind = it[0]
            if kind == "ldreg":
                if name not in self.regs:
                    self.regs[name] = eng.alloc_register("cnt_" + name)
                eng.reg_load(self.regs[name], it[1])
                i += 1
            elif kind == "if":
                thr = it[1]
                j = i + 1
                while not (isinstance(items[j], tuple) and items[j][0] == "endif"):
                    j += 1
                body = items[i + 1:j]
                comp = items[j][1]
                with eng.If_lt(self.regs[name], thr + 1):
                    comp(eng)
                with eng.Else():
                    for f in body:
                        f(eng)
                i = j + 1
            else:
                raise RuntimeError("bad prog item")

    def flush(self):
        nc = self.nc
        with nc.Block() as block:
            @block.sync
            def _(e):
                self._run("sp", e)

            @block.scalar
            def _(e):
                self._run("act", e)

            @block.vector
            def _(e):
                self._run("dve", e)

            @block.gpsimd
            def _(e):
                self._run("pool", e)

            @block.tensor
            def _(e):
                self._run("pe", e)
        self.prog = {e: [] for e in self.ENGS}


class Rot:
    def __init__(self, items):
        self.items = items
        self.i = 0

    def next(self):
        it = self.items[self.i % len(self.items)]
        self.i += 1
        return it


def ntiles512(n_tok):
    out = []
    t = 0
    while t < n_tok:
        w = min(512, n_tok - t)
        out.append((t, w))
        t += w
    return out


class Builder:
    def __init__(self, nc, dbg=None, layers=(0, 1), stop_after=None):
        self.nc = nc
        self.dbg = dbg or []
        self.layers = layers
        self.stop_after = stop_after

    def sb(self, st, name, shape, dt):
        self._uid = getattr(self, "_uid", 0) + 1
        return st.enter_context(self.nc.sbuf_tensor("%s_%d" % (name, self._uid), list(shape), dt))

    def rot_sb(self, st, name, shape, dt, n):
        return Rot([(self.sb(st, "%s%d" % (name, i), shape, dt), Res()) for i in range(n)])

    def end_phase(self):
        self.S.barrier()
        self.S.flush()

    def declare(self):
        nc = self.nc
        di = lambda n, s, dt=F32: nc.dram_tensor(n, list(s), dt, kind="ExternalInput").ap()
        self.xin = di("xin", [T, D])
        self.cvec = di("cvec", [2, D])
        self.ada_w = di("ada_w", [DEPTH, D, 6 * D])
        self.ada_b = di("ada_b", [DEPTH, 6 * D])
        self.norm1_g = di("norm1_g", [DEPTH, D])
        self.norm2_g = di("norm2_g", [DEPTH, D])
        self.w_in = di("w_in", [DEPTH, D, INC])
        self.conv_w = di("conv_w", [DEPTH, 3, 512])
        self.natab = di("natab", [DEPTH, 2, 128, 8, NJ * 64])
        self.qkgain = di("qkgain", [DEPTH, 640])
        self.ropec = di("ropec", [TL, 640])
        self.ropes = di("ropes", [TL, 640])
        self.w_br = di("w_br", [DEPTH, 3, 512, D])
        self.w_out = di("w_out", [DEPTH, D, D])
        self.router_w = di("router_w", [DEPTH, D, NE])
        self.router_b = di("router_b", [DEPTH, NE])
        self.w_gu = di("w_gu", [DEPTH, NE, D, 2 * D])
        self.b_gu = di("b_gu", [DEPTH, NE, 2 * D])
        self.w_down = di("w_down", [DEPTH, NE, D, D])
        self.b_down = di("b_down", [DEPTH, NE, D])
        self.final_g = di("final_g", [1, D])
        self.out = nc.dram_tensor("out", [TL, D], F32, kind="ExternalOutput").ap()

        def scr(n, s, dt):
            kind = "ExternalOutput" if n in self.dbg else "Internal"
            return nc.dram_tensor(n, list(s), dt, kind=kind).ap()
        self.X = scr("X", [T, D], F32)
        self.MOD = scr("MOD", [DEPTH, 2, 6 * D], F32)
        self.FT = scr("FT", [INC, T], BF16)
        self.TM = scr("TM", [T, 1280], BF16)
        self.YT = scr("YT", [1536, T], BF16)
        self.XE = scr("XE", [NE * CAP, D], BF16)
        self.YE = scr("YE", [NE * CAP, D], F32)
        self.rX = [Res() for _ in range(NT)]
        self.rMOD = Res()
        self.rFT = Res()
        self.rTM = Res()
        self.rYT = Res()
        self.rXE = Res()
        self.rYE = Res()
        self.rOUT = Res()

    def build(self):
        nc = self.nc
        self.declare()
        with ExitStack() as gst:
            S = self.S = Sched(nc, gst)
            self.pA = Rot([(gst.enter_context(nc.psum_tensor("pA%d" % i, [128, 512], F32)), Res()) for i in range(4)])
            self.pB = Rot([(gst.enter_context(nc.psum_tensor("pB%d" % i, [128, 512], F32)), Res()) for i in range(2)])
            self.pT = Rot([(gst.enter_context(nc.psum_tensor("pT%d" % i, [128, 1024], BF16)), Res()) for i in range(2)])
            self.identf = self.sb(gst, "identf", [128, 128], F32)
            self.identb = self.sb(gst, "identb", [128, 128], BF16)
            self.r_id = Res()
            idf, idb = self.identf, self.identb
            S.op("pool", lambda e: e.memset(idf[:], 0.0), writes=[self.r_id])
            S.op("pool", lambda e: e.affine_select(out=idf[:], in_=idf[:], pattern=[[-1, 128]],
                                                   compare_op=ALU.not_equal, fill=1.0, base=0, channel_multiplier=1),
                 reads=[self.r_id], writes=[self.r_id])
            S.op("dve", lambda e: e.tensor_copy(out=idb[:], in_=idf[:]), reads=[self.r_id], writes=[self.r_id])
            self.DEST = self.sb(gst, "DEST", [128, NT, 4], I32)
            self.GATES = self.sb(gst, "GATES", [128, NT, 4], F32)
            self.rROUTE = [Res() for _ in range(NT)]
            self.CNTI = self.sb(gst, "CNTI", [128, NE], I32)
            self.end_phase()

            self.phase_mods()
            if self.stop_after == "mods":
                return self.finish()
            for l in self.layers:
                last = (l == DEPTH - 1)
                self.Xsrc = self.xin if l == 0 else self.X
                self.nt_act = 32 if last else NT
                self.phase_AB(l)
                if self.stop_after == "AB%d" % l:
                    return self.finish()
                self.phase_conv(l)
                self.phase_gqa(l, last)
                self.phase_na(l, last)
                if self.stop_after == "attn%d" % l:
                    return self.finish()
                self.phase_merge(l)
                if self.stop_after == "merge%d" % l:
                    return self.finish()
                self.phase_route(l)
                self.phase_experts(l)
                if self.stop_after == "exp%d" % l:
                    return self.finish()
                self.phase_combine(l, last)
                if self.stop_after == "comb%d" % l:
                    return self.finish()
            return self.finish()

    def finish(self):
        S = self.S
        S.barrier()
        S.flush()

    def phase_mods(self):
        nc, S = self.nc, self.S
        with ExitStack() as st:
            cs = self.sb(st, "cs", [128, 8, 2], F32)
            ca = self.sb(st, "ca", [128, 8, 2], F32)
            r_cs = Res()
            for j in range(2):
                S.dma("sp", lambda e, j=j: e.dma_start(out=cs[:, :, j], in_=self.cvec[j, :].rearrange("(p k) -> p k", k=8), allow_slow_non_contiguous=True),
                      writes=[r_cs])
            S.op("act", lambda e: e.activation(out=ca[:], in_=cs[:], func=AF.Silu), reads=[r_cs], writes=[r_cs])
            wrot = self.rot_sb(st, "adaw", [128, 8, 512], F32, 2)
            bias = self.sb(st, "adab", [2, 6 * D], F32)
            modsb = self.sb(st, "modsb", [2, 6 * D], F32)
            r_b = Res()
            r_m = Res()
            for l in range(DEPTH):
                S.dma("sp", lambda e, l=l: e.dma_start(out=bias[:], in_=self.ada_b[l:l + 1, :].to_broadcast([2, 6 * D])),
                      writes=[r_b])
                wv = self.ada_w[l].rearrange("(p k) f -> p k f", k=8)
                for fb in range(12):
                    wt, r_w = wrot.next()
                    S.dma("sp" if fb % 2 == 0 else "act",
                          lambda e, wt=wt, fb=fb, wv=wv: e.dma_start(out=wt[:], in_=wv[:, :, fb * 512:(fb + 1) * 512]),
                          writes=[r_w])
                    pt, r_p = self.pA.next()
                    for k in range(8):
                        S.op("pe", lambda e, pt=pt, wt=wt, k=k: e.matmul(pt[0:2, :], lhsT=ca[:, k, :], rhs=wt[:, k, :],
                                                                        start=(k == 0), stop=(k == 7)),
                             reads=[r_cs, r_w], writes=[r_p], signal=(k == 7))
                    S.op("dve", lambda e, pt=pt, fb=fb: e.tensor_tensor(out=modsb[:, fb * 512:(fb + 1) * 512], in0=pt[0:2, :],
                                                                        in1=bias[:, fb * 512:(fb + 1) * 512], op=ALU.add),
                         reads=[r_p, r_b], writes=[r_m])
                S.dma("sp", lambda e, l=l: e.dma_start(out=self.MOD[l], in_=modsb[:]), reads=[r_m], writes=[self.rMOD])
            self.end_phase()

    def load_feat(self, queue, tile, res, src_row):
        self.S.dma(queue, lambda e: e.dma_start(out=tile[:], in_=src_row.rearrange("(k p) -> p k", p=128),
                                                allow_slow_non_contiguous=True),
                   reads=[self.rMOD], writes=[res])

    def load_bc(self, queue, tile_ap, res, src_row2d, n=128):
        F = src_row2d.shape[-1]
        self.S.dma(queue, lambda e: e.dma_start(out=tile_ap, in_=src_row2d.to_broadcast([n, F])),
                   reads=[self.rMOD], writes=[res])

    def rms_rstd(self, st_tiles, xt, r_x, width):
        S = self.S
        junk, ss, ms, rstd, r_s = st_tiles
        S.op("act", lambda e: e.activation(out=junk[:, 0:width], in_=xt[:, 0:width], func=AF.Square, accum_out=ss[:, 0:1]),
             reads=[r_x], writes=[r_s])
        S.op("dve", lambda e: e.tensor_scalar(out=ms[:], in0=ss[:], scalar1=1.0 / width, scalar2=EPS, op0=ALU.mult, op1=ALU.add),
             reads=[r_s], writes=[r_s])
        S.op("act", lambda e: e.activation(out=ms[:], in_=ms[:], func=AF.Sqrt), reads=[r_s], writes=[r_s])
        S.op("dve", lambda e: e.reciprocal(out=rstd[:], in_=ms[:]), reads=[r_s], writes=[r_s])
        return rstd, r_s

    def stat_tiles(self, st, name, n=2):
        items = []
        for i in range(n):
            items.append((self.sb(st, "%sj%d" % (name, i), [128, 1024], BF16), self.sb(st, "%ss%d" % (name, i), [128, 1], F32),
                          self.sb(st, "%sm%d" % (name, i), [128, 1], F32), self.sb(st, "%sr%d" % (name, i), [128, 1], F32), Res()))
        return Rot(items)

    def phase_AB(self, l):
        nc, S = self.nc, self.S
        with ExitStack() as st:
            hT = self.sb(st, "hT", [128, 8, T], BF16)
            r_h = [Res() for _ in range(NT)]
            G1 = self.sb(st, "G1", [128, 2, 8], F32)
            SH1 = self.sb(st, "SH1", [128, 2, 8], F32)
            ng = self.sb(st, "ng", [128, 8], F32)
            r_g = Res()
            self.load_feat("sp", ng, r_g, self.norm1_g[l, :])
            for j in range(2):
                S.dma("sp", lambda e, j=j: e.dma_start(out=SH1[:, j, :], in_=self.MOD[l, j, 0:D].rearrange("(k p) -> p k", p=128),
                                                       allow_slow_non_contiguous=True), reads=[self.rMOD], writes=[r_g])
                S.dma("sp", lambda e, j=j: e.dma_start(out=G1[:, j, :], in_=self.MOD[l, j, D:2 * D].rearrange("(k p) -> p k", p=128),
                                                       allow_slow_non_contiguous=True), reads=[self.rMOD], writes=[r_g])
            for j in range(2):
                S.op("dve", lambda e, j=j: e.scalar_tensor_tensor(out=G1[:, j, :], in0=G1[:, j, :], scalar=1.0, in1=ng[:],
                                                                  op0=ALU.add, op1=ALU.mult), reads=[r_g], writes=[r_g])
            xrot = self.rot_sb(st, "xt", [128, D], F32, 2)
            xsrot = self.rot_sb(st, "xs", [128, D], BF16, 2)
            strot = self.stat_tiles(st, "st")
            for i in range(NT):
                xt, r_x = xrot.next()
                S.dma("sp", lambda e, xt=xt, i=i: e.dma_start(out=xt[:], in_=self.Xsrc[i * 128:(i + 1) * 128, :]),
                      reads=[self.rX[i]], writes=[r_x])
                stt = strot.next()
                rstd, r_s = self.rms_rstd(stt, xt, r_x, D)
                xs, r_xs = xsrot.next()
                S.op("act", lambda e, xs=xs, xt=xt, rstd=rstd: e.activation(out=xs[:], in_=xt[:], func=AF.Copy, scale=rstd[:, 0:1]),
                     reads=[r_x, r_s], writes=[r_xs])
                pt, r_p = self.pT.next()
                for k in range(8):
                    S.op("pe", lambda e, pt=pt, xs=xs, k=k: e.transpose(out=pt[:, k * 128:(k + 1) * 128], in_=xs[:, k * 128:(k + 1) * 128],
                                                                        identity=self.identb[:]),
                         reads=[r_xs, self.r_id], writes=[r_p], signal=(k == 7))
                j = 0 if i < 32 else 1
                for k in range(8):
                    S.op("act", lambda e, pt=pt, k=k, i=i, j=j: e.activation(out=hT[:, k, i * 128:(i + 1) * 128], in_=pt[:, k * 128:(k + 1) * 128],
                                                                             func=AF.Identity, scale=G1[:, j, k:k + 1], bias=SH1[:, j, k:k + 1]),
                         reads=[r_p, r_g], writes=[r_h[i]])
            wv = self.w_in[l].rearrange("(k p) c -> p k c", p=128)
            wrot = self.rot_sb(st, "wblk", [128, 8, 512], BF16, 2)
            stg = self.rot_sb(st, "stg", [128, T], BF16, 2)
            tmst = self.rot_sb(st, "tmst", [128, 512], BF16, 3)
            nts = ntiles512(T)
            ev_i = 0
            for cb in range(14):
                c0 = cb * 512
                cw = min(512, INC - c0)
                wt, r_w = wrot.next()
                S.dma("pool", lambda e, wt=wt, c0=c0, cw=cw: e.dma_start(out=wt[:, :, 0:cw], in_=wv[:, :, c0:c0 + cw]), writes=[r_w])
                tm_lo, tm_hi = max(c0, 2560), min(c0 + cw, 3840)
                for cc in range(cw // 128):
                    col = c0 + cc * 128
                    if 2560 <= col < 3840:
                        continue
                    is_gate = col >= 3840
                    sg, r_sg = stg.next()
                    for (t0, tw) in nts:
                        pt, r_p = self.pA.next()
                        rh = r_h[t0 // 128:(t0 + tw) // 128]
                        for k in range(8):
                            S.op("pe", lambda e, pt=pt, wt=wt, k=k, cc=cc, t0=t0, tw=tw: e.matmul(
                                pt[:, 0:tw], lhsT=wt[:, k, cc * 128:(cc + 1) * 128], rhs=hT[:, k, t0:t0 + tw], start=(k == 0), stop=(k == 7)),
                                reads=[r_w] + rh, writes=[r_p], signal=(k == 7))
                        if is_gate:
                            S.op("act", lambda e, pt=pt, sg=sg, t0=t0, tw=tw: e.activation(out=sg[:, t0:t0 + tw], in_=pt[:, 0:tw], func=AF.Sigmoid),
                                 reads=[r_p], writes=[r_sg])
                        elif ev_i % 2 == 0:
                            S.op("act", lambda e, pt=pt, sg=sg, t0=t0, tw=tw: e.activation(out=sg[:, t0:t0 + tw], in_=pt[:, 0:tw], func=AF.Copy),
                                 reads=[r_p], writes=[r_sg])
                        else:
                            S.op("dve", lambda e, pt=pt, sg=sg, t0=t0, tw=tw: e.tensor_copy(out=sg[:, t0:t0 + tw], in_=pt[:, 0:tw]),
                                 reads=[r_p], writes=[r_sg])
                        ev_i += 1
                    S.dma("sp", lambda e, sg=sg, col=col: e.dma_start(out=self.FT[col:col + 128, :], in_=sg[:]), reads=[r_sg], writes=[self.rFT])
                if tm_lo < tm_hi:
                    w0, wn = tm_lo - c0, tm_hi - tm_lo
                    for i in range(NT):
                        pt, r_p = self.pA.next()
                        for k in range(8):
                            S.op("pe", lambda e, pt=pt, wt=wt, k=k, i=i, w0=w0, wn=wn: e.matmul(
                                pt[:, 0:wn], lhsT=hT[:, k, i * 128:(i + 1) * 128], rhs=wt[:, k, w0:w0 + wn], start=(k == 0), stop=(k == 7)),
                                reads=[r_w, r_h[i]], writes=[r_p], signal=(k == 7))
                        ts, r_ts = tmst.next()
                        if i % 2 == 0:
                            S.op("act", lambda e, pt=pt, ts=ts, wn=wn: e.activation(out=ts[:, 0:wn], in_=pt[:, 0:wn], func=AF.Copy),
                                 reads=[r_p], writes=[r_ts])
                        else:
                            S.op("dve", lambda e, pt=pt, ts=ts, wn=wn: e.tensor_copy(out=ts[:, 0:wn], in_=pt[:, 0:wn]),
                                 reads=[r_p], writes=[r_ts])
                        S.dma("sp", lambda e, ts=ts, i=i, wn=wn, tm_lo=tm_lo: e.dma_start(
                            out=self.TM[i * 128:(i + 1) * 128, tm_lo - 2560:tm_lo - 2560 + wn], in_=ts[:, 0:wn]), reads=[r_ts], writes=[self.rTM])
            self.end_phase()

    def phase_conv(self, l):
        nc, S = self.nc, self.S
        with ExitStack() as st:
            cw = self.sb(st, "cw", [128, 4, 3], F32)
            r_cw = Res()
            for kk in range(3):
                S.dma("sp", lambda e, kk=kk: e.dma_start(out=cw[:, :, kk], in_=self.conv_w[l, kk, :].rearrange("(j p) -> p j", p=128),
                                                         allow_slow_non_contiguous=True), writes=[r_cw])
            inrot = self.rot_sb(st, "cin", [128, 3, T], BF16, 2)
            u = self.sb(st, "cu", [128, T], F32)
            acc = self.sb(st, "cacc", [128, T], F32)
            yrot = self.rot_sb(st, "cy", [128, T], BF16, 2)
            r_u, r_a = Res(), Res()
            for j in range(4):
                ci, r_ci = inrot.next()
                for b in range(3):
                    S.dma("sp" if b != 1 else "act", lambda e, ci=ci, b=b, j=j: e.dma_start(
                        out=ci[:, b, :], in_=self.FT[b * 512 + j * 128:b * 512 + (j + 1) * 128, :]), reads=[self.rFT], writes=[r_ci])
                S.op("pool", lambda e, ci=ci: e.tensor_tensor(out=u[:], in0=ci[:, 1, :], in1=ci[:, 2, :], op=ALU.mult),
                     reads=[r_ci], writes=[r_u])
                S.op("dve", lambda e, j=j: e.tensor_scalar(out=acc[:], in0=u[:], scalar1=cw[:, j, 1:2], scalar2=None, op0=ALU.mult),
                     reads=[r_u, r_cw], writes=[r_a])
                for (a, b) in ((0, TL), (TL, T)):
                    S.op("dve", lambda e, j=j, a=a, b=b: e.scalar_tensor_tensor(out=acc[:, a + 1:b], in0=u[:, a:b - 1], scalar=cw[:, j, 0:1],
                                                                              in1=acc[:, a + 1:b], op0=ALU.mult, op1=ALU.add),
                         reads=[r_u, r_cw, r_a], writes=[r_a])
                    S.op("dve", lambda e, j=j, a=a, b=b: e.scalar_tensor_tensor(out=acc[:, a:b - 1], in0=u[:, a + 1:b], scalar=cw[:, j, 2:3],
                                                                              in1=acc[:, a:b - 1], op0=ALU.mult, op1=ALU.add),
                         reads=[r_u, r_cw, r_a], writes=[r_a])
                y, r_y = yrot.next()
                S.op("pool", lambda e, y=y, ci=ci: e.tensor_tensor(out=y[:], in0=ci[:, 0, :], in1=acc[:], op=ALU.mult),
                     reads=[r_ci, r_a], writes=[r_y])
                S.dma("sp", lambda e, y=y, j=j: e.dma_start(out=self.YT[j * 128:(j + 1) * 128, :], in_=y[:]), reads=[r_y], writes=[self.rYT])
            self.end_phase()

    def attn_block(self, kt_ap_fn, q_ap, va_ap_fn, chunks, N, acc, r_acc, prot, reads, tab_fn=None, addrot=None, scale=0.125):
        S = self.S
        n = len(chunks)
        pend = []

        def qk(si):
            s = chunks[si]
            ps, r_ps = self.pA.next()
            kt = kt_ap_fn(s)
            S.op("pe", lambda e: e.matmul(ps[:, 0:N], lhsT=kt, rhs=q_ap, start=True, stop=True), reads=reads, writes=[r_ps])
            pe_t, r_pe = prot.next()
            tb = tab_fn(s) if tab_fn is not None else None
            if tb is not None:
                ad, r_ad = addrot.next()
                S.op("dve", lambda e: e.scalar_tensor_tensor(out=ad[:, 0:N], in0=ps[:, 0:N], scalar=scale, in1=tb, op0=ALU.mult, op1=ALU.add),
                     reads=[r_ps] + reads, writes=[r_ad])
                S.op("act", lambda e: e.activation(out=pe_t[:, 0:N], in_=ad[:, 0:N], func=AF.Exp), reads=[r_ad], writes=[r_pe])
            else:
                S.op("act", lambda e: e.activation(out=pe_t[:, 0:N], in_=ps[:, 0:N], func=AF.Exp, scale=scale), reads=[r_ps], writes=[r_pe])
            return (s, pe_t, r_pe)

        pend.append(qk(0))
        for si in range(n):
            if si + 1 < n:
                pend.append(qk(si + 1))
            s, pe_t, r_pe = pend.pop(0)
            va_ = va_ap_fn(s)
            S.op("pe", lambda e, va_=va_, pe_t=pe_t, si=si: e.matmul(acc[:, 0:N], lhsT=va_, rhs=pe_t[:, 0:N], start=(si == 0), stop=(si == n - 1)),
                 reads=[r_pe] + reads, writes=[r_acc], signal=(si == n - 1))

    def attn_finish(self, acc, r_acc, N, out_ap, r_out, recrot):
        S = self.S
        rec, r_rec = recrot.next()
        S.op("act", lambda e: e.activation(out=rec[0:64, 0:N], in_=acc[64:128, 0:N], func=AF.Copy), reads=[r_acc], writes=[r_rec])
        S.op("dve", lambda e: e.reciprocal(out=rec[0:64, 0:N], in_=rec[0:64, 0:N]), reads=[r_rec], writes=[r_rec])
        S.op("dve", lambda e: e.tensor_tensor(out=out_ap, in0=acc[0:64, 0:N], in1=rec[0:64, 0:N], op=ALU.mult),
             reads=[r_acc, r_rec], writes=[r_out])

    def phase_gqa(self, l, last):
        nc, S = self.nc, self.S
        with ExitStack() as st:
            QT = self.sb(st, "QT", [128, 8, T], BF16)
            KT2 = self.sb(st, "KT2", [128, 2, T], BF16)
            VA = self.sb(st, "VA", [128, NT, 2, 128], BF16)
            r_q = Res()
            r_vat = [Res() for _ in range(NT)]
            S.op("pool", lambda e: e.memset(VA[:, :, :, 64:128], 1.0), writes=r_vat)
            S.op("pool", lambda e: e.memset(QT[:], 0.0), writes=[r_q])
            for i in range(NT):
                S.dma("sp" if i % 2 == 0 else "act", lambda e, i=i: e.dma_start(
                    out=VA[:, i, :, 0:64], in_=self.TM[i * 128:(i + 1) * 128, 1152:1280].rearrange("p (g d) -> p g d", g=2)),
                    reads=[self.rTM], writes=[r_vat[i]])
            gain = self.sb(st, "gain", [128, 640], F32)
            r_gn = Res()
            S.dma("sp", lambda e: e.dma_start(out=gain[:], in_=self.qkgain[l:l + 1, :].to_broadcast([128, 640])), writes=[r_gn])
            with ExitStack() as st2:
                inrot = self.rot_sb(st2, "gin", [128, 640], BF16, 2)
                sqrot = self.rot_sb(st2, "gsq", [128, 640], F32, 2)
                xnrot = self.rot_sb(st2, "gxn", [128, 640], F32, 2)
                swrot = self.rot_sb(st2, "gsw", [128, 640], F32, 2)
                cosrot = self.rot_sb(st2, "gcos", [128, 640], F32, 2)
                sinrot = self.rot_sb(st2, "gsin", [128, 640], F32, 2)
                qbrot = self.rot_sb(st2, "gqb", [128, 640], BF16, 2)
                ssrot = Rot([(self.sb(st2, "gss%d" % i, [128, 10], F32), Res()) for i in range(2)])
                for i in range(NT):
                    xi, r_xi = inrot.next()
                    S.dma("sp", lambda e, xi=xi, i=i: e.dma_start(out=xi[:], in_=self.TM[i * 128:(i + 1) * 128, 512:1152]), reads=[self.rTM], writes=[r_xi])
                    sq, r_sq = sqrot.next()
                    S.op("pool", lambda e, sq=sq, xi=xi: e.tensor_tensor(out=sq[:], in0=xi[:], in1=xi[:], op=ALU.mult), reads=[r_xi], writes=[r_sq])
                    ss, r_ss = ssrot.next()
                    S.op("dve", lambda e, ss=ss, sq=sq: e.tensor_reduce(out=ss[:], in_=sq[:].rearrange("p (h d) -> p h d", d=64), axis=AX.X, op=ALU.add),
                         reads=[r_sq], writes=[r_ss])
                    S.op("dve", lambda e, ss=ss: e.tensor_scalar(out=ss[:], in0=ss[:], scalar1=1.0 / 64, scalar2=EPS, op0=ALU.mult, op1=ALU.add),
                         reads=[r_ss], writes=[r_ss])
                    S.op("act", lambda e, ss=ss: e.activation(out=ss[:], in_=ss[:], func=AF.Sqrt), reads=[r_ss], writes=[r_ss])
                    S.op("dve", lambda e, ss=ss: e.reciprocal(out=ss[:], in_=ss[:]), reads=[r_ss], writes=[r_ss])
                    xn, r_xn = xnrot.next()
                    r_xh = [Res() for _ in range(10)]
                    for h in range(10):
                        S.op("dve", lambda e, xn=xn, xi=xi, ss=ss, h=h: e.tensor_scalar(
                            out=xn[:, h * 64:(h + 1) * 64], in0=xi[:, h * 64:(h + 1) * 64], scalar1=ss[:, h:h + 1], scalar2=None, op0=ALU.mult),
                            reads=[r_xi, r_ss, r_xn], writes=[r_xh[h]])
                    S.op("dve", lambda e, xn=xn: e.tensor_tensor(out=xn[:], in0=xn[:], in1=gain[:], op=ALU.mult), reads=r_xh + [r_gn], writes=[r_xn])
                    qb, r_qb = qbrot.next()
                    if i < 32:
                        co, r_co = cosrot.next()
                        si_, r_si = sinrot.next()
                        S.dma("act", lambda e, co=co, i=i: e.dma_start(out=co[:], in_=self.ropec[i * 128:(i + 1) * 128, :]), writes=[r_co])
                        S.dma("act", lambda e, si_=si_, i=i: e.dma_start(out=si_[:], in_=self.ropes[i * 128:(i + 1) * 128, :]), writes=[r_si])
                        sw, r_sw = swrot.next()
                        xv = xn[:].rearrange("p (g two d) -> p g two d", two=2, d=16)
                        swv = sw[:].rearrange("p (g two d) -> p g two d", two=2, d=16)
                        S.op("pool", lambda e, swv=swv, xv=xv: e.tensor_copy(out=swv[:, :, 0, :], in_=xv[:, :, 1, :]), reads=[r_xn], writes=[r_sw])
                        S.op("pool", lambda e, swv=swv, xv=xv: e.tensor_copy(out=swv[:, :, 1, :], in_=xv[:, :, 0, :]), reads=[r_xn, r_sw], writes=[r_sw])
                        S.op("pool", lambda e, sw=sw, si_=si_: e.tensor_tensor(out=sw[:], in0=sw[:], in1=si_[:], op=ALU.mult), reads=[r_sw, r_si], writes=[r_sw])
                        S.op("dve", lambda e, xn=xn, co=co: e.tensor_tensor(out=xn[:], in0=xn[:], in1=co[:], op=ALU.mult), reads=[r_xn, r_co], writes=[r_xn])
                        S.op("dve", lambda e, qb=qb, xn=xn, sw=sw: e.tensor_tensor(out=qb[:], in0=xn[:], in1=sw[:], op=ALU.add), reads=[r_xn, r_sw], writes=[r_qb])
                    else:
                        S.op("dve", lambda e, qb=qb, xn=xn: e.tensor_copy(out=qb[:], in_=xn[:]), reads=[r_xn], writes=[r_qb])
                    pt, r_p = self.pT.next()
                    for c in range(5):
                        S.op("pe", lambda e, pt=pt, qb=qb, c=c: e.transpose(out=pt[:, c * 128:(c + 1) * 128], in_=qb[:, c * 128:(c + 1) * 128], identity=self.identb[:]),
                             reads=[r_qb, self.r_id], writes=[r_p], signal=(c == 4))
                    tsl = slice(i * 128, (i + 1) * 128)
                    QTv = QT[:].rearrange("p (c two) t -> p c two t", two=2)
                    S.op("act", lambda e, pt=pt, tsl=tsl, QTv=QTv: e.activation(out=QTv[0:64, :, 0, tsl], in_=pt[0:64, 0:512].rearrange("p (c t) -> p c t", c=4), func=AF.Copy),
                         reads=[r_p, r_q], writes=[r_q])
                    S.op("act", lambda e, pt=pt, tsl=tsl, QTv=QTv: e.activation(out=QTv[64:128, :, 1, tsl], in_=pt[64:128, 0:512].rearrange("p (c t) -> p c t", c=4), func=AF.Copy),
                         reads=[r_p, r_q], writes=[r_q])
                    S.op("dve", lambda e, pt=pt, tsl=tsl: e.tensor_copy(out=KT2[0:64, 0, tsl], in_=pt[0:64, 512:640]), reads=[r_p, r_q], writes=[r_q])
                    S.op("dve", lambda e, pt=pt, tsl=tsl: e.tensor_copy(out=KT2[64:128, 1, tsl], in_=pt[64:128, 512:640]), reads=[r_p, r_q], writes=[r_q])
                    S.op("act", lambda e, pt=pt, tsl=tsl: e.activation(out=KT2[64:128, 0, tsl], in_=pt[0:64, 512:640], func=AF.Copy), reads=[r_p, r_q], writes=[r_q])
                    S.op("act", lambda e, pt=pt, tsl=tsl: e.activation(out=KT2[0:64, 1, tsl], in_=pt[64:128, 512:640], func=AF.Copy), reads=[r_p, r_q], writes=[r_q])
                self.S.barrier()
            prot = self.rot_sb(st, "gpe", [128, 512], BF16, 3)
            recrot = self.rot_sb(st, "grec", [64, 512], F32, 2)
            ysrot = self.rot_sb(st, "gys", [64, T], BF16, 2)
            for h in range(8):
                g, c, hh = h // 4, h // 2, h % 2
                ps_ = slice(hh * 64, (hh + 1) * 64)
                ys, r_ys = ysrot.next()
                blocks = [(n * 512, 512, list(range(NT))) for n in range(8)]
                if not last:
                    blocks.append((TL, TC, [32, 33]))
                for (q0, N, chunks) in blocks:
                    acc, r_acc = self.pB.next()
                    self.attn_block(lambda s: KT2[:, g, s * 128:(s + 1) * 128], QT[:, h, q0:q0 + N],
                                    lambda s: VA[:, s, g, :], chunks, N, acc, r_acc, prot, [r_q] + r_vat)
                    self.attn_finish(acc, r_acc, N, ys[:, q0:q0 + N], r_ys, recrot)
                ncol = T if not last else TL
                S.dma("sp", lambda e, ys=ys, h=h, ncol=ncol: e.dma_start(out=self.YT[1024 + h * 64:1024 + (h + 1) * 64, 0:ncol], in_=ys[:, 0:ncol]),
                      reads=[r_ys], writes=[self.rYT])
            self.end_phase()

    def phase_na(self, l, last):
        nc, S = self.nc, self.S
        qblocks = [(0, 4, 1, [0, 2, 4, 6])]
        for r0 in range(4, 60, 8):
            qblocks.append((r0, 8, 0, list(range(r0 - 4, r0 + 12, 2))))
        qblocks.append((60, 1, 0, [56, 58, 60, 62]))
        qblocks.append((61, 3, 1, [56, 58, 60, 62]))
        with ExitStack() as st:
            qkrot = self.rot_sb(st, "nqk", [128, 2, T], BF16, 2)
            qzrot = self.rot_sb(st, "nqz", [128, 2, T], BF16, 2)
            varot = self.rot_sb(st, "nva", [128, NT, 2, 128], BF16, 2)
            tabrot = self.rot_sb(st, "ntab", [128, 2, 2, NJ * 64], BF16, 2)
            prot = self.rot_sb(st, "npe", [128, 512], BF16, 3)
            addrot = self.rot_sb(st, "nad", [128, 512], F32, 2)
            recrot = self.rot_sb(st, "nrec", [64, 512], F32, 2)
            ysrot = self.rot_sb(st, "nys", [64, T], BF16, 2)
            for c in range(4):
                qk, r_qk = qkrot.next()
                va, r_va = varot.next()
                tab, r_tab = tabrot.next()
                S.dma("sp", lambda e, qk=qk, c=c: e.dma_start(out=qk[:, 0, :], in_=self.FT[1536 + c * 128:1536 + (c + 1) * 128, :]), reads=[self.rFT], writes=[r_qk])
                S.dma("act", lambda e, qk=qk, c=c: e.dma_start(out=qk[:, 1, :], in_=self.FT[2048 + c * 128:2048 + (c + 1) * 128, :]), reads=[self.rFT], writes=[r_qk])
                S.op("pool", lambda e, va=va: e.memset(va[:, :, :, 64:128], 1.0), writes=[r_va])
                qz, r_qz = qzrot.next()
                S.op("pool", lambda e, qz=qz: e.memset(qz[:], 0.0), writes=[r_qz])
                S.op("dve", lambda e, qz=qz, qk=qk: e.tensor_copy(out=qz[0:64, 0, :], in_=qk[0:64, 0, :]), reads=[r_qk, r_qz], writes=[r_qz])
                S.op("pool", lambda e, qz=qz, qk=qk: e.tensor_copy(out=qz[64:128, 1, :], in_=qk[64:128, 0, :]), reads=[r_qk, r_qz], writes=[r_qz])
                r_vt = [Res() for _ in range(NT)]
                for i in range(NT):
                    S.dma("sp" if i % 2 == 0 else "act", lambda e, va=va, i=i, c=c: e.dma_start(
                        out=va[:, i, :, 0:64], in_=self.TM[i * 128:(i + 1) * 128, c * 128:(c + 1) * 128].rearrange("p (g d) -> p g d", g=2)),
                        reads=[self.rTM, r_va], writes=[r_vt[i]])
                for v in range(2):
                    S.dma("pool", lambda e, tab=tab, v=v, c=c: e.dma_start(out=tab[:, v, :, :], in_=self.natab[l, v, :, 2 * c:2 * c + 2, :]), writes=[r_tab])
                for hh in range(2):
                    h = 2 * c + hh
                    ps_ = slice(hh * 64, (hh + 1) * 64)
                    ys, r_ys = ysrot.next()
                    for (r0, R, v, krows) in qblocks:
                        N = 64 * R
                        q0 = r0 * 64
                        chunks = [kr // 2 for kr in krows] + [32, 33]

                        def tab_fn(s, r0=r0, v=v, N=N, tab=tab, hh=hh):
                            if s >= 32:
                                return None
                            j0 = r0 - 2 * s + JOFF
                            return tab[:, v, hh, j0 * 64:j0 * 64 + N]
                        acc, r_acc = self.pB.next()
                        self.attn_block(lambda s, qk=qk: qk[:, 1, s * 128:(s + 1) * 128], qz[:, hh, q0:q0 + N],
                                        lambda s, va=va, hh=hh: va[:, s, hh, :], chunks, N, acc, r_acc, prot, [r_qk, r_qz, r_va, r_tab] + r_vt,
                                        tab_fn=tab_fn, addrot=addrot)
                        self.attn_finish(acc, r_acc, N, ys[:, q0:q0 + N], r_ys, recrot)
                    if not last:
                        acc, r_acc = self.pB.next()
                        self.attn_block(lambda s, qk=qk: qk[:, 1, s * 128:(s + 1) * 128], qz[:, hh, TL:T],
                                        lambda s, va=va, hh=hh: va[:, s, hh, :], [32, 33], TC, acc, r_acc, prot, [r_qk, r_qz, r_va] + r_vt)
                        self.attn_finish(acc, r_acc, TC, ys[:, TL:T], r_ys, recrot)
                    ncol = T if not last else TL
                    S.dma("sp", lambda e, ys=ys, h=h, ncol=ncol: e.dma_start(out=self.YT[512 + h * 64:512 + (h + 1) * 64, 0:ncol], in_=ys[:, 0:ncol]),
                          reads=[r_ys], writes=[self.rYT])
            self.end_phase()

    def phase_merge(self, l):
        nc, S = self.nc, self.S
        ntok = self.nt_act * 128
        with ExitStack() as st:
            WBR = self.sb(st, "WBR", [128, 12, D], BF16)
            WO = self.sb(st, "WO", [128, 8, D], BF16)
            r_w = Res()
            for b in range(3):
                S.dma("pool", lambda e, b=b: e.dma_start(out=WBR[:, b * 4:(b + 1) * 4, :], in_=self.w_br[l, b].rearrange("(k p) f -> p k f", p=128)), writes=[r_w])
            S.dma("pool", lambda e: e.dma_start(out=WO[:], in_=self.w_out[l].rearrange("(k p) f -> p k f", p=128)), writes=[r_w])
            GT = self.sb(st, "GT1", [128, 2, D], F32)
            r_gt = Res()
            for j in range(2):
                self.load_bc("sp", GT[:, j, :], r_gt, self.MOD[l, j:j + 1, 2 * D:3 * D])
            ytrot = self.rot_sb(st, "mYT", [128, 12, 512], BF16, 2)
            sgrot = self.rot_sb(st, "mSG", [128, 24, 512], BF16, 2)
            mrot = self.rot_sb(st, "mT", [128, 8, 512], BF16, 2)
            m0rot = self.rot_sb(st, "m0", [128, 512], F32, 2)
            m1rot = self.rot_sb(st, "m1", [128, 512], F32, 2)
            m2rot = self.rot_sb(st, "m2", [128, 512], F32, 2)
            xrot = self.rot_sb(st, "mx", [128, D], F32, 2)
            xorot = self.rot_sb(st, "mxo", [128, D], F32, 2)
            ytv = self.YT.rearrange("(j p) t -> p j t", p=128)
            sgv = self.FT[3840:INC, :].rearrange("(j p) t -> p j t", p=128)
            for (t0, tw) in ntiles512(ntok):
                yt, r_yt = ytrot.next()
                sg, r_sg = sgrot.next()
                S.dma("sp", lambda e, yt=yt, t0=t0, tw=tw: e.dma_start(out=yt[:, :, 0:tw], in_=ytv[:, :, t0:t0 + tw]), reads=[self.rYT], writes=[r_yt])
                S.dma("act", lambda e, sg=sg, t0=t0, tw=tw: e.dma_start(out=sg[:, :, 0:tw], in_=sgv[:, :, t0:t0 + tw]), reads=[self.rFT], writes=[r_sg])
                mT, r_m = mrot.next()
                for f in range(8):
                    pb = []
                    for b in range(3):
                        pt, r_p = self.pA.next()
                        for kc in range(4):
                            S.op("pe", lambda e, pt=pt, b=b, kc=kc, f=f, yt=yt, tw=tw: e.matmul(
                                pt[:, 0:tw], lhsT=WBR[:, b * 4 + kc, f * 128:(f + 1) * 128], rhs=yt[:, b * 4 + kc, 0:tw], start=(kc == 0), stop=(kc == 3)),
                                reads=[r_w, r_yt], writes=[r_p], signal=(kc == 3))
                        pb.append((pt, r_p))
                    a0, r_a0 = m0rot.next()
                    a1, r_a1 = m1rot.next()
                    a2, r_a2 = m2rot.next()
                    for b, (a, r_a) in enumerate(((a0, r_a0), (a1, r_a1), (a2, r_a2))):
                        pt, r_p = pb[b]
                        S.op("dve", lambda e, a=a, pt=pt, sg=sg, b=b, f=f, tw=tw: e.tensor_tensor(out=a[:, 0:tw], in0=pt[:, 0:tw], in1=sg[:, b * 8 + f, 0:tw], op=ALU.mult),
                             reads=[r_p, r_sg], writes=[r_a])
                    S.op("pool", lambda e, a0=a0, a1=a1, tw=tw: e.tensor_tensor(out=a0[:, 0:tw], in0=a0[:, 0:tw], in1=a1[:, 0:tw], op=ALU.add),
                         reads=[r_a0, r_a1], writes=[r_a0])
                    S.op("pool", lambda e, a0=a0, a2=a2, mT=mT, f=f, tw=tw: e.tensor_tensor(out=mT[:, f, 0:tw], in0=a0[:, 0:tw], in1=a2[:, 0:tw], op=ALU.add),
                         reads=[r_a0, r_a2], writes=[r_m])
                for ts in range(tw // 128):
                    i = t0 // 128 + ts
                    j = 0 if i < 32 else 1
                    xt, r_x = xrot.next()
                    S.dma("sp", lambda e, xt=xt, i=i: e.dma_start(out=xt[:], in_=self.Xsrc[i * 128:(i + 1) * 128, :]), reads=[self.rX[i]], writes=[r_x])
                    xo, r_xo = xorot.next()
                    for half in range(2):
                        pt, r_p = self.pA.next()
                        for f in range(8):
                            S.op("pe", lambda e, pt=pt, f=f, mT=mT, ts=ts, half=half: e.matmul(
                                pt[:, :], lhsT=mT[:, f, ts * 128:(ts + 1) * 128], rhs=WO[:, f, half * 512:(half + 1) * 512], start=(f == 0), stop=(f == 7)),
                                reads=[r_w, r_m], writes=[r_p], signal=(f == 7))
                        hs = slice(half * 512, (half + 1) * 512)
                        S.op("dve", lambda e, pt=pt, xo=xo, hs=hs, j=j: e.tensor_tensor(out=xo[:, hs], in0=pt[:, :], in1=GT[:, j, hs], op=ALU.mult),
                             reads=[r_p, r_gt], writes=[r_xo])
                    S.op("pool", lambda e, xo=xo, xt=xt: e.tensor_tensor(out=xo[:], in0=xo[:], in1=xt[:], op=ALU.add), reads=[r_xo, r_x], writes=[r_xo])
                    S.dma("sp", lambda e, xo=xo, i=i: e.dma_start(out=self.X[i * 128:(i + 1) * 128, :], in_=xo[:]), reads=[r_xo], writes=[self.rX[i]])
            self.end_phase()
            self.Xsrc = self.X

    def phase_route(self, l):
        nc, S = self.nc, self.S
        with ExitStack() as st:
            G2 = self.sb(st, "G2", [128, 2, D], F32)
            SH2 = self.sb(st, "SH2", [128, 2, D], F32)
            NG = self.sb(st, "NG2", [128, D], F32)
            r_g = Res()
            self.load_bc("sp", NG[:], r_g, self.norm2_g[l:l + 1, :])
            for j in range(2):
                self.load_bc("sp", SH2[:, j, :], r_g, self.MOD[l, j:j + 1, 3 * D:4 * D])
                self.load_bc("act", G2[:, j, :], r_g, self.MOD[l, j:j + 1, 4 * D:5 * D])
            for j in range(2):
                S.op("dve", lambda e, j=j: e.scalar_tensor_tensor(out=G2[:, j, :], in0=G2[:, j, :], scalar=1.0, in1=NG[:], op0=ALU.add, op1=ALU.mult),
                     reads=[r_g], writes=[r_g])
            RW = self.sb(st, "RW", [128, 8, NE], F32)
            RB = self.sb(st, "RB", [128, NE], F32)
            S.dma("sp", lambda e: e.dma_start(out=RW[:], in_=self.router_w[l].rearrange("(k p) e -> p k e", p=128)), writes=[r_g])
            self.load_bc("sp", RB[:], r_g, self.router_b[l:l + 1, :])
            UTf = self.sb(st, "UTf", [128, 128], F32)
            UT = self.sb(st, "UT", [128, 128], BF16)
            ONES = self.sb(st, "ONES", [128, 128], BF16)
            EB = self.sb(st, "EB", [128, NE], F32)
            CNT = self.sb(st, "CNT", [128, NE], F32)
            r_c = Res()
            r_cnt = Res()
            S.op("pool", lambda e: e.memset(UTf[:], 1.0), writes=[r_c])
            S.op("pool", lambda e: e.affine_select(out=UTf[:], in_=UTf[:], pattern=[[1, 128]], compare_op=ALU.is_gt, fill=0.0, base=0, channel_multiplier=-1),
                 reads=[r_c], writes=[r_c])
            S.op("dve", lambda e: e.tensor_copy(out=UT[:], in_=UTf[:]), reads=[r_c], writes=[r_c])
            S.op("pool", lambda e: e.memset(ONES[:], 1.0), writes=[r_c])
            S.op("pool", lambda e: e.iota(EB[:], pattern=[[CAP, NE]], base=0, channel_multiplier=0, allow_small_or_imprecise_dtypes=True), writes=[r_c])
            S.op("pool", lambda e: e.memset(CNT[:], 0.0), writes=[r_cnt])
            xrot = self.rot_sb(st, "rx", [128, D], F32, 2)
            hrot = self.rot_sb(st, "rh", [128, D], F32, 2)
            hbrot = self.rot_sb(st, "rhb", [128, D], BF16, 6)
            htrot = self.rot_sb(st, "rht", [128, 8, 128], F32, 2)
            strot = self.stat_tiles(st, "rst")
            smrot = Rot([({n: self.sb(st, "rs%s%d" % (n, i), [128, w], dt) for n, w, dt in (
                ("lg", NE, F32), ("t8", 8, F32), ("nm", 1, F32), ("e4", 4, F32), ("sm", 1, F32), ("mk", NE, BF16),
                ("pos", NE, F32), ("oh", NE, F32), ("df", 4, F32))}, Res()) for i in range(2)])
            for i in range(self.nt_act):
                j = 0 if i < 32 else 1
                xt, r_x = xrot.next()
                S.dma("sp", lambda e, xt=xt, i=i: e.dma_start(out=xt[:], in_=self.X[i * 128:(i + 1) * 128, :]), reads=[self.rX[i]], writes=[r_x])
                stt = strot.next()
                rstd, r_s = self.rms_rstd(stt, xt, r_x, D)
                h2, r_h = hrot.next()
                S.op("dve", lambda e, h2=h2, xt=xt, rstd=rstd, j=j: e.scalar_tensor_tensor(out=h2[:], in0=xt[:], scalar=rstd[:, 0:1], in1=G2[:, j, :], op0=ALU.mult, op1=ALU.mult),
                     reads=[r_x, r_s, r_g], writes=[r_h])
                S.op("pool", lambda e, h2=h2, j=j: e.tensor_tensor(out=h2[:], in0=h2[:], in1=SH2[:, j, :], op=ALU.add), reads=[r_h, r_g], writes=[r_h])
                hb, r_hb = hbrot.next()
                S.op("act", lambda e, hb=hb, h2=h2: e.activation(out=hb[:], in_=h2[:], func=AF.Copy), reads=[r_h], writes=[r_hb])
                ht, r_ht = htrot.next()
                for half in range(2):
                    pt, r_p = self.pA.next()
                    for kk in range(4):
                        k = half * 4 + kk
                        S.op("pe", lambda e, pt=pt, h2=h2, k=k, kk=kk: e.transpose(out=pt[:, kk * 128:(kk + 1) * 128], in_=h2[:, k * 128:(k + 1) * 128], identity=self.identf[:]),
                             reads=[r_h, self.r_id], writes=[r_p], signal=(kk == 3))
                    if half == 0:
                        S.op("act", lambda e, pt=pt, ht=ht: e.activation(out=ht[:, 0:4, :], in_=pt[:, :].rearrange("p (k t) -> p k t", k=4), func=AF.Copy), reads=[r_p], writes=[r_ht])
                    else:
                        S.op("dve", lambda e, pt=pt, ht=ht: e.tensor_copy(out=ht[:, 4:8, :], in_=pt[:, :].rearrange("p (k t) -> p k t", k=4)), reads=[r_p, r_ht], writes=[r_ht])
                pl, r_pl = self.pB.next()
                for k in range(8):
                    S.op("pe", lambda e, pl=pl, ht=ht, k=k: e.matmul(pl[:, 0:NE], lhsT=ht[:, k, :], rhs=RW[:, k, :], start=(k == 0), stop=(k == 7)),
                         reads=[r_ht, r_g], writes=[r_pl], signal=(k == 7))
                sm, r_sm = smrot.next()
                S.op("dve", lambda e, sm=sm, pl=pl: e.tensor_tensor(out=sm["lg"][:], in0=pl[:, 0:NE], in1=RB[:], op=ALU.add), reads=[r_pl, r_g], writes=[r_sm])
                S.op("dve", lambda e, sm=sm: e.max(out=sm["t8"][:], in_=sm["lg"][:]), reads=[r_sm], writes=[r_sm])
                S.op("dve", lambda e, sm=sm: e.tensor_scalar(out=sm["nm"][:], in0=sm["t8"][:, 0:1], scalar1=-1.0, scalar2=None, op0=ALU.mult), reads=[r_sm], writes=[r_sm])
                S.op("act", lambda e, sm=sm: e.activation(out=sm["e4"][:], in_=sm["t8"][:, 0:4], func=AF.Exp, bias=sm["nm"][:, 0:1], accum_out=sm["sm"][:, 0:1]), reads=[r_sm], writes=[r_sm])
                S.op("dve", lambda e, sm=sm: e.reciprocal(out=sm["sm"][:], in_=sm["sm"][:]), reads=[r_sm], writes=[r_sm])
                S.op("dve", lambda e, sm=sm, i=i: e.tensor_scalar(out=self.GATES[:, i, :], in0=sm["e4"][:], scalar1=sm["sm"][:, 0:1], scalar2=None, op0=ALU.mult),
                     reads=[r_sm], writes=[self.rROUTE[i]])
                S.op("dve", lambda e, sm=sm: e.tensor_scalar(out=sm["mk"][:], in0=sm["lg"][:], scalar1=sm["t8"][:, 3:4], scalar2=None, op0=ALU.is_ge), reads=[r_sm], writes=[r_sm])
                pp, r_pp = self.pB.next()
                S.op("pe", lambda e, pp=pp, sm=sm: e.matmul(pp[:, 0:NE], lhsT=UT[:], rhs=sm["mk"][:], start=True, stop=True), reads=[r_sm, r_c], writes=[r_pp], signal=False)
                S.op("pe", lambda e, pp=pp, sm=sm: e.matmul(pp[:, 64:64 + NE], lhsT=ONES[:], rhs=sm["mk"][:], start=True, stop=True), reads=[r_sm, r_c], writes=[r_pp])
                S.op("dve", lambda e, sm=sm, pp=pp: e.tensor_tensor(out=sm["pos"][:], in0=pp[:, 0:NE], in1=CNT[:], op=ALU.add), reads=[r_pp, r_cnt, r_sm], writes=[r_sm])
                S.op("dve", lambda e, pp=pp: e.tensor_tensor(out=CNT[:], in0=pp[:, 64:64 + NE], in1=CNT[:], op=ALU.add), reads=[r_pp, r_cnt], writes=[r_cnt])
                S.op("dve", lambda e, sm=sm: e.scalar_tensor_tensor(out=sm["pos"][:], in0=sm["pos"][:], scalar=float(CAP - 1), in1=EB[:], op0=ALU.min, op1=ALU.add),
                     reads=[r_sm, r_c], writes=[r_sm])
                for k in range(4):
                    S.op("dve", lambda e, sm=sm, k=k: e.scalar_tensor_tensor(out=sm["oh"][:], in0=sm["lg"][:], scalar=sm["t8"][:, k:k + 1], in1=sm["pos"][:], op0=ALU.is_equal, op1=ALU.mult),
                         reads=[r_sm], writes=[r_sm])
                    S.op("dve", lambda e, sm=sm, k=k: e.reduce_sum(out=sm["df"][:, k:k + 1], in_=sm["oh"][:], axis=AX.X), reads=[r_sm], writes=[r_sm])
                S.op("dve", lambda e, sm=sm, i=i: e.tensor_copy(out=self.DEST[:, i, :], in_=sm["df"][:]), reads=[r_sm, self.rROUTE[i]], writes=[self.rROUTE[i]])
                for k in range(4):
                    S.dma("pool", lambda e, hb=hb, i=i, k=k: e.indirect_dma_start(
                        out=self.XE[:, :], out_offset=bass.IndirectOffsetOnAxis(ap=self.DEST[:, i, k:k + 1], axis=0), in_=hb[:], in_offset=None),
                        reads=[r_hb, self.rROUTE[i]], writes=[self.rXE])
            S.op("dve", lambda e: e.tensor_copy(out=self.CNTI[:], in_=CNT[:]), reads=[r_cnt], writes=[r_cnt])
            self.end_phase()

    def phase_experts(self, l):
        nc, S = self.nc, self.S
        NTE = CAP // 512
        with ExitStack() as st:
            xerot = self.rot_sb(st, "xe", [128, 4, D], BF16, 2)
            xtrot = self.rot_sb(st, "xeT", [128, 8, 512], BF16, 2)
            wgrot = self.rot_sb(st, "wg", [128, 8, D], BF16, 2)
            wurot = self.rot_sb(st, "wu", [128, 8, D], BF16, 2)
            wdrot = self.rot_sb(st, "wd", [128, 8, D], BF16, 1)
            stgrot = self.rot_sb(st, "wstg", [128, 4, 512], F32, 3)
            bgrot = self.rot_sb(st, "bgu", [128, 8, 2], F32, 2)
            bdrot = self.rot_sb(st, "bd", [128, D], F32, 2)
            atrot = self.rot_sb(st, "aT", [128, 8, 512], BF16, 2)
            gsrot = self.rot_sb(st, "gs", [128, 512], F32, 2)
            sgrot = self.rot_sb(st, "sg", [128, 512], F32, 2)
            usrot = self.rot_sb(st, "us", [128, 512], F32, 2)
            yrot = self.rot_sb(st, "ye", [128, D], F32, 2)
            for e_ in range(NE):
                wg, r_wg = wgrot.next()
                wu, r_wu = wurot.next()
                wd, r_wd = wdrot.next()
                wguv = self.w_gu[l, e_].rearrange("(k p) c -> p k c", p=128)
                for kh in range(2):
                    for q in range(4):
                        sgt, r_st = stgrot.next()
                        S.dma("sp" if (kh * 4 + q) % 2 == 0 else "act", lambda e, sgt=sgt, q=q, kh=kh, wguv=wguv: e.dma_start(
                            out=sgt[:], in_=wguv[:, kh * 4:(kh + 1) * 4, q * 512:(q + 1) * 512]), writes=[r_st])
                        sv = sgt[:].rearrange("p k (c two) -> p k c two", two=2)
                        S.op("pool", lambda e, wg=wg, sv=sv, q=q, kh=kh: e.tensor_copy(out=wg[:, kh * 4:(kh + 1) * 4, q * 256:(q + 1) * 256], in_=sv[:, :, :, 0]),
                             reads=[r_st], writes=[r_wg])
                        S.op("act", lambda e, wu=wu, sv=sv, q=q, kh=kh: e.activation(out=wu[:, kh * 4:(kh + 1) * 4, q * 256:(q + 1) * 256], in_=sv[:, :, :, 1], func=AF.Copy),
                             reads=[r_st], writes=[r_wu])
                S.dma("pool", lambda e, wd=wd, e_=e_: e.dma_start(out=wd[:], in_=self.w_down[l, e_].rearrange("(k p) c -> p k c", p=128)), writes=[r_wd])
                bg, r_bg = bgrot.next()
                S.dma("sp", lambda e, bg=bg, e_=e_: e.dma_start(out=bg[:], in_=self.b_gu[l, e_, :].rearrange("(k p two) -> p k two", p=128, two=2),
                                                               allow_slow_non_contiguous=True), writes=[r_bg])
                bd, r_bd = bdrot.next()
                S.dma("act", lambda e, bd=bd, e_=e_: e.dma_start(out=bd[:], in_=self.b_down[l, e_:e_ + 1, :].to_broadcast([128, D])), writes=[r_bd])
                S.load_count(self.CNTI[0:1, e_:e_ + 1])
                for nt in range(NTE):
                    row0 = e_ * CAP + nt * 512
                    if nt > 0 and DYNAMIC_SKIP:
                        S.cond_begin(512 * nt)
                    self.expert_ntile(row0, (wg, r_wg), (wu, r_wu), (wd, r_wd), (bg, r_bg), (bd, r_bd),
                                      xerot, xtrot, atrot, gsrot, sgrot, usrot, yrot)
                    if nt > 0 and DYNAMIC_SKIP:
                        S.cond_end()
            self.end_phase()

    def expert_ntile(self, row0, wg_, wu_, wd_, bg_, bd_, xerot, xtrot, atrot, gsrot, sgrot, usrot, yrot):
        S = self.S
        wg, r_wg = wg_
        wu, r_wu = wu_
        wd, r_wd = wd_
        bg, r_bg = bg_
        bd, r_bd = bd_
        xe, r_xe = xerot.next()
        S.dma("sp", lambda e: e.dma_start(out=xe[:], in_=self.XE[row0:row0 + 512, :].rearrange("(j p) d -> p j d", p=128)),
              reads=[self.rXE], writes=[r_xe])
        xT, r_xT = xtrot.next()
        for jt in range(4):
            pt, r_p = self.pT.next()
            for k in range(8):
                S.op("pe", lambda e, pt=pt, jt=jt, k=k: e.transpose(out=pt[:, k * 128:(k + 1) * 128], in_=xe[:, jt, k * 128:(k + 1) * 128], identity=self.identb[:]),
                     reads=[r_xe, self.r_id], writes=[r_p], signal=(k == 7))
            S.op("dve", lambda e, pt=pt, jt=jt: e.tensor_copy(out=xT[:, :, jt * 128:(jt + 1) * 128], in_=pt[:, :].rearrange("p (k t) -> p k t", k=8)),
                 reads=[r_p], writes=[r_xT])
        aT, r_aT = atrot.next()
        for fk in range(8):
            pg, r_pg = self.pA.next()
            pu, r_pu = self.pA.next()
            for k in range(8):
                S.op("pe", lambda e, pg=pg, k=k, fk=fk: e.matmul(pg[:, :], lhsT=wg[:, k, fk * 128:(fk + 1) * 128], rhs=xT[:, k, :], start=(k == 0), stop=(k == 7)),
                     reads=[r_wg, r_xT], writes=[r_pg], signal=(k == 7))
            for k in range(8):
                S.op("pe", lambda e, pu=pu, k=k, fk=fk: e.matmul(pu[:, :], lhsT=wu[:, k, fk * 128:(fk + 1) * 128], rhs=xT[:, k, :], start=(k == 0), stop=(k == 7)),
                     reads=[r_wu, r_xT], writes=[r_pu], signal=(k == 7))
            gs, r_gs = gsrot.next()
            sg, r_sg = sgrot.next()
            us, r_us = usrot.next()
            S.op("dve", lambda e, gs=gs, pg=pg, fk=fk: e.tensor_scalar(out=gs[:], in0=pg[:, :], scalar1=bg[:, fk, 0:1], scalar2=7.0, op0=ALU.add, op1=ALU.min),
                 reads=[r_pg, r_bg], writes=[r_gs])
            S.op("act", lambda e, sg=sg, gs=gs: e.activation(out=sg[:], in_=gs[:], func=AF.Sigmoid, scale=1.702), reads=[r_gs], writes=[r_sg])
            S.op("dve", lambda e, us=us, pu=pu, fk=fk: e.tensor_scalar(out=us[:], in0=pu[:, :], scalar1=bg[:, fk, 1:2], scalar2=7.0, op0=ALU.add, op1=ALU.min),
                 reads=[r_pu, r_bg], writes=[r_us])
            S.op("dve", lambda e, us=us: e.tensor_scalar(out=us[:], in0=us[:], scalar1=-7.0, scalar2=1.0, op0=ALU.max, op1=ALU.add),
                 reads=[r_us], writes=[r_us])
            S.op("pool", lambda e, gs=gs, sg=sg: e.tensor_tensor(out=gs[:], in0=gs[:], in1=sg[:], op=ALU.mult), reads=[r_gs, r_sg], writes=[r_gs])
            S.op("dve", lambda e, us=us, gs=gs, fk=fk: e.tensor_tensor(out=aT[:, fk, :], in0=us[:], in1=gs[:], op=ALU.mult),
                 reads=[r_us, r_gs], writes=[r_aT])
        for jt in range(4):
            ye, r_ye = yrot.next()
            for half in range(2):
                pt, r_p = self.pA.next()
                for fk in range(8):
                    S.op("pe", lambda e, pt=pt, fk=fk, jt=jt, half=half: e.matmul(
                        pt[:, :], lhsT=aT[:, fk, jt * 128:(jt + 1) * 128], rhs=wd[:, fk, half * 512:(half + 1) * 512], start=(fk == 0), stop=(fk == 7)),
                        reads=[r_aT, r_wd], writes=[r_p], signal=(fk == 7))
                hs = slice(half * 512, (half + 1) * 512)
                S.op("dve", lambda e, ye=ye, pt=pt, hs=hs: e.tensor_tensor(out=ye[:, hs], in0=pt[:, :], in1=bd[:, hs], op=ALU.add),
                     reads=[r_p, r_bd], writes=[r_ye])
            S.dma("sp", lambda e, ye=ye, jt=jt: e.dma_start(out=self.YE[row0 + jt * 128:row0 + (jt + 1) * 128, :], in_=ye[:]),
                  reads=[r_ye], writes=[self.rYE])

    def phase_combine(self, l, last):
        nc, S = self.nc, self.S
        with ExitStack() as st:
            GT = self.sb(st, "GT2", [128, 2, D], F32)
            r_gt = Res()
            for j in range(2):
                self.load_bc("sp", GT[:, j, :], r_gt, self.MOD[l, j:j + 1, 5 * D:6 * D])
            if last:
                FG = self.sb(st, "FG", [128, D], F32)
                self.load_bc("sp", FG[:], r_gt, self.final_g[0:1, :])
            ykrot = self.rot_sb(st, "yk", [128, 4, D], F32, 2)
            accrot = self.rot_sb(st, "kacc", [128, D], F32, 2)
            xrot = self.rot_sb(st, "kx", [128, D], F32, 2)
            orot = self.rot_sb(st, "ko", [128, D], F32, 2)
            strot = self.stat_tiles(st, "kst")
            for i in range(self.nt_act):
                j = 0 if i < 32 else 1
                yk, r_yk = ykrot.next()
                for k in range(4):
                    S.dma("pool", lambda e, yk=yk, i=i, k=k: e.indirect_dma_start(
                        out=yk[:, k, :], out_offset=None, in_=self.YE[:, :], in_offset=bass.IndirectOffsetOnAxis(ap=self.DEST[:, i, k:k + 1], axis=0)),
                        reads=[self.rYE, self.rROUTE[i]], writes=[r_yk])
                xt, r_x = xrot.next()
                S.dma("sp", lambda e, xt=xt, i=i: e.dma_start(out=xt[:], in_=self.X[i * 128:(i + 1) * 128, :]), reads=[self.rX[i]], writes=[r_x])
                acc, r_a = accrot.next()
                S.op("dve", lambda e, acc=acc, yk=yk, i=i: e.tensor_scalar(out=acc[:], in0=yk[:, 0, :], scalar1=self.GATES[:, i, 0:1], scalar2=None, op0=ALU.mult),
                     reads=[r_yk, self.rROUTE[i]], writes=[r_a])
                for k in range(1, 4):
                    S.op("dve", lambda e, acc=acc, yk=yk, i=i, k=k: e.scalar_tensor_tensor(out=acc[:], in0=yk[:, k, :], scalar=self.GATES[:, i, k:k + 1], in1=acc[:], op0=ALU.mult, op1=ALU.add),
                         reads=[r_yk, self.rROUTE[i], r_a], writes=[r_a])
                S.op("pool", lambda e, acc=acc, j=j: e.tensor_tensor(out=acc[:], in0=acc[:], in1=GT[:, j, :], op=ALU.mult), reads=[r_a, r_gt], writes=[r_a])
                xo, r_xo = orot.next()
                S.op("pool", lambda e, xo=xo, acc=acc, xt=xt: e.tensor_tensor(out=xo[:], in0=acc[:], in1=xt[:], op=ALU.add), reads=[r_a, r_x], writes=[r_xo])
                if not last:
                    S.dma("sp", lambda e, xo=xo, i=i: e.dma_start(out=self.X[i * 128:(i + 1) * 128, :], in_=xo[:]), reads=[r_xo], writes=[self.rX[i]])
                else:
                    stt = strot.next()
                    rstd, r_s = self.rms_rstd(stt, xo, r_xo, D)
                    S.op("dve", lambda e, xo=xo, rstd=rstd: e.scalar_tensor_tensor(out=xo[:], in0=xo[:], scalar=rstd[:, 0:1], in1=FG[:], op0=ALU.mult, op1=ALU.mult),
                         reads=[r_xo, r_s, r_gt], writes=[r_xo])
                    S.dma("sp", lambda e, xo=xo, i=i: e.dma_start(out=self.out[i * 128:(i + 1) * 128, :], in_=xo[:]), reads=[r_xo], writes=[self.rOUT])
            self.end_phase()


def _na_table(rpb):
    a = np.arange(2)[:, None, None, None]
    kc = np.arange(64)[None, :, None, None]
    j = np.arange(NJ)[None, None, :, None]
    qc = np.arange(64)[None, None, None, :]
    dr = a - (j - JOFF) + 0 * kc + 0 * qc
    cs = np.clip(qc - 8, 0, 48)
    colv = (kc >= cs) & (kc < cs + 16)
    dc = kc - qc + 0 * a + 0 * j
    out = np.empty((2, 128, 8, NJ * 64), np.float32)
    for v in range(2):
        rowv = ((dr >= -4) & (dr <= 3)) if v == 0 else ((dr >= -7) & (dr <= 7))
        valid = np.broadcast_to(rowv & colv, (2, 64, NJ, 64))
        ri = np.clip(dr + 7, 0, 14)
        ci = np.clip(dc + 15, 0, 30)
        ri = np.broadcast_to(ri, (2, 64, NJ, 64))
        ci = np.broadcast_to(ci, (2, 64, NJ, 64))
        for h in range(8):
            vals = rpb[h][ri, ci]
            tbl = np.where(valid, vals, np.float32(NEG)).astype(np.float32)
            out[v, :, h, :] = tbl.reshape(128, NJ * 64)
    return out


def _rope_tables():
    half = 32
    inv = (10000.0 ** (-np.arange(0, half, 2, dtype=np.float32) / half)).astype(np.float32)
    t = np.arange(TL)
    ang_r = (t // 64).astype(np.float32)[:, None] * inv
    ang_c = (t % 64).astype(np.float32)[:, None] * inv
    cos = np.zeros((TL, 64), np.float32)
    sin = np.zeros((TL, 64), np.float32)
    for base, ang in ((0, ang_r), (32, ang_c)):
        c, s = np.cos(ang).astype(np.float32), np.sin(ang).astype(np.float32)
        cos[:, base:base + 16] = c
        cos[:, base + 16:base + 32] = c
        sin[:, base:base + 16] = -s
        sin[:, base + 16:base + 32] = s
    return np.tile(cos, (1, 10)), np.tile(sin, (1, 10))


def make_in_maps(inputs, n_cores=8):
    f = lambda a: np.ascontiguousarray(np.asarray(a, dtype=np.float32))
    x, c, ctx, c_ctx = f(inputs["x"]), f(inputs["c"]), f(inputs["ctx"]), f(inputs["c_ctx"])
    natab = np.stack([_na_table(f(inputs["na_rpb"])[l]) for l in range(DEPTH)])
    qg, kg = f(inputs["q_norm_g"]), f(inputs["k_norm_g"])
    qkgain = np.concatenate([np.tile(qg, (1, 8)), np.tile(kg, (1, 2))], axis=1)
    ropec, ropes = _rope_tables()
    w_br = np.stack([f(inputs["w_br_conv"]), f(inputs["w_br_na"]), f(inputs["w_br_gqa"])], axis=1)
    shared = dict(
        ada_w=f(inputs["ada_w"]), ada_b=f(inputs["ada_b"]), norm1_g=f(inputs["norm1_g"]), norm2_g=f(inputs["norm2_g"]),
        w_in=f(inputs["w_in"]), conv_w=f(inputs["conv_w"]), natab=natab, qkgain=np.ascontiguousarray(qkgain),
        ropec=ropec, ropes=ropes, w_br=np.ascontiguousarray(w_br), w_out=f(inputs["w_out"]),
        router_w=f(inputs["router_w"]), router_b=f(inputs["router_b"]), w_gu=f(inputs["w_gu"]), b_gu=f(inputs["b_gu"]),
        w_down=f(inputs["w_down"]), b_down=f(inputs["b_down"]), final_g=f(inputs["final_g"]).reshape(1, D))
    maps = []
    for b in range(n_cores):
        m = dict(shared)
        m["xin"] = np.ascontiguousarray(np.concatenate([x[b], ctx[b]], axis=0))
        m["cvec"] = np.ascontiguousarray(np.stack([c[b], c_ctx], axis=0))
        maps.append(m)
    return maps


def kernel(**inputs):
    nc = bass.Bass("TRN2", target_bir_lowering=False)
    Builder(nc).build()
    maps = make_in_maps(inputs)
    res = run_bass_kernel_spmd(nc, maps, core_ids=list(range(8)))
    return np.stack([np.asarray(r["out"], dtype=np.float32) for r in res.results], axis=0)
```

```python
import numpy as np
from contextlib import ExitStack
import concourse.bass as bass
import concourse.mybir as mybir
from concourse.bass_utils import run_bass_kernel_spmd

F32 = mybir.dt.float32
BF16 = mybir.dt.bfloat16
I32 = mybir.dt.int32
AF = mybir.ActivationFunctionType
ALU = mybir.AluOpType
AX = mybir.AxisListType

D = 1024
TL = 4096
TC = 256
T = TL + TC
NT = T // 128
DEPTH = 2
NE = 32
CAP = 2048
EPS = 1e-6
NEG = -30000.0
NJ = 22
JOFF = 10
INC = 6912
DYNAMIC_SKIP = True


class Res:
    __slots__ = ("w", "rs")

    def __init__(self):
        self.w = None
        self.rs = {}


class Sched:
    ENGS = ("pe", "act", "dve", "pool", "sp")
    NQ = 20

    def __init__(self, nc, stack, same_engine_sync=True):
        self.nc = nc
        self.eng = {"pe": nc.tensor, "act": nc.scalar, "dve": nc.vector,
                    "pool": nc.gpsimd, "sp": nc.sync}
        self.sem = {}
        self.cnt = {}
        self.seen = {e: {} for e in self.ENGS}
        self.prog = {e: [] for e in self.ENGS}
        self.same_engine_sync = same_engine_sync
        for e in self.ENGS:
            self.sem[e] = stack.enter_context(nc.semaphore("c_" + e))
            self.cnt[e] = 0
        self.dq = {}
        for q in ("sp", "act", "pool"):
            keys = []
            for i in range(self.NQ):
                k = "d_%s_%d" % (q, i)
                self.sem[k] = stack.enter_context(nc.semaphore(k))
                self.cnt[k] = 0
                keys.append(k)
            self.dq[q] = [keys, 0]

    def _deps(self, engine, reads, writes, extra=()):
        need = {}
        for r in reads:
            if r.w is not None:
                k, v = r.w
                if need.get(k, 0) < v:
                    need[k] = v
        for w in writes:
            if w.w is not None:
                k, v = w.w
                if need.get(k, 0) < v:
                    need[k] = v
            for k, v in w.rs.items():
                if need.get(k, 0) < v:
                    need[k] = v
        for k, v in extra:
            if need.get(k, 0) < v:
                need[k] = v
        out = []
        seen = self.seen[engine]
        for k, v in need.items():
            if k == engine and (engine == "pe" or not self.same_engine_sync):
                continue
            if seen.get(k, 0) >= v:
                continue
            seen[k] = v
            out.append((k, v))
        return out

    def _mark(self, ev, reads, writes):
        k, v = ev
        for r in reads:
            if r.rs.get(k, 0) < v:
                r.rs[k] = v
        for w in writes:
            w.w = ev
            w.rs = {}

    def op(self, engine, fn, reads=(), writes=(), signal=True):
        waits = self._deps(engine, reads, writes)
        sem = self.sem[engine]
        if signal:
            self.cnt[engine] += 1
        ev = (engine, self.cnt[engine] if signal else self.cnt[engine] + 1)
        sems = self.sem

        def emit(eng):
            for k, v in waits:
                eng.wait_ge(sems[k], v)
            ins = fn(eng)
            if signal:
                ins.then_inc(sem, 1)

        self.prog[engine].append(emit)
        self._mark(ev, reads, writes)
        return ev

    def dma(self, queue, fn, reads=(), writes=()):
        keys, idx = self.dq[queue]
        k = keys[idx % len(keys)]
        self.dq[queue][1] = idx + 1
        prev = self.cnt[k]
        extra = [(k, prev)] if prev > 0 else []
        waits = self._deps(queue, reads, writes, extra)
        self.cnt[k] = prev + 16
        ev = (k, prev + 16)
        sems = self.sem

        def emit(eng):
            for kk, v in waits:
                eng.wait_ge(sems[kk], v)
            fn(eng).then_inc(sems[k], 16)

        self.prog[queue].append(emit)
        self._mark(ev, reads, writes)
        return ev

    def load_count(self, ap):
        for e in self.ENGS:
            self.prog[e].append(("ldreg", ap))

    def cond_begin(self, thr):
        self._cond = dict(thr=thr, cnt=dict(self.cnt), seen={e: dict(v) for e, v in self.seen.items()},
                          dq={q: self.dq[q][1] for q in self.dq})
        for e in self.ENGS:
            self.prog[e].append(("if", thr))

    def cond_end(self):
        c = self._cond
        sems = self.sem
        for e in self.ENGS:
            delta = self.cnt[e] - c["cnt"][e]
            dl = []
            if e in self.dq:
                keys = self.dq[e][0]
                run = dict()
                for idx in range(c["dq"][e], self.dq[e][1]):
                    k = keys[idx % len(keys)]
                    prev = run.get(k, c["cnt"][k])
                    dl.append((k, prev))
                    run[k] = prev + 16

            def comp(eng, e=e, delta=delta, dl=dl):
                if e != "sp":
                    eng.drain()
                if delta > 0:
                    eng.sem_inc(sems[e], delta)
                for k, prev in dl:
                    if prev > 0:
                        eng.wait_ge(sems[k], prev)
                    eng.sem_inc(sems[k], 16)

            self.prog[e].append(("endif", comp))
        self.seen = c["seen"]
        self._cond = None

    def barrier(self):
        sems = self.sem
        for e in self.ENGS:
            waits = []
            seen = self.seen[e]
            for k, v in self.cnt.items():
                if v > 0 and seen.get(k, 0) < v:
                    seen[k] = v
                    waits.append((k, v))

            def emit(eng, waits=waits):
                for k, v in waits:
                    eng.wait_ge(sems[k], v)

            self.prog[e].append(emit)

    def _run(self, name, eng):
        items = self.prog[name]
        if not hasattr(self, "regs"):
            self.regs = {}
        i = 0
        n = len(items)
        while i < n:
            it = items[i]
            if callable(it):
                it(eng)
                i += 1
                continue
            kind = it[0]
            if kind == "ldreg":
                if name not in self.regs:
                    self.regs[name] = eng.alloc_register("cnt_" + name)
                eng.reg_load(self.regs[name], it[1])
                i += 1
            elif kind == "if":
                thr = it[1]
                j = i + 1
                while not (isinstance(items[j], tuple) and items[j][0] == "endif"):
                    j += 1
                body = items[i + 1:j]
                comp = items[j][1]
                with eng.If_lt(self.regs[name], thr + 1):
                    comp(eng)
                with eng.Else():
                    for f in body:
                        f(eng)
                i = j + 1
            else:
                raise RuntimeError("bad prog item")

    def flush(self):
        nc = self.nc
        with nc.Block() as block:
            @block.sync
            def _(e):
                self._run("sp", e)

            @block.scalar
            def _(e):
                self._run("act", e)

            @block.vector
            def _(e):
                self._run("dve", e)

            @block.gpsimd
            def _(e):
                self._run("pool", e)

            @block.tensor
            def _(e):
                self._run("pe", e)
        self.prog = {e: [] for e in self.ENGS}


class Rot:
    def __init__(self, items):
        self.items = items
        self.i = 0

    def next(self):
        it = self.items[self.i % len(self.items)]
        self.i += 1
        return it


def ntiles512(n_tok):
    out = []
    t = 0
    while t < n_tok:
        w = min(512, n_tok - t)
        out.append((t, w))
        t += w
    return out


class Builder:
    def __init__(self, nc, dbg=None, layers=(0, 1), stop_after=None):
        self.nc = nc
        self.dbg = dbg or []
        self.layers = layers
        self.stop_after = stop_after

    def sb(self, st, name, shape, dt):
        self._uid = getattr(self, "_uid", 0) + 1
        return st.enter_context(self.nc.sbuf_tensor("%s_%d" % (name, self._uid), list(shape), dt))

    def rot_sb(self, st, name, shape, dt, n):
        return Rot([(self.sb(st, "%s%d" % (name, i), shape, dt), Res()) for i in range(n)])

    def end_phase(self):
        self.S.barrier()
        self.S.flush()

    def declare(self):
        nc = self.nc
        di = lambda n, s, dt=F32: nc.dram_tensor(n, list(s), dt, kind="ExternalInput").ap()
        self.xin = di("xin", [T, D])
        self.cvec = di("cvec", [2, D])
        self.ada_w = di("ada_w", [DEPTH, D, 6 * D])
        self.ada_b = di("ada_b", [DEPTH, 6 * D])
        self.norm1_g = di("norm1_g", [DEPTH, D])
        self.norm2_g = di("norm2_g", [DEPTH, D])
        self.w_in = di("w_in", [DEPTH, D, INC])
        self.conv_w = di("conv_w", [DEPTH, 3, 512])
        self.natab = di("natab", [DEPTH, 2, 128, 8, NJ * 64])
        self.qkgain = di("qkgain", [DEPTH, 640])
        self.ropec = di("ropec", [TL, 640])
        self.ropes = di("ropes", [TL, 640])
        self.w_br = di("w_br", [DEPTH, 3, 512, D])
        self.w_out = di("w_out", [DEPTH, D, D])
        self.router_w = di("router_w", [DEPTH, D, NE])
        self.router_b = di("router_b", [DEPTH, NE])
        self.w_gu = di("w_gu", [DEPTH, NE, D, 2 * D])
        self.b_gu = di("b_gu", [DEPTH, NE, 2 * D])
        self.w_down = di("w_down", [DEPTH, NE, D, D])
        self.b_down = di("b_down", [DEPTH, NE, D])
        self.final_g = di("final_g", [1, D])
        self.out = nc.dram_tensor("out", [TL, D], F32, kind="ExternalOutput").ap()

        def scr(n, s, dt):
            kind = "ExternalOutput" if n in self.dbg else "Internal"
            return nc.dram_tensor(n, list(s), dt, kind=kind).ap()
        self.X = scr("X", [T, D], F32)
        self.MOD = scr("MOD", [DEPTH, 2, 6 * D], F32)
        self.FT = scr("FT", [INC, T], BF16)
        self.TM = scr("TM", [T, 1280], BF16)
        self.YT = scr("YT", [1536, T], BF16)
        self.XE = scr("XE", [NE * CAP, D], BF16)
        self.YE = scr("YE", [NE * CAP, D], F32)
        self.rX = [Res() for _ in range(NT)]
        self.rMOD = Res()
        self.rFT = Res()
        self.rTM = Res()
        self.rYT = Res()
        self.rXE = Res()
        self.rYE = Res()
        self.rOUT = Res()

    def build(self):
        nc = self.nc
        self.declare()
        with ExitStack() as gst:
            S = self.S = Sched(nc, gst)
            self.pA = Rot([(gst.enter_context(nc.psum_tensor("pA%d" % i, [128, 512], F32)), Res()) for i in range(4)])
            self.pB = Rot([(gst.enter_context(nc.psum_tensor("pB%d" % i, [128, 512], F32)), Res()) for i in range(2)])
            self.pT = Rot([(gst.enter_context(nc.psum_tensor("pT%d" % i, [128, 1024], BF16)), Res()) for i in range(2)])
            self.identf = self.sb(gst, "identf", [128, 128], F32)
            self.identb = self.sb(gst, "identb", [128, 128], BF16)
            self.r_id = Res()
            idf, idb = self.identf, self.identb
            S.op("pool", lambda e: e.memset(idf[:], 0.0), writes=[self.r_id])
            S.op("pool", lambda e: e.affine_select(out=idf[:], in_=idf[:], pattern=[[-1, 128]],
                                                   compare_op=ALU.not_equal, fill=1.0, base=0, channel_multiplier=1),
                 reads=[self.r_id], writes=[self.r_id])
            S.op("dve", lambda e: e.tensor_copy(out=idb[:], in_=idf[:]), reads=[self.r_id], writes=[self.r_id])
            self.DEST = self.sb(gst, "DEST", [128, NT, 4], I32)
            self.GATES = self.sb(gst, "GATES", [128, NT, 4], F32)
            self.rROUTE = [Res() for _ in range(NT)]
            self.CNTI = self.sb(gst, "CNTI", [128, NE], I32)
            self.end_phase()

            self.phase_mods()
            if self.stop_after == "mods":
                return self.finish()
            for l in self.layers:
                last = (l == DEPTH - 1)
                self.Xsrc = self.xin if l == 0 else self.X
                self.nt_act = 32 if last else NT
                self.phase_AB(l)
                if self.stop_after == "AB%d" % l:
                    return self.finish()
                self.phase_conv(l)
                self.phase_gqa(l, last)
                self.phase_na(l, last)
                if self.stop_after == "attn%d" % l:
                    return self.finish()
                self.phase_merge(l)
                if self.stop_after == "merge%d" % l:
                    return self.finish()
                self.phase_route(l)
                self.phase_experts(l)
                if self.stop_after == "exp%d" % l:
                    return self.finish()
                self.phase_combine(l, last)
                if self.stop_after == "comb%d" % l:
                    return self.finish()
            return self.finish()

    def finish(self):
        S = self.S
        S.barrier()
        S.flush()

    def phase_mods(self):
        nc, S = self.nc, self.S
        with ExitStack() as st:
            cs = self.sb(st, "cs", [128, 8, 2], F32)
            ca = self.sb(st, "ca", [128, 8, 2], F32)
            r_cs = Res()
            for j in range(2):
                S.dma("sp", lambda e, j=j: e.dma_start(out=cs[:, :, j], in_=self.cvec[j, :].rearrange("(p k) -> p k", k=8), allow_slow_non_contiguous=True),
                      writes=[r_cs])
            S.op("act", lambda e: e.activation(out=ca[:], in_=cs[:], func=AF.Silu), reads=[r_cs], writes=[r_cs])
            wrot = self.rot_sb(st, "adaw", [128, 8, 512], F32, 2)
            bias = self.sb(st, "adab", [2, 6 * D], F32)
            modsb = self.sb(st, "modsb", [2, 6 * D], F32)
            r_b = Res()
            r_m = Res()
            for l in range(DEPTH):
                S.dma("sp", lambda e, l=l: e.dma_start(out=bias[:], in_=self.ada_b[l:l + 1, :].to_broadcast([2, 6 * D])),
                      writes=[r_b])
                wv = self.ada_w[l].rearrange("(p k) f -> p k f", k=8)
                for fb in range(12):
                    wt, r_w = wrot.next()
                    S.dma("sp" if fb % 2 == 0 else "act",
                          lambda e, wt=wt, fb=fb, wv=wv: e.dma_start(out=wt[:], in_=wv[:, :, fb * 512:(fb + 1) * 512]),
                          writes=[r_w])
                    pt, r_p = self.pA.next()
                    for k in range(8):
                        S.op("pe", lambda e, pt=pt, wt=wt, k=k: e.matmul(pt[0:2, :], lhsT=ca[:, k, :], rhs=wt[:, k, :],
                                                                        start=(k == 0), stop=(k == 7)),
                             reads=[r_cs, r_w], writes=[r_p], signal=(k == 7))
                    S.op("dve", lambda e, pt=pt, fb=fb: e.tensor_tensor(out=modsb[:, fb * 512:(fb + 1) * 512], in0=pt[0:2, :],
                                                                        in1=bias[:, fb * 512:(fb + 1) * 512], op=ALU.add),
                         reads=[r_p, r_b], writes=[r_m])
                S.dma("sp", lambda e, l=l: e.dma_start(out=self.MOD[l], in_=modsb[:]), reads=[r_m], writes=[self.rMOD])
            self.end_phase()

    def load_feat(self, queue, tile, res, src_row):
        self.S.dma(queue, lambda e: e.dma_start(out=tile[:], in_=src_row.rearrange("(k p) -> p k", p=128),
                                                allow_slow_non_contiguous=True),
                   reads=[self.rMOD], writes=[res])

    def load_bc(self, queue, tile_ap, res, src_row2d, n=128):
        F = src_row2d.shape[-1]
        self.S.dma(queue, lambda e: e.dma_start(out=tile_ap, in_=src_row2d.to_broadcast([n, F])),
                   reads=[self.rMOD], writes=[res])

    def rms_rstd(self, st_tiles, xt, r_x, width):
        S = self.S
        junk, ss, ms, rstd, r_s = st_tiles
        S.op("act", lambda e: e.activation(out=junk[:, 0:width], in_=xt[:, 0:width], func=AF.Square, accum_out=ss[:, 0:1]),
             reads=[r_x], writes=[r_s])
        S.op("dve", lambda e: e.tensor_scalar(out=ms[:], in0=ss[:], scalar1=1.0 / width, scalar2=EPS, op0=ALU.mult, op1=ALU.add),
             reads=[r_s], writes=[r_s])
        S.op("act", lambda e: e.activation(out=ms[:], in_=ms[:], func=AF.Sqrt), reads=[r_s], writes=[r_s])
        S.op("dve", lambda e: e.reciprocal(out=rstd[:], in_=ms[:]), reads=[r_s], writes=[r_s])
        return rstd, r_s

    def stat_tiles(self, st, name, n=2):
        items = []
        for i in range(n):
            items.append((self.sb(st, "%sj%d" % (name, i), [128, 1024], BF16), self.sb(st, "%ss%d" % (name, i), [128, 1], F32),
                          self.sb(st, "%sm%d" % (name, i), [128, 1], F32), self.sb(st, "%sr%d" % (name, i), [128, 1], F32), Res()))
        return Rot(items)

    def phase_AB(self, l):
        nc, S = self.nc, self.S
        with ExitStack() as st:
            hT = self.sb(st, "hT", [128, 8, T], BF16)
            r_h = [Res() for _ in range(NT)]
            G1 = self.sb(st, "G1", [128, 2, 8], F32)
            SH1 = self.sb(st, "SH1", [128, 2, 8], F32)
            ng = self.sb(st, "ng", [128, 8], F32)
            r_g = Res()
            self.load_feat("sp", ng, r_g, self.norm1_g[l, :])
            for j in range(2):
                S.dma("sp", lambda e, j=j: e.dma_start(out=SH1[:, j, :], in_=self.MOD[l, j, 0:D].rearrange("(k p) -> p k", p=128),
                                                       allow_slow_non_contiguous=True), reads=[self.rMOD], writes=[r_g])
                S.dma("sp", lambda e, j=j: e.dma_start(out=G1[:, j, :], in_=self.MOD[l, j, D:2 * D].rearrange("(k p) -> p k", p=128),
                                                       allow_slow_non_contiguous=True), reads=[self.rMOD], writes=[r_g])
            for j in range(2):
                S.op("dve", lambda e, j=j: e.scalar_tensor_tensor(out=G1[:, j, :], in0=G1[:, j, :], scalar=1.0, in1=ng[:],
                                                                  op0=ALU.add, op1=ALU.mult), reads=[r_g], writes=[r_g])
            xrot = self.rot_sb(st, "xt", [128, D], F32, 2)
            xsrot = self.rot_sb(st, "xs", [128, D], BF16, 2)
            strot = self.stat_tiles(st, "st")
            for i in range(NT):
                xt, r_x = xrot.next()
                S.dma("sp", lambda e, xt=xt, i=i: e.dma_start(out=xt[:], in_=self.Xsrc[i * 128:(i + 1) * 128, :]),
                      reads=[self.rX[i]], writes=[r_x])
                stt = strot.next()
                rstd, r_s = self.rms_rstd(stt, xt, r_x, D)
                xs, r_xs = xsrot.next()
                S.op("act", lambda e, xs=xs, xt=xt, rstd=rstd: e.activation(out=xs[:], in_=xt[:], func=AF.Copy, scale=rstd[:, 0:1]),
                     reads=[r_x, r_s], writes=[r_xs])
                pt, r_p = self.pT.next()
                for k in range(8):
                    S.op("pe", lambda e, pt=pt, xs=xs, k=k: e.transpose(out=pt[:, k * 128:(k + 1) * 128], in_=xs[:, k * 128:(k + 1) * 128],
                                                                        identity=self.identb[:]),
                         reads=[r_xs, self.r_id], writes=[r_p], signal=(k == 7))
                j = 0 if i < 32 else 1
                for k in range(8):
                    S.op("act", lambda e, pt=pt, k=k, i=i, j=j: e.activation(out=hT[:, k, i * 128:(i + 1) * 128], in_=pt[:, k * 128:(k + 1) * 128],
                                                                             func=AF.Identity, scale=G1[:, j, k:k + 1], bias=SH1[:, j, k:k + 1]),
                         reads=[r_p, r_g], writes=[r_h[i]])
            wv = self.w_in[l].rearrange("(k p) c -> p k c", p=128)
            wrot = self.rot_sb(st, "wblk", [128, 8, 512], BF16, 2)
            stg = self.rot_sb(st, "stg", [128, T], BF16, 2)
            tmst = self.rot_sb(st, "tmst", [128, 512], BF16, 3)
            nts = ntiles512(T)
            ev_i = 0
            for cb in range(14):
                c0 = cb * 512
                cw = min(512, INC - c0)
                wt, r_w = wrot.next()
                S.dma("pool", lambda e, wt=wt, c0=c0, cw=cw: e.dma_start(out=wt[:, :, 0:cw], in_=wv[:, :, c0:c0 + cw]), writes=[r_w])
                tm_lo, tm_hi = max(c0, 2560), min(c0 + cw, 3840)
                for cc in range(cw // 128):
                    col = c0 + cc * 128
                    if 2560 <= col < 3840:
                        continue
                    is_gate = col >= 3840
                    sg, r_sg = stg.next()
                    for (t0, tw) in nts:
                        pt, r_p = self.pA.next()
                        rh = r_h[t0 // 128:(t0 + tw) // 128]
                        for k in range(8):
                            S.op("pe", lambda e, pt=pt, wt=wt, k=k, cc=cc, t0=t0, tw=tw: e.matmul(
                                pt[:, 0:tw], lhsT=wt[:, k, cc * 128:(cc + 1) * 128], rhs=hT[:, k, t0:t0 + tw], start=(k == 0), stop=(k == 7)),
                                reads=[r_w] + rh, writes=[r_p], signal=(k == 7))
                        if is_gate:
                            S.op("act", lambda e, pt=pt, sg=sg, t0=t0, tw=tw: e.activation(out=sg[:, t0:t0 + tw], in_=pt[:, 0:tw], func=AF.Sigmoid),
                                 reads=[r_p], writes=[r_sg])
                        elif ev_i % 2 == 0:
                            S.op("act", lambda e, pt=pt, sg=sg, t0=t0, tw=tw: e.activation(out=sg[:, t0:t0 + tw], in_=pt[:, 0:tw], func=AF.Copy),
                                 reads=[r_p], writes=[r_sg])
                        else:
                            S.op("dve", lambda e, pt=pt, sg=sg, t0=t0, tw=tw: e.tensor_copy(out=sg[:, t0:t0 + tw], in_=pt[:, 0:tw]),
                                 reads=[r_p], writes=[r_sg])
                        ev_i += 1
                    S.dma("sp", lambda e, sg=sg, col=col: e.dma_start(out=self.FT[col:col + 128, :], in_=sg[:]), reads=[r_sg], writes=[self.rFT])
                if tm_lo < tm_hi:
                    w0, wn = tm_lo - c0, tm_hi - tm_lo
                    for i in range(NT):
                        pt, r_p = self.pA.next()
                        for k in range(8):
                            S.op("pe", lambda e, pt=pt, wt=wt, k=k, i=i, w0=w0, wn=wn: e.matmul(
                                pt[:, 0:wn], lhsT=hT[:, k, i * 128:(i + 1) * 128], rhs=wt[:, k, w0:w0 + wn], start=(k == 0), stop=(k == 7)),
                                reads=[r_w, r_h[i]], writes=[r_p], signal=(k == 7))
                        ts, r_ts = tmst.next()
                        if i % 2 == 0:
                            S.op("act", lambda e, pt=pt, ts=ts, wn=wn: e.activation(out=ts[:, 0:wn], in_=pt[:, 0:wn], func=AF.Copy),
                                 reads=[r_p], writes=[r_ts])
                        else:
                            S.op("dve", lambda e, pt=pt, ts=ts, wn=wn: e.tensor_copy(out=ts[:, 0:wn], in_=pt[:, 0:wn]),
                                 reads=[r_p], writes=[r_ts])
                        S.dma("sp", lambda e, ts=ts, i=i, wn=wn, tm_lo=tm_lo: e.dma_start(
                            out=self.TM[i * 128:(i + 1) * 128, tm_lo - 2560:tm_lo - 2560 + wn], in_=ts[:, 0:wn]), reads=[r_ts], writes=[self.rTM])
            self.end_phase()

    def phase_conv(self, l):
        nc, S = self.nc, self.S
        with ExitStack() as st:
            cw = self.sb(st, "cw", [128, 4, 3], F32)
            r_cw = Res()
            for kk in range(3):
                S.dma("sp", lambda e, kk=kk: e.dma_start(out=cw[:, :, kk], in_=self.conv_w[l, kk, :].rearrange("(j p) -> p j", p=128),
                                                         allow_slow_non_contiguous=True), writes=[r_cw])
            inrot = self.rot_sb(st, "cin", [128, 3, T], BF16, 2)
            u = self.sb(st, "cu", [128, T], F32)
            acc = self.sb(st, "cacc", [128, T], F32)
            yrot = self.rot_sb(st, "cy", [128, T], BF16, 2)
            r_u, r_a = Res(), Res()
            for j in range(4):
                ci, r_ci = inrot.next()
                for b in range(3):
                    S.dma("sp" if b != 1 else "act", lambda e, ci=ci, b=b, j=j: e.dma_start(
                        out=ci[:, b, :], in_=self.FT[b * 512 + j * 128:b * 512 + (j + 1) * 128, :]), reads=[self.rFT], writes=[r_ci])
                S.op("pool", lambda e, ci=ci: e.tensor_tensor(out=u[:], in0=ci[:, 1, :], in1=ci[:, 2, :], op=ALU.mult),
                     reads=[r_ci], writes=[r_u])
                S.op("dve", lambda e, j=j: e.tensor_scalar(out=acc[:], in0=u[:], scalar1=cw[:, j, 1:2], scalar2=None, op0=ALU.mult),
                     reads=[r_u, r_cw], writes=[r_a])
                for (a, b) in ((0, TL), (TL, T)):
                    S.op("dve", lambda e, j=j, a=a, b=b: e.scalar_tensor_tensor(out=acc[:, a + 1:b], in0=u[:, a:b - 1], scalar=cw[:, j, 0:1],
                                                                              in1=acc[:, a + 1:b], op0=ALU.mult, op1=ALU.add),
                         reads=[r_u, r_cw, r_a], writes=[r_a])
                    S.op("dve", lambda e, j=j, a=a, b=b: e.scalar_tensor_tensor(out=acc[:, a:b - 1], in0=u[:, a + 1:b], scalar=cw[:, j, 2:3],
                                                                              in1=acc[:, a:b - 1], op0=ALU.mult, op1=ALU.add),
                         reads=[r_u, r_cw, r_a], writes=[r_a])
                y, r_y = yrot.next()
                S.op("pool", lambda e, y=y, ci=ci: e.tensor_tensor(out=y[:], in0=ci[:, 0, :], in1=acc[:], op=ALU.mult),
                     reads=[r_ci, r_a], writes=[r_y])
                S.dma("sp", lambda e, y=y, j=j: e.dma_start(out=self.YT[j * 128:(j + 1) * 128, :], in_=y[:]), reads=[r_y], writes=[self.rYT])
            self.end_phase()

    def attn_block(self, kt_ap_fn, q_ap, va_ap_fn, chunks, N, acc, r_acc, prot, reads, tab_fn=None, addrot=None, scale=0.125):
        S = self.S
        n = len(chunks)
        pend = []

        def qk(si):
            s = chunks[si]
            ps, r_ps = self.pA.next()
            kt = kt_ap_fn(s)
            S.op("pe", lambda e: e.matmul(ps[:, 0:N], lhsT=kt, rhs=q_ap, start=True, stop=True), reads=reads, writes=[r_ps])
            pe_t, r_pe = prot.next()
            tb = tab_fn(s) if tab_fn is not None else None
            if tb is not None:
                ad, r_ad = addrot.next()
                S.op("dve", lambda e: e.scalar_tensor_tensor(out=ad[:, 0:N], in0=ps[:, 0:N], scalar=scale, in1=tb, op0=ALU.mult, op1=ALU.add),
                     reads=[r_ps] + reads, writes=[r_ad])
                S.op("act", lambda e: e.activation(out=pe_t[:, 0:N], in_=ad[:, 0:N], func=AF.Exp), reads=[r_ad], writes=[r_pe])
            else:
                S.op("act", lambda e: e.activation(out=pe_t[:, 0:N], in_=ps[:, 0:N], func=AF.Exp, scale=scale), reads=[r_ps], writes=[r_pe])
            return (s, pe_t, r_pe)

        LA = 2
        for si in range(min(LA, n)):
            pend.append(qk(si))
        for si in range(n):
            if si + LA < n:
                pend.append(qk(si + LA))
            s, pe_t, r_pe = pend.pop(0)
            va_ = va_ap_fn(s)
            S.op("pe", lambda e, va_=va_, pe_t=pe_t, si=si: e.matmul(acc[:, 0:N], lhsT=va_, rhs=pe_t[:, 0:N], start=(si == 0), stop=(si == n - 1)),
                 reads=[r_pe] + reads, writes=[r_acc], signal=(si == n - 1))

    def attn_finish(self, acc, r_acc, N, out_ap, r_out, recrot):
        S = self.S
        rec, r_rec = recrot.next()
        S.op("act", lambda e: e.activation(out=rec[0:64, 0:N], in_=acc[64:128, 0:N], func=AF.Copy), reads=[r_acc], writes=[r_rec])
        S.op("dve", lambda e: e.reciprocal(out=rec[0:64, 0:N], in_=rec[0:64, 0:N]), reads=[r_rec], writes=[r_rec])
        S.op("dve", lambda e: e.tensor_tensor(out=out_ap, in0=acc[0:64, 0:N], in1=rec[0:64, 0:N], op=ALU.mult),
             reads=[r_acc, r_rec], writes=[r_out])

    def phase_gqa(self, l, last):
        nc, S = self.nc, self.S
        with ExitStack() as st:
            QT = self.sb(st, "QT", [128, 8, T], BF16)
            KT2 = self.sb(st, "KT2", [128, 2, T], BF16)
            VA = self.sb(st, "VA", [128, NT, 2, 128], BF16)
            r_q = Res()
            r_vat = [Res() for _ in range(NT)]
            S.op("pool", lambda e: e.memset(VA[:, :, :, 64:128], 1.0), writes=r_vat)
            S.op("pool", lambda e: e.memset(QT[:], 0.0), writes=[r_q])
            for i in range(NT):
                S.dma("sp" if i % 2 == 0 else "act", lambda e, i=i: e.dma_start(
                    out=VA[:, i, :, 0:64], in_=self.TM[i * 128:(i + 1) * 128, 1152:1280].rearrange("p (g d) -> p g d", g=2)),
                    reads=[self.rTM], writes=[r_vat[i]])
            gain = self.sb(st, "gain", [128, 640], F32)
            r_gn = Res()
            S.dma("sp", lambda e: e.dma_start(out=gain[:], in_=self.qkgain[l:l + 1, :].to_broadcast([128, 640])), writes=[r_gn])
            with ExitStack() as st2:
                inrot = self.rot_sb(st2, "gin", [128, 640], BF16, 2)
                sqrot = self.rot_sb(st2, "gsq", [128, 640], F32, 2)
                xnrot = self.rot_sb(st2, "gxn", [128, 640], F32, 2)
                swrot = self.rot_sb(st2, "gsw", [128, 640], F32, 2)
                cosrot = self.rot_sb(st2, "gcos", [128, 640], F32, 2)
                sinrot = self.rot_sb(st2, "gsin", [128, 640], F32, 2)
                qbrot = self.rot_sb(st2, "gqb", [128, 640], BF16, 2)
                ssrot = Rot([(self.sb(st2, "gss%d" % i, [128, 10], F32), Res()) for i in range(2)])
                for i in range(NT):
                    xi, r_xi = inrot.next()
                    S.dma("sp", lambda e, xi=xi, i=i: e.dma_start(out=xi[:], in_=self.TM[i * 128:(i + 1) * 128, 512:1152]), reads=[self.rTM], writes=[r_xi])
                    sq, r_sq = sqrot.next()
                    S.op("pool", lambda e, sq=sq, xi=xi: e.tensor_tensor(out=sq[:], in0=xi[:], in1=xi[:], op=ALU.mult), reads=[r_xi], writes=[r_sq])
                    ss, r_ss = ssrot.next()
                    S.op("dve", lambda e, ss=ss, sq=sq: e.tensor_reduce(out=ss[:], in_=sq[:].rearrange("p (h d) -> p h d", d=64), axis=AX.X, op=ALU.add),
                         reads=[r_sq], writes=[r_ss])
                    S.op("dve", lambda e, ss=ss: e.tensor_scalar(out=ss[:], in0=ss[:], scalar1=1.0 / 64, scalar2=EPS, op0=ALU.mult, op1=ALU.add),
                         reads=[r_ss], writes=[r_ss])
                    S.op("act", lambda e, ss=ss: e.activation(out=ss[:], in_=ss[:], func=AF.Sqrt), reads=[r_ss], writes=[r_ss])
                    S.op("dve", lambda e, ss=ss: e.reciprocal(out=ss[:], in_=ss[:]), reads=[r_ss], writes=[r_ss])
                    xn, r_xn = xnrot.next()
                    r_xh = [Res() for _ in range(10)]
                    for h in range(10):
                        S.op("dve", lambda e, xn=xn, xi=xi, ss=ss, h=h: e.tensor_scalar(
                            out=xn[:, h * 64:(h + 1) * 64], in0=xi[:, h * 64:(h + 1) * 64], scalar1=ss[:, h:h + 1], scalar2=None, op0=ALU.mult),
                            reads=[r_xi, r_ss, r_xn], writes=[r_xh[h]])
                    S.op("dve", lambda e, xn=xn: e.tensor_tensor(out=xn[:], in0=xn[:], in1=gain[:], op=ALU.mult), reads=r_xh + [r_gn], writes=[r_xn])
                    qb, r_qb = qbrot.next()
                    if i < 32:
                        co, r_co = cosrot.next()
                        si_, r_si = sinrot.next()
                        S.dma("act", lambda e, co=co, i=i: e.dma_start(out=co[:], in_=self.ropec[i * 128:(i + 1) * 128, :]), writes=[r_co])
                        S.dma("act", lambda e, si_=si_, i=i: e.dma_start(out=si_[:], in_=self.ropes[i * 128:(i + 1) * 128, :]), writes=[r_si])
                        sw, r_sw = swrot.next()
                        xv = xn[:].rearrange("p (g two d) -> p g two d", two=2, d=16)
                        swv = sw[:].rearrange("p (g two d) -> p g two d", two=2, d=16)
                        S.op("pool", lambda e, swv=swv, xv=xv: e.tensor_copy(out=swv[:, :, 0, :], in_=xv[:, :, 1, :]), reads=[r_xn], writes=[r_sw])
                        S.op("pool", lambda e, swv=swv, xv=xv: e.tensor_copy(out=swv[:, :, 1, :], in_=xv[:, :, 0, :]), reads=[r_xn, r_sw], writes=[r_sw])
                        S.op("pool", lambda e, sw=sw, si_=si_: e.tensor_tensor(out=sw[:], in0=sw[:], in1=si_[:], op=ALU.mult), reads=[r_sw, r_si], writes=[r_sw])
                        S.op("dve", lambda e, xn=xn, co=co: e.tensor_tensor(out=xn[:], in0=xn[:], in1=co[:], op=ALU.mult), reads=[r_xn, r_co], writes=[r_xn])
                        S.op("dve", lambda e, qb=qb, xn=xn, sw=sw: e.tensor_tensor(out=qb[:], in0=xn[:], in1=sw[:], op=ALU.add), reads=[r_xn, r_sw], writes=[r_qb])
                    else:
                        S.op("dve", lambda e, qb=qb, xn=xn: e.tensor_copy(out=qb[:], in_=xn[:]), reads=[r_xn], writes=[r_qb])
                    pt, r_p = self.pT.next()
                    for c in range(5):
                        S.op("pe", lambda e, pt=pt, qb=qb, c=c: e.transpose(out=pt[:, c * 128:(c + 1) * 128], in_=qb[:, c * 128:(c + 1) * 128], identity=self.identb[:]),
                             reads=[r_qb, self.r_id], writes=[r_p], signal=(c == 4))
                    tsl = slice(i * 128, (i + 1) * 128)
                    QTv = QT[:].rearrange("p (c two) t -> p c two t", two=2)
                    S.op("act", lambda e, pt=pt, tsl=tsl, QTv=QTv: e.activation(out=QTv[0:64, :, 0, tsl], in_=pt[0:64, 0:512].rearrange("p (c t) -> p c t", c=4), func=AF.Copy),
                         reads=[r_p, r_q], writes=[r_q])
                    S.op("act", lambda e, pt=pt, tsl=tsl, QTv=QTv: e.activation(out=QTv[64:128, :, 1, tsl], in_=pt[64:128, 0:512].rearrange("p (c t) -> p c t", c=4), func=AF.Copy),
                         reads=[r_p, r_q], writes=[r_q])
                    S.op("dve", lambda e, pt=pt, tsl=tsl: e.tensor_copy(out=KT2[0:64, 0, tsl], in_=pt[0:64, 512:640]), reads=[r_p, r_q], writes=[r_q])
                    S.op("dve", lambda e, pt=pt, tsl=tsl: e.tensor_copy(out=KT2[64:128, 1, tsl], in_=pt[64:128, 512:640]), reads=[r_p, r_q], writes=[r_q])
                    S.op("act", lambda e, pt=pt, tsl=tsl: e.activation(out=KT2[64:128, 0, tsl], in_=pt[0:64, 512:640], func=AF.Copy), reads=[r_p, r_q], writes=[r_q])
                    S.op("act", lambda e, pt=pt, tsl=tsl: e.activation(out=KT2[0:64, 1, tsl], in_=pt[64:128, 512:640], func=AF.Copy), reads=[r_p, r_q], writes=[r_q])
                self.S.barrier()
            prot = self.rot_sb(st, "gpe", [128, 512], BF16, 5)
            recrot = self.rot_sb(st, "grec", [64, 512], F32, 2)
            ysrot = self.rot_sb(st, "gys", [64, T], BF16, 2)
            for h in range(8):
                g, c, hh = h // 4, h // 2, h % 2
                ps_ = slice(hh * 64, (hh + 1) * 64)
                ys, r_ys = ysrot.next()
                blocks = [(n * 512, 512, list(range(NT))) for n in range(8)]
                if not last:
                    blocks.append((TL, TC, [32, 33]))
                for (q0, N, chunks) in blocks:
                    acc, r_acc = self.pB.next()
                    self.attn_block(lambda s: KT2[:, g, s * 128:(s + 1) * 128], QT[:, h, q0:q0 + N],
                                    lambda s: VA[:, s, g, :], chunks, N, acc, r_acc, prot, [r_q] + r_vat)
                    self.attn_finish(acc, r_acc, N, ys[:, q0:q0 + N], r_ys, recrot)
                ncol = T if not last else TL
                S.dma("sp", lambda e, ys=ys, h=h, ncol=ncol: e.dma_start(out=self.YT[1024 + h * 64:1024 + (h + 1) * 64, 0:ncol], in_=ys[:, 0:ncol]),
                      reads=[r_ys], writes=[self.rYT])
            self.end_phase()

    def phase_na(self, l, last):
        nc, S = self.nc, self.S
        qblocks = [(0, 4, 1, [0, 2, 4, 6])]
        for r0 in range(4, 60, 8):
            qblocks.append((r0, 8, 0, list(range(r0 - 4, r0 + 12, 2))))
        qblocks.append((60, 1, 0, [56, 58, 60, 62]))
        qblocks.append((61, 3, 1, [56, 58, 60, 62]))
        with ExitStack() as st:
            qkrot = self.rot_sb(st, "nqk", [128, 2, T], BF16, 2)
            qzrot = self.rot_sb(st, "nqz", [128, 2, T], BF16, 2)
            varot = self.rot_sb(st, "nva", [128, NT, 2, 128], BF16, 2)
            tabrot = self.rot_sb(st, "ntab", [128, 2, 2, NJ * 64], BF16, 2)
            prot = self.rot_sb(st, "npe", [128, 512], BF16, 5)
            addrot = self.rot_sb(st, "nad", [128, 512], F32, 4)
            recrot = self.rot_sb(st, "nrec", [64, 512], F32, 2)
            ysrot = self.rot_sb(st, "nys", [64, T], BF16, 2)
            for c in range(4):
                qk, r_qk = qkrot.next()
                va, r_va = varot.next()
                tab, r_tab = tabrot.next()
                S.dma("sp", lambda e, qk=qk, c=c: e.dma_start(out=qk[:, 0, :], in_=self.FT[1536 + c * 128:1536 + (c + 1) * 128, :]), reads=[self.rFT], writes=[r_qk])
                S.dma("act", lambda e, qk=qk, c=c: e.dma_start(out=qk[:, 1, :], in_=self.FT[2048 + c * 128:2048 + (c + 1) * 128, :]), reads=[self.rFT], writes=[r_qk])
                S.op("pool", lambda e, va=va: e.memset(va[:, :, :, 64:128], 1.0), writes=[r_va])
                qz, r_qz = qzrot.next()
                S.op("pool", lambda e, qz=qz: e.memset(qz[:], 0.0), writes=[r_qz])
                S.op("dve", lambda e, qz=qz, qk=qk: e.tensor_copy(out=qz[0:64, 0, :], in_=qk[0:64, 0, :]), reads=[r_qk, r_qz], writes=[r_qz])
                S.op("pool", lambda e, qz=qz, qk=qk: e.tensor_copy(out=qz[64:128, 1, :], in_=qk[64:128, 0, :]), reads=[r_qk, r_qz], writes=[r_qz])
                r_vt = [Res() for _ in range(NT)]
                for i in range(NT):
                    S.dma("sp" if i % 2 == 0 else "act", lambda e, va=va, i=i, c=c: e.dma_start(
                        out=va[:, i, :, 0:64], in_=self.TM[i * 128:(i + 1) * 128, c * 128:(c + 1) * 128].rearrange("p (g d) -> p g d", g=2)),
                        reads=[self.rTM, r_va], writes=[r_vt[i]])
                for v in range(2):
                    S.dma("pool", lambda e, tab=tab, v=v, c=c: e.dma_start(out=tab[:, v, :, :], in_=self.natab[l, v, :, 2 * c:2 * c + 2, :]), writes=[r_tab])
                for hh in range(2):
                    h = 2 * c + hh
                    ps_ = slice(hh * 64, (hh + 1) * 64)
                    ys, r_ys = ysrot.next()
                    for (r0, R, v, krows) in qblocks:
                        N = 64 * R
                        q0 = r0 * 64
                        chunks = [kr // 2 for kr in krows] + [32, 33]

                        def tab_fn(s, r0=r0, v=v, N=N, tab=tab, hh=hh):
                            if s >= 32:
                                return None
                            j0 = r0 - 2 * s + JOFF
                            return tab[:, v, hh, j0 * 64:j0 * 64 + N]
                        acc, r_acc = self.pB.next()
                        self.attn_block(lambda s, qk=qk: qk[:, 1, s * 128:(s + 1) * 128], qz[:, hh, q0:q0 + N],
                                        lambda s, va=va, hh=hh: va[:, s, hh, :], chunks, N, acc, r_acc, prot, [r_qk, r_qz, r_va, r_tab] + r_vt,
                                        tab_fn=tab_fn, addrot=addrot)
                        self.attn_finish(acc, r_acc, N, ys[:, q0:q0 + N], r_ys, recrot)
                    if not last:
                        acc, r_acc = self.pB.next()
                        self.attn_block(lambda s, qk=qk: qk[:, 1, s * 128:(s + 1) * 128], qz[:, hh, TL:T],
                                        lambda s, va=va, hh=hh: va[:, s, hh, :], [32, 33], TC, acc, r_acc, prot, [r_qk, r_qz, r_va] + r_vt)
                        self.attn_finish(acc, r_acc, TC, ys[:, TL:T], r_ys, recrot)
                    ncol = T if not last else TL
                    S.dma("sp", lambda e, ys=ys, h=h, ncol=ncol: e.dma_start(out=self.YT[512 + h * 64:512 + (h + 1) * 64, 0:ncol], in_=ys[:, 0:ncol]),
                          reads=[r_ys], writes=[self.rYT])
            self.end_phase()

    def phase_merge(self, l):
        nc, S = self.nc, self.S
        ntok = self.nt_act * 128
        with ExitStack() as st:
            WBR = self.sb(st, "WBR", [128, 12, D], BF16)
            WO = self.sb(st, "WO", [128, 8, D], BF16)
            r_w = Res()
            for b in range(3):
                S.dma("pool", lambda e, b=b: e.dma_start(out=WBR[:, b * 4:(b + 1) * 4, :], in_=self.w_br[l, b].rearrange("(k p) f -> p k f", p=128)), writes=[r_w])
            S.dma("pool", lambda e: e.dma_start(out=WO[:], in_=self.w_out[l].rearrange("(k p) f -> p k f", p=128)), writes=[r_w])
            GT = self.sb(st, "GT1", [128, 2, D], F32)
            r_gt = Res()
            for j in range(2):
                self.load_bc("sp", GT[:, j, :], r_gt, self.MOD[l, j:j + 1, 2 * D:3 * D])
            ytrot = self.rot_sb(st, "mYT", [128, 12, 512], BF16, 2)
            sgrot = self.rot_sb(st, "mSG", [128, 24, 512], BF16, 2)
            mrot = self.rot_sb(st, "mT", [128, 8, 512], BF16, 2)
            m0rot = self.rot_sb(st, "m0", [128, 512], F32, 2)
            m1rot = self.rot_sb(st, "m1", [128, 512], F32, 2)
            m2rot = self.rot_sb(st, "m2", [128, 512], F32, 2)
            xrot = self.rot_sb(st, "mx", [128, D], F32, 2)
            xorot = self.rot_sb(st, "mxo", [128, D], F32, 2)
            ytv = self.YT.rearrange("(j p) t -> p j t", p=128)
            sgv = self.FT[3840:INC, :].rearrange("(j p) t -> p j t", p=128)
            for (t0, tw) in ntiles512(ntok):
                yt, r_yt = ytrot.next()
                sg, r_sg = sgrot.next()
                S.dma("sp", lambda e, yt=yt, t0=t0, tw=tw: e.dma_start(out=yt[:, :, 0:tw], in_=ytv[:, :, t0:t0 + tw]), reads=[self.rYT], writes=[r_yt])
                S.dma("act", lambda e, sg=sg, t0=t0, tw=tw: e.dma_start(out=sg[:, :, 0:tw], in_=sgv[:, :, t0:t0 + tw]), reads=[self.rFT], writes=[r_sg])
                mT, r_m = mrot.next()
                for f in range(8):
                    pb = []
                    for b in range(3):
                        pt, r_p = self.pA.next()
                        for kc in range(4):
                            S.op("pe", lambda e, pt=pt, b=b, kc=kc, f=f, yt=yt, tw=tw: e.matmul(
                                pt[:, 0:tw], lhsT=WBR[:, b * 4 + kc, f * 128:(f + 1) * 128], rhs=yt[:, b * 4 + kc, 0:tw], start=(kc == 0), stop=(kc == 3)),
                                reads=[r_w, r_yt], writes=[r_p], signal=(kc == 3))
                        pb.append((pt, r_p))
                    a0, r_a0 = m0rot.next()
                    a1, r_a1 = m1rot.next()
                    a2, r_a2 = m2rot.next()
                    for b, (a, r_a) in enumerate(((a0, r_a0), (a1, r_a1), (a2, r_a2))):
                        pt, r_p = pb[b]
                        S.op("dve", lambda e, a=a, pt=pt, sg=sg, b=b, f=f, tw=tw: e.tensor_tensor(out=a[:, 0:tw], in0=pt[:, 0:tw], in1=sg[:, b * 8 + f, 0:tw], op=ALU.mult),
                             reads=[r_p, r_sg], writes=[r_a])
                    S.op("pool", lambda e, a0=a0, a1=a1, tw=tw: e.tensor_tensor(out=a0[:, 0:tw], in0=a0[:, 0:tw], in1=a1[:, 0:tw], op=ALU.add),
                         reads=[r_a0, r_a1], writes=[r_a0])
                    S.op("pool", lambda e, a0=a0, a2=a2, mT=mT, f=f, tw=tw: e.tensor_tensor(out=mT[:, f, 0:tw], in0=a0[:, 0:tw], in1=a2[:, 0:tw], op=ALU.add),
                         reads=[r_a0, r_a2], writes=[r_m])
                for ts in range(tw // 128):
                    i = t0 // 128 + ts
                    j = 0 if i < 32 else 1
                    xt, r_x = xrot.next()
                    S.dma("sp", lambda e, xt=xt, i=i: e.dma_start(out=xt[:], in_=self.Xsrc[i * 128:(i + 1) * 128, :]), reads=[self.rX[i]], writes=[r_x])
                    xo, r_xo = xorot.next()
                    for half in range(2):
                        pt, r_p = self.pA.next()
                        for f in range(8):
                            S.op("pe", lambda e, pt=pt, f=f, mT=mT, ts=ts, half=half: e.matmul(
                                pt[:, :], lhsT=mT[:, f, ts * 128:(ts + 1) * 128], rhs=WO[:, f, half * 512:(half + 1) * 512], start=(f == 0), stop=(f == 7)),
                                reads=[r_w, r_m], writes=[r_p], signal=(f == 7))
                        hs = slice(half * 512, (half + 1) * 512)
                        S.op("dve", lambda e, pt=pt, xo=xo, hs=hs, j=j: e.tensor_tensor(out=xo[:, hs], in0=pt[:, :], in1=GT[:, j, hs], op=ALU.mult),
                             reads=[r_p, r_gt], writes=[r_xo])
                    S.op("pool", lambda e, xo=xo, xt=xt: e.tensor_tensor(out=xo[:], in0=xo[:], in1=xt[:], op=ALU.add), reads=[r_xo, r_x], writes=[r_xo])
                    S.dma("sp", lambda e, xo=xo, i=i: e.dma_start(out=self.X[i * 128:(i + 1) * 128, :], in_=xo[:]), reads=[r_xo], writes=[self.rX[i]])
            self.end_phase()
            self.Xsrc = self.X

    def phase_route(self, l):
        nc, S = self.nc, self.S
        with ExitStack() as st:
            G2 = self.sb(st, "G2", [128, 2, D], F32)
            SH2 = self.sb(st, "SH2", [128, 2, D], F32)
            NG = self.sb(st, "NG2", [128, D], F32)
            r_g = Res()
            self.load_bc("sp", NG[:], r_g, self.norm2_g[l:l + 1, :])
            for j in range(2):
                self.load_bc("sp", SH2[:, j, :], r_g, self.MOD[l, j:j + 1, 3 * D:4 * D])
                self.load_bc("act", G2[:, j, :], r_g, self.MOD[l, j:j + 1, 4 * D:5 * D])
            for j in range(2):
                S.op("dve", lambda e, j=j: e.scalar_tensor_tensor(out=G2[:, j, :], in0=G2[:, j, :], scalar=1.0, in1=NG[:], op0=ALU.add, op1=ALU.mult),
                     reads=[r_g], writes=[r_g])
            RW = self.sb(st, "RW", [128, 8, NE], F32)
            RB = self.sb(st, "RB", [128, NE], F32)
            S.dma("sp", lambda e: e.dma_start(out=RW[:], in_=self.router_w[l].rearrange("(k p) e -> p k e", p=128)), writes=[r_g])
            self.load_bc("sp", RB[:], r_g, self.router_b[l:l + 1, :])
            UTf = self.sb(st, "UTf", [128, 128], F32)
            UT = self.sb(st, "UT", [128, 128], BF16)
            ONES = self.sb(st, "ONES", [128, 128], BF16)
            EB = self.sb(st, "EB", [128, NE], F32)
            CNT = self.sb(st, "CNT", [128, NE], F32)
            r_c = Res()
            r_cnt = Res()
            S.op("pool", lambda e: e.memset(UTf[:], 1.0), writes=[r_c])
            S.op("pool", lambda e: e.affine_select(out=UTf[:], in_=UTf[:], pattern=[[1, 128]], compare_op=ALU.is_gt, fill=0.0, base=0, channel_multiplier=-1),
                 reads=[r_c], writes=[r_c])
            S.op("dve", lambda e: e.tensor_copy(out=UT[:], in_=UTf[:]), reads=[r_c], writes=[r_c])
            S.op("pool", lambda e: e.memset(ONES[:], 1.0), writes=[r_c])
            S.op("pool", lambda e: e.iota(EB[:], pattern=[[CAP, NE]], base=0, channel_multiplier=0, allow_small_or_imprecise_dtypes=True), writes=[r_c])
            S.op("pool", lambda e: e.memset(CNT[:], 0.0), writes=[r_cnt])
            xrot = self.rot_sb(st, "rx", [128, D], F32, 3)
            hrot = self.rot_sb(st, "rh", [128, D], F32, 3)
            hbrot = self.rot_sb(st, "rhb", [128, D], BF16, 6)
            htrot = self.rot_sb(st, "rht", [128, 8, 128], F32, 3)
            strot = self.stat_tiles(st, "rst", 3)
            smrot = Rot([({n: self.sb(st, "rs%s%d" % (n, i), [128, w], dt) for n, w, dt in (
                ("lg", NE, F32), ("t8", 8, F32), ("nm", 1, F32), ("e4", 4, F32), ("sm", 1, F32), ("mk", NE, BF16),
                ("pos", NE, F32), ("oh", NE, F32), ("df", 4, F32))}, Res()) for i in range(4)])
            for i in range(self.nt_act):
                j = 0 if i < 32 else 1
                xt, r_x = xrot.next()
                S.dma("sp", lambda e, xt=xt, i=i: e.dma_start(out=xt[:], in_=self.X[i * 128:(i + 1) * 128, :]), reads=[self.rX[i]], writes=[r_x])
                stt = strot.next()
                rstd, r_s = self.rms_rstd(stt, xt, r_x, D)
                h2, r_h = hrot.next()
                S.op("dve", lambda e, h2=h2, xt=xt, rstd=rstd, j=j: e.scalar_tensor_tensor(out=h2[:], in0=xt[:], scalar=rstd[:, 0:1], in1=G2[:, j, :], op0=ALU.mult, op1=ALU.mult),
                     reads=[r_x, r_s, r_g], writes=[r_h])
                S.op("pool", lambda e, h2=h2, j=j: e.tensor_tensor(out=h2[:], in0=h2[:], in1=SH2[:, j, :], op=ALU.add), reads=[r_h, r_g], writes=[r_h])
                hb, r_hb = hbrot.next()
                S.op("act", lambda e, hb=hb, h2=h2: e.activation(out=hb[:], in_=h2[:], func=AF.Copy), reads=[r_h], writes=[r_hb])
                ht, r_ht = htrot.next()
                for half in range(2):
                    pt, r_p = self.pA.next()
                    for kk in range(4):
                        k = half * 4 + kk
                        S.op("pe", lambda e, pt=pt, h2=h2, k=k, kk=kk: e.transpose(out=pt[:, kk * 128:(kk + 1) * 128], in_=h2[:, k * 128:(k + 1) * 128], identity=self.identf[:]),
                             reads=[r_h, self.r_id], writes=[r_p], signal=(kk == 3))
                    if half == 0:
                        S.op("act", lambda e, pt=pt, ht=ht: e.activation(out=ht[:, 0:4, :], in_=pt[:, :].rearrange("p (k t) -> p k t", k=4), func=AF.Copy), reads=[r_p], writes=[r_ht])
                    else:
                        S.op("dve", lambda e, pt=pt, ht=ht: e.tensor_copy(out=ht[:, 4:8, :], in_=pt[:, :].rearrange("p (k t) -> p k t", k=4)), reads=[r_p, r_ht], writes=[r_ht])
                pl, r_pl = self.pB.next()
                for k in range(8):
                    S.op("pe", lambda e, pl=pl, ht=ht, k=k: e.matmul(pl[:, 0:NE], lhsT=ht[:, k, :], rhs=RW[:, k, :], start=(k == 0), stop=(k == 7)),
                         reads=[r_ht, r_g], writes=[r_pl], signal=(k == 7))
                sm, r_sm = smrot.next()
                S.op("dve", lambda e, sm=sm, pl=pl: e.tensor_tensor(out=sm["lg"][:], in0=pl[:, 0:NE], in1=RB[:], op=ALU.add), reads=[r_pl, r_g], writes=[r_sm])
                S.op("dve", lambda e, sm=sm: e.max(out=sm["t8"][:], in_=sm["lg"][:]), reads=[r_sm], writes=[r_sm])
                S.op("dve", lambda e, sm=sm: e.tensor_scalar(out=sm["nm"][:], in0=sm["t8"][:, 0:1], scalar1=-1.0, scalar2=None, op0=ALU.mult), reads=[r_sm], writes=[r_sm])
                S.op("act", lambda e, sm=sm: e.activation(out=sm["e4"][:], in_=sm["t8"][:, 0:4], func=AF.Exp, bias=sm["nm"][:, 0:1], accum_out=sm["sm"][:, 0:1]), reads=[r_sm], writes=[r_sm])
                S.op("dve", lambda e, sm=sm: e.reciprocal(out=sm["sm"][:], in_=sm["sm"][:]), reads=[r_sm], writes=[r_sm])
                S.op("dve", lambda e, sm=sm, i=i: e.tensor_scalar(out=self.GATES[:, i, :], in0=sm["e4"][:], scalar1=sm["sm"][:, 0:1], scalar2=None, op0=ALU.mult),
                     reads=[r_sm], writes=[self.rROUTE[i]])
                S.op("dve", lambda e, sm=sm: e.tensor_scalar(out=sm["mk"][:], in0=sm["lg"][:], scalar1=sm["t8"][:, 3:4], scalar2=None, op0=ALU.is_ge), reads=[r_sm], writes=[r_sm])
                pp, r_pp = self.pB.next()
                S.op("pe", lambda e, pp=pp, sm=sm: e.matmul(pp[:, 0:NE], lhsT=UT[:], rhs=sm["mk"][:], start=True, stop=True), reads=[r_sm, r_c], writes=[r_pp], signal=False)
                S.op("pe", lambda e, pp=pp, sm=sm: e.matmul(pp[:, 64:64 + NE], lhsT=ONES[:], rhs=sm["mk"][:], start=True, stop=True), reads=[r_sm, r_c], writes=[r_pp])
                S.op("dve", lambda e, sm=sm, pp=pp: e.tensor_tensor(out=sm["pos"][:], in0=pp[:, 0:NE], in1=CNT[:], op=ALU.add), reads=[r_pp, r_cnt, r_sm], writes=[r_sm])
                S.op("dve", lambda e, pp=pp: e.tensor_tensor(out=CNT[:], in0=pp[:, 64:64 + NE], in1=CNT[:], op=ALU.add), reads=[r_pp, r_cnt], writes=[r_cnt])
                S.op("dve", lambda e, sm=sm: e.scalar_tensor_tensor(out=sm["pos"][:], in0=sm["pos"][:], scalar=float(CAP - 1), in1=EB[:], op0=ALU.min, op1=ALU.add),
                     reads=[r_sm, r_c], writes=[r_sm])
                for k in range(4):
                    S.op("dve", lambda e, sm=sm, k=k: e.scalar_tensor_tensor(out=sm["oh"][:], in0=sm["lg"][:], scalar=sm["t8"][:, k:k + 1], in1=sm["pos"][:], op0=ALU.is_equal, op1=ALU.mult),
                         reads=[r_sm], writes=[r_sm])
                    S.op("dve", lambda e, sm=sm, k=k: e.reduce_sum(out=sm["df"][:, k:k + 1], in_=sm["oh"][:], axis=AX.X), reads=[r_sm], writes=[r_sm])
                S.op("dve", lambda e, sm=sm, i=i: e.tensor_copy(out=self.DEST[:, i, :], in_=sm["df"][:]), reads=[r_sm, self.rROUTE[i]], writes=[self.rROUTE[i]])
                for k in range(4):
                    S.dma("pool", lambda e, hb=hb, i=i, k=k: e.indirect_dma_start(
                        out=self.XE[:, :], out_offset=bass.IndirectOffsetOnAxis(ap=self.DEST[:, i, k:k + 1], axis=0), in_=hb[:], in_offset=None),
                        reads=[r_hb, self.rROUTE[i]], writes=[self.rXE])
            S.op("dve", lambda e: e.tensor_copy(out=self.CNTI[:], in_=CNT[:]), reads=[r_cnt], writes=[r_cnt])
            self.end_phase()

    def phase_experts(self, l):
        nc, S = self.nc, self.S
        NTE = CAP // 512
        with ExitStack() as st:
            xe0rot = self.rot_sb(st, "xe0", [128, 4, D], BF16, 2)
            xeCrot = self.rot_sb(st, "xeC", [128, 2, D], BF16, 2)
            xtrot = self.rot_sb(st, "xeT", [128, 8, 512], BF16, 2)
            wgrot = self.rot_sb(st, "wg", [128, 8, D], BF16, 2)
            wurot = self.rot_sb(st, "wu", [128, 8, D], BF16, 2)
            wdrot = self.rot_sb(st, "wd", [128, 8, D], BF16, 1)
            stgrot = self.rot_sb(st, "wstg", [128, 2, 1024], F32, 3)
            bgrot = self.rot_sb(st, "bgu", [128, 8, 2], F32, 2)
            bdrot = self.rot_sb(st, "bd", [128, D], F32, 2)
            atrot = self.rot_sb(st, "aT", [128, 8, 512], BF16, 2)
            gsrot = self.rot_sb(st, "gs", [128, 512], F32, 2)
            sgrot = self.rot_sb(st, "sg", [128, 512], F32, 2)
            usrot = self.rot_sb(st, "us", [128, 512], F32, 2)
            yrot = self.rot_sb(st, "ye", [128, D], F32, 2)
            def load_xe(buf, row0, nsub):
                t_, r_ = buf
                S.dma("sp", lambda e: e.dma_start(out=t_[:, 0:nsub, :], in_=self.XE[row0:row0 + nsub * 128, :].rearrange("(j p) d -> p j d", p=128)),
                      reads=[self.rXE], writes=[r_])

            x0_cur = xe0rot.next()
            load_xe(x0_cur, 0, 4)
            for e_ in range(NE):
                wg, r_wg = wgrot.next()
                wu, r_wu = wurot.next()
                wd, r_wd = wdrot.next()
                wguv = self.w_gu[l, e_].rearrange("(k p) c -> p k c", p=128)
                for kh in range(4):
                    for q in range(2):
                        sgt, r_st = stgrot.next()
                        S.dma("sp" if (kh * 2 + q) % 2 == 0 else "act", lambda e, sgt=sgt, q=q, kh=kh, wguv=wguv: e.dma_start(
                            out=sgt[:], in_=wguv[:, kh * 2:(kh + 1) * 2, q * 1024:(q + 1) * 1024]), writes=[r_st])
                        sv = sgt[:].rearrange("p k (c two) -> p k c two", two=2)
                        S.op("pool", lambda e, wg=wg, sv=sv, q=q, kh=kh: e.tensor_copy(out=wg[:, kh * 2:(kh + 1) * 2, q * 512:(q + 1) * 512], in_=sv[:, :, :, 0]),
                             reads=[r_st], writes=[r_wg])
                        S.op("act", lambda e, wu=wu, sv=sv, q=q, kh=kh: e.activation(out=wu[:, kh * 2:(kh + 1) * 2, q * 512:(q + 1) * 512], in_=sv[:, :, :, 1], func=AF.Copy),
                             reads=[r_st], writes=[r_wu])
                S.dma("pool", lambda e, wd=wd, e_=e_: e.dma_start(out=wd[:], in_=self.w_down[l, e_].rearrange("(k p) c -> p k c", p=128)), writes=[r_wd])
                bg, r_bg = bgrot.next()
                S.dma("sp", lambda e, bg=bg, e_=e_: e.dma_start(out=bg[:], in_=self.b_gu[l, e_, :].rearrange("(k p two) -> p k two", p=128, two=2),
                                                               allow_slow_non_contiguous=True), writes=[r_bg])
                bd, r_bd = bdrot.next()
                S.dma("act", lambda e, bd=bd, e_=e_: e.dma_start(out=bd[:], in_=self.b_down[l, e_:e_ + 1, :].to_broadcast([128, D])), writes=[r_bd])
                S.load_count(self.CNTI[0:1, e_:e_ + 1])
                args = ((wg, r_wg), (wu, r_wu), (wd, r_wd), (bg, r_bg), (bd, r_bd), xtrot, atrot, gsrot, sgrot, usrot, yrot)
                x0_next = None
                if e_ + 1 < NE:
                    x0_next = xe0rot.next()
                    load_xe(x0_next, (e_ + 1) * CAP, 4)
                xc_cur = xeCrot.next()
                load_xe(xc_cur, e_ * CAP + 512, 2)
                self.expert_ntile(e_ * CAP, 4, x0_cur, *args)
                regs = list(range(512, CAP, 256))
                for ri, r in enumerate(regs):
                    S.cond_begin(r)
                    xc_next = None
                    if ri + 1 < len(regs):
                        xc_next = xeCrot.next()
                        load_xe(xc_next, e_ * CAP + regs[ri + 1], 2)
                    self.expert_ntile(e_ * CAP + r, 2, xc_cur, *args)
                    S.cond_end()
                    if xc_next is not None:
                        xc_cur = xc_next
                x0_cur = x0_next
            self.end_phase()

    def expert_ntile(self, row0, nsub, xe_, wg_, wu_, wd_, bg_, bd_, xtrot, atrot, gsrot, sgrot, usrot, yrot):
        S = self.S
        W = nsub * 128
        wg, r_wg = wg_
        wu, r_wu = wu_
        wd, r_wd = wd_
        bg, r_bg = bg_
        bd, r_bd = bd_
        xe, r_xe = xe_
        xT, r_xT = xtrot.next()
        for jt in range(nsub):
            pt, r_p = self.pT.next()
            for k in range(8):
                S.op("pe", lambda e, pt=pt, jt=jt, k=k: e.transpose(out=pt[:, k * 128:(k + 1) * 128], in_=xe[:, jt, k * 128:(k + 1) * 128], identity=self.identb[:]),
                     reads=[r_xe, self.r_id], writes=[r_p], signal=(k == 7))
            S.op("dve", lambda e, pt=pt, jt=jt: e.tensor_copy(out=xT[:, :, jt * 128:(jt + 1) * 128], in_=pt[:, :].rearrange("p (k t) -> p k t", k=8)),
                 reads=[r_p], writes=[r_xT])
        aT, r_aT = atrot.next()
        for fk in range(8):
            pg, r_pg = self.pA.next()
            pu, r_pu = self.pA.next()
            for k in range(8):
                S.op("pe", lambda e, pg=pg, k=k, fk=fk: e.matmul(pg[:, 0:W], lhsT=wg[:, k, fk * 128:(fk + 1) * 128], rhs=xT[:, k, 0:W], start=(k == 0), stop=(k == 7)),
                     reads=[r_wg, r_xT], writes=[r_pg], signal=(k == 7))
            for k in range(8):
                S.op("pe", lambda e, pu=pu, k=k, fk=fk: e.matmul(pu[:, 0:W], lhsT=wu[:, k, fk * 128:(fk + 1) * 128], rhs=xT[:, k, 0:W], start=(k == 0), stop=(k == 7)),
                     reads=[r_wu, r_xT], writes=[r_pu], signal=(k == 7))
            gs, r_gs = gsrot.next()
            sg, r_sg = sgrot.next()
            us, r_us = usrot.next()
            S.op("dve", lambda e, gs=gs, pg=pg, fk=fk: e.tensor_scalar(out=gs[:, 0:W], in0=pg[:, 0:W], scalar1=bg[:, fk, 0:1], scalar2=7.0, op0=ALU.add, op1=ALU.min),
                 reads=[r_pg, r_bg], writes=[r_gs])
            S.op("act", lambda e, sg=sg, gs=gs: e.activation(out=sg[:, 0:W], in_=gs[:, 0:W], func=AF.Sigmoid, scale=1.702), reads=[r_gs], writes=[r_sg])
            S.op("dve", lambda e, us=us, pu=pu, fk=fk: e.tensor_scalar(out=us[:, 0:W], in0=pu[:, 0:W], scalar1=bg[:, fk, 1:2], scalar2=7.0, op0=ALU.add, op1=ALU.min),
                 reads=[r_pu, r_bg], writes=[r_us])
            S.op("dve", lambda e, us=us: e.tensor_scalar(out=us[:, 0:W], in0=us[:, 0:W], scalar1=-7.0, scalar2=1.0, op0=ALU.max, op1=ALU.add),
                 reads=[r_us], writes=[r_us])
            S.op("pool", lambda e, gs=gs, sg=sg: e.tensor_tensor(out=gs[:, 0:W], in0=gs[:, 0:W], in1=sg[:, 0:W], op=ALU.mult), reads=[r_gs, r_sg], writes=[r_gs])
            S.op("dve", lambda e, us=us, gs=gs, fk=fk: e.tensor_tensor(out=aT[:, fk, 0:W], in0=us[:, 0:W], in1=gs[:, 0:W], op=ALU.mult),
                 reads=[r_us, r_gs], writes=[r_aT])
        for jt in range(nsub):
            ye, r_ye = yrot.next()
            for half in range(2):
                pt, r_p = self.pA.next()
                for fk in range(8):
                    S.op("pe", lambda e, pt=pt, fk=fk, jt=jt, half=half: e.matmul(
                        pt[:, :], lhsT=aT[:, fk, jt * 128:(jt + 1) * 128], rhs=wd[:, fk, half * 512:(half + 1) * 512], start=(fk == 0), stop=(fk == 7)),
                        reads=[r_aT, r_wd], writes=[r_p], signal=(fk == 7))
                hs = slice(half * 512, (half + 1) * 512)
                S.op("dve", lambda e, ye=ye, pt=pt, hs=hs: e.tensor_tensor(out=ye[:, hs], in0=pt[:, :], in1=bd[:, hs], op=ALU.add),
                     reads=[r_p, r_bd], writes=[r_ye])
            S.dma("sp", lambda e, ye=ye, jt=jt: e.dma_start(out=self.YE[row0 + jt * 128:row0 + (jt + 1) * 128, :], in_=ye[:]),
                  reads=[r_ye], writes=[self.rYE])

    def phase_combine(self, l, last):
        nc, S = self.nc, self.S
        with ExitStack() as st:
            GT = self.sb(st, "GT2", [128, 2, D], F32)
            r_gt = Res()
            for j in range(2):
                self.load_bc("sp", GT[:, j, :], r_gt, self.MOD[l, j:j + 1, 5 * D:6 * D])
            if last:
                FG = self.sb(st, "FG", [128, D], F32)
                self.load_bc("sp", FG[:], r_gt, self.final_g[0:1, :])
            ykrot = self.rot_sb(st, "yk", [128, 4, D], F32, 4)
            accrot = self.rot_sb(st, "kacc", [128, D], F32, 3)
            xrot = self.rot_sb(st, "kx", [128, D], F32, 3)
            orot = self.rot_sb(st, "ko", [128, D], F32, 3)
            strot = self.stat_tiles(st, "kst")
            for i in range(self.nt_act):
                j = 0 if i < 32 else 1
                yk, r_yk = ykrot.next()
                for k in range(4):
                    S.dma("pool", lambda e, yk=yk, i=i, k=k: e.indirect_dma_start(
                        out=yk[:, k, :], out_offset=None, in_=self.YE[:, :], in_offset=bass.IndirectOffsetOnAxis(ap=self.DEST[:, i, k:k + 1], axis=0)),
                        reads=[self.rYE, self.rROUTE[i]], writes=[r_yk])
                xt, r_x = xrot.next()
                S.dma("sp", lambda e, xt=xt, i=i: e.dma_start(out=xt[:], in_=self.X[i * 128:(i + 1) * 128, :]), reads=[self.rX[i]], writes=[r_x])
                acc, r_a = accrot.next()
                S.op("dve", lambda e, acc=acc, yk=yk, i=i: e.tensor_scalar(out=acc[:], in0=yk[:, 0, :], scalar1=self.GATES[:, i, 0:1], scalar2=None, op0=ALU.mult),
                     reads=[r_yk, self.rROUTE[i]], writes=[r_a])
                for k in range(1, 4):
                    S.op("dve", lambda e, acc=acc, yk=yk, i=i, k=k: e.scalar_tensor_tensor(out=acc[:], in0=yk[:, k, :], scalar=self.GATES[:, i, k:k + 1], in1=acc[:], op0=ALU.mult, op1=ALU.add),
                         reads=[r_yk, self.rROUTE[i], r_a], writes=[r_a])
                S.op("pool", lambda e, acc=acc, j=j: e.tensor_tensor(out=acc[:], in0=acc[:], in1=GT[:, j, :], op=ALU.mult), reads=[r_a, r_gt], writes=[r_a])
                xo, r_xo = orot.next()
                S.op("pool", lambda e, xo=xo, acc=acc, xt=xt: e.tensor_tensor(out=xo[:], in0=acc[:], in1=xt[:], op=ALU.add), reads=[r_a, r_x], writes=[r_xo])
                if not last:
                    S.dma("sp", lambda e, xo=xo, i=i: e.dma_start(out=self.X[i * 128:(i + 1) * 128, :], in_=xo[:]), reads=[r_xo], writes=[self.rX[i]])
                else:
                    stt = strot.next()
                    rstd, r_s = self.rms_rstd(stt, xo, r_xo, D)
                    S.op("dve", lambda e, xo=xo, rstd=rstd: e.scalar_tensor_tensor(out=xo[:], in0=xo[:], scalar=rstd[:, 0:1], in1=FG[:], op0=ALU.mult, op1=ALU.mult),
                         reads=[r_xo, r_s, r_gt], writes=[r_xo])
                    S.dma("sp", lambda e, xo=xo, i=i: e.dma_start(out=self.out[i * 128:(i + 1) * 128, :], in_=xo[:]), reads=[r_xo], writes=[self.rOUT])
            self.end_phase()


def _na_table(rpb):
    a = np.arange(2)[:, None, None, None]
    kc = np.arange(64)[None, :, None, None]
    j = np.arange(NJ)[None, None, :, None]
    qc = np.arange(64)[None, None, None, :]
    dr = a - (j - JOFF) + 0 * kc + 0 * qc
    cs = np.clip(qc - 8, 0, 48)
    colv = (kc >= cs) & (kc < cs + 16)
    dc = kc - qc + 0 * a + 0 * j
    out = np.empty((2, 128, 8, NJ * 64), np.float32)
    for v in range(2):
        rowv = ((dr >= -4) & (dr <= 3)) if v == 0 else ((dr >= -7) & (dr <= 7))
        valid = np.broadcast_to(rowv & colv, (2, 64, NJ, 64))
        ri = np.clip(dr + 7, 0, 14)
        ci = np.clip(dc + 15, 0, 30)
        ri = np.broadcast_to(ri, (2, 64, NJ, 64))
        ci = np.broadcast_to(ci, (2, 64, NJ, 64))
        for h in range(8):
            vals = rpb[h][ri, ci]
            tbl = np.where(valid, vals, np.float32(NEG)).astype(np.float32)
            out[v, :, h, :] = tbl.reshape(128, NJ * 64)
    return out


def _rope_tables():
    half = 32
    inv = (10000.0 ** (-np.arange(0, half, 2, dtype=np.float32) / half)).astype(np.float32)
    t = np.arange(TL)
    ang_r = (t // 64).astype(np.float32)[:, None] * inv
    ang_c = (t % 64).astype(np.float32)[:, None] * inv
    cos = np.zeros((TL, 64), np.float32)
    sin = np.zeros((TL, 64), np.float32)
    for base, ang in ((0, ang_r), (32, ang_c)):
        c, s = np.cos(ang).astype(np.float32), np.sin(ang).astype(np.float32)
        cos[:, base:base + 16] = c
        cos[:, base + 16:base + 32] = c
        sin[:, base:base + 16] = -s
        sin[:, base + 16:base + 32] = s
    return np.tile(cos, (1, 10)), np.tile(sin, (1, 10))


def make_in_maps(inputs, n_cores=8):
    f = lambda a: np.ascontiguousarray(np.asarray(a, dtype=np.float32))
    x, c, ctx, c_ctx = f(inputs["x"]), f(inputs["c"]), f(inputs["ctx"]), f(inputs["c_ctx"])
    natab = np.stack([_na_table(f(inputs["na_rpb"])[l]) for l in range(DEPTH)])
    qg, kg = f(inputs["q_norm_g"]), f(inputs["k_norm_g"])
    qkgain = np.concatenate([np.tile(qg, (1, 8)), np.tile(kg, (1, 2))], axis=1)
    ropec, ropes = _rope_tables()
    w_br = np.stack([f(inputs["w_br_conv"]), f(inputs["w_br_na"]), f(inputs["w_br_gqa"])], axis=1)
    shared = dict(
        ada_w=f(inputs["ada_w"]), ada_b=f(inputs["ada_b"]), norm1_g=f(inputs["norm1_g"]), norm2_g=f(inputs["norm2_g"]),
        w_in=f(inputs["w_in"]), conv_w=f(inputs["conv_w"]), natab=natab, qkgain=np.ascontiguousarray(qkgain),
        ropec=ropec, ropes=ropes, w_br=np.ascontiguousarray(w_br), w_out=f(inputs["w_out"]),
        router_w=f(inputs["router_w"]), router_b=f(inputs["router_b"]), w_gu=f(inputs["w_gu"]), b_gu=f(inputs["b_gu"]),
        w_down=f(inputs["w_down"]), b_down=f(inputs["b_down"]), final_g=f(inputs["final_g"]).reshape(1, D))
    maps = []
    for b in range(n_cores):
        m = dict(shared)
        m["xin"] = np.ascontiguousarray(np.concatenate([x[b], ctx[b]], axis=0))
        m["cvec"] = np.ascontiguousarray(np.stack([c[b], c_ctx], axis=0))
        maps.append(m)
    return maps


def kernel(**inputs):
    nc = bass.Bass("TRN2", target_bir_lowering=False)
    Builder(nc).build()
    maps = make_in_maps(inputs)
    res = run_bass_kernel_spmd(nc, maps, core_ids=list(range(8)))
    return np.stack([np.asarray(r["out"], dtype=np.float32) for r in res.results], axis=0)
```

```python
import numpy as np
from contextlib import ExitStack
import concourse.bass as bass
import concourse.mybir as mybir
from concourse.bass_utils import run_bass_kernel_spmd

F32 = mybir.dt.float32
BF16 = mybir.dt.bfloat16
I32 = mybir.dt.int32
AF = mybir.ActivationFunctionType
ALU = mybir.AluOpType
AX = mybir.AxisListType

D = 1024
TL = 4096
TC = 256
T = TL + TC
NT = T // 128
DEPTH = 2
NE = 32
CAP = 2048
EPS = 1e-6
NEG = -30000.0
NJ = 22
JOFF = 10
INC = 6912
DYNAMIC_SKIP = True


class Res:
    __slots__ = ("w", "rs")

    def __init__(self):
        self.w = None
        self.rs = {}


class Sched:
    ENGS = ("pe", "act", "dve", "pool", "sp")
    NQ = 20

    def __init__(self, nc, stack, same_engine_sync=True):
        self.nc = nc
        self.eng = {"pe": nc.tensor, "act": nc.scalar, "dve": nc.vector,
                    "pool": nc.gpsimd, "sp": nc.sync}
        self.sem = {}
        self.cnt = {}
        self.seen = {e: {} for e in self.ENGS}
        self.prog = {e: [] for e in self.ENGS}
        self.same_engine_sync = same_engine_sync
        for e in self.ENGS:
            self.sem[e] = stack.enter_context(nc.semaphore("c_" + e))
            self.cnt[e] = 0
        self.dq = {}
        for q in ("sp", "act", "pool"):
            keys = []
            for i in range(self.NQ):
                k = "d_%s_%d" % (q, i)
                self.sem[k] = stack.enter_context(nc.semaphore(k))
                self.cnt[k] = 0
                keys.append(k)
            self.dq[q] = [keys, 0]

    def _deps(self, engine, reads, writes, extra=()):
        need = {}
        for r in reads:
            if r.w is not None:
                k, v = r.w
                if need.get(k, 0) < v:
                    need[k] = v
        for w in writes:
            if w.w is not None:
                k, v = w.w
                if need.get(k, 0) < v:
                    need[k] = v
            for k, v in w.rs.items():
                if need.get(k, 0) < v:
                    need[k] = v
        for k, v in extra:
            if need.get(k, 0) < v:
                need[k] = v
        out = []
        seen = self.seen[engine]
        for k, v in need.items():
            if k == engine and (engine == "pe" or not self.same_engine_sync):
                continue
            if seen.get(k, 0) >= v:
                continue
            seen[k] = v
            out.append((k, v))
        return out

    def _mark(self, ev, reads, writes):
        k, v = ev
        for r in reads:
            if r.rs.get(k, 0) < v:
                r.rs[k] = v
        for w in writes:
            w.w = ev
            w.rs = {}

    def op(self, engine, fn, reads=(), writes=(), signal=True):
        waits = self._deps(engine, reads, writes)
        sem = self.sem[engine]
        if signal:
            self.cnt[engine] += 1
        ev = (engine, self.cnt[engine] if signal else self.cnt[engine] + 1)
        sems = self.sem

        def emit(eng):
            for k, v in waits:
                eng.wait_ge(sems[k], v)
            ins = fn(eng)
            if signal:
                ins.then_inc(sem, 1)

        self.prog[engine].append(emit)
        self._mark(ev, reads, writes)
        return ev

    def dma(self, queue, fn, reads=(), writes=()):
        keys, idx = self.dq[queue]
        k = keys[idx % len(keys)]
        self.dq[queue][1] = idx + 1
        prev = self.cnt[k]
        extra = [(k, prev)] if prev > 0 else []
        waits = self._deps(queue, reads, writes, extra)
        self.cnt[k] = prev + 16
        ev = (k, prev + 16)
        sems = self.sem

        def emit(eng):
            for kk, v in waits:
                eng.wait_ge(sems[kk], v)
            fn(eng).then_inc(sems[k], 16)

        self.prog[queue].append(emit)
        self._mark(ev, reads, writes)
        return ev

    def load_count(self, ap):
        for e in self.ENGS:
            self.prog[e].append(("ldreg", ap))

    def cond_begin(self, thr):
        self._cond = dict(thr=thr, cnt=dict(self.cnt), seen={e: dict(v) for e, v in self.seen.items()},
                          dq={q: self.dq[q][1] for q in self.dq})
        for e in self.ENGS:
            self.prog[e].append(("if", thr))

    def cond_end(self):
        c = self._cond
        sems = self.sem
        for e in self.ENGS:
            delta = self.cnt[e] - c["cnt"][e]
            dl = []
            if e in self.dq:
                keys = self.dq[e][0]
                run = dict()
                for idx in range(c["dq"][e], self.dq[e][1]):
                    k = keys[idx % len(keys)]
                    prev = run.get(k, c["cnt"][k])
                    dl.append((k, prev))
                    run[k] = prev + 16

            def comp(eng, e=e, delta=delta, dl=dl):
                if e != "sp":
                    eng.drain()
                if delta > 0:
                    eng.sem_inc(sems[e], delta)
                for k, prev in dl:
                    if prev > 0:
                        eng.wait_ge(sems[k], prev)
                    eng.sem_inc(sems[k], 16)

            self.prog[e].append(("endif", comp))
        self.seen = c["seen"]
        self._cond = None

    def barrier(self):
        sems = self.sem
        for e in self.ENGS:
            waits = []
            seen = self.seen[e]
            for k, v in self.cnt.items():
                if v > 0 and seen.get(k, 0) < v:
                    seen[k] = v
                    waits.append((k, v))

            def emit(eng, waits=waits):
                for k, v in waits:
                    eng.wait_ge(sems[k], v)

            self.prog[e].append(emit)

    def _run(self, name, eng):
        items = self.prog[name]
        if not hasattr(self, "regs"):
            self.regs = {}
        i = 0
        n = len(items)
        while i < n:
            it = items[i]
            if callable(it):
                it(eng)
                i += 1
                continue
            kind = it[0]
            if kind == "ldreg":
                if name not in self.regs:
                    self.regs[name] = eng.alloc_register("cnt_" + name)
                eng.reg_load(self.regs[name], it[1])
                i += 1
            elif kind == "if":
                thr = it[1]
                j = i + 1
                while not (isinstance(items[j], tuple) and items[j][0] == "endif"):
                    j += 1
                body = items[i + 1:j]
                comp = items[j][1]
                with eng.If_lt(self.regs[name], thr + 1):
                    comp(eng)
                with eng.Else():
                    for f in body:
                        f(eng)
                i = j + 1
            else:
                raise RuntimeError("bad prog item")

    def flush(self):
        nc = self.nc
        with nc.Block() as block:
            @block.sync
            def _(e):
                self._run("sp", e)

            @block.scalar
            def _(e):
                self._run("act", e)

            @block.vector
            def _(e):
                self._run("dve", e)

            @block.gpsimd
            def _(e):
                self._run("pool", e)

            @block.tensor
            def _(e):
                self._run("pe", e)
        self.prog = {e: [] for e in self.ENGS}


class Rot:
    def __init__(self, items):
        self.items = items
        self.i = 0

    def next(self):
        it = self.items[self.i % len(self.items)]
        self.i += 1
        return it


def ntiles512(n_tok):
    out = []
    t = 0
    while t < n_tok:
        w = min(512, n_tok - t)
        out.append((t, w))
        t += w
    return out


class Builder:
    def __init__(self, nc, dbg=None, layers=(0, 1), stop_after=None):
        self.nc = nc
        self.dbg = dbg or []
        self.layers = layers
        self.stop_after = stop_after

    def sb(self, st, name, shape, dt):
        self._uid = getattr(self, "_uid", 0) + 1
        return st.enter_context(self.nc.sbuf_tensor("%s_%d" % (name, self._uid), list(shape), dt))

    def rot_sb(self, st, name, shape, dt, n):
        return Rot([(self.sb(st, "%s%d" % (name, i), shape, dt), Res()) for i in range(n)])

    def end_phase(self):
        self.S.barrier()
        self.S.flush()

    def declare(self):
        nc = self.nc
        di = lambda n, s, dt=F32: nc.dram_tensor(n, list(s), dt, kind="ExternalInput").ap()
        self.xin = di("xin", [T, D])
        self.cvec = di("cvec", [2, D])
        self.ada_w = di("ada_w", [DEPTH, D, 6 * D])
        self.ada_b = di("ada_b", [DEPTH, 6 * D])
        self.norm1_g = di("norm1_g", [DEPTH, D])
        self.norm2_g = di("norm2_g", [DEPTH, D])
        self.w_in = di("w_in", [DEPTH, D, INC])
        self.conv_w = di("conv_w", [DEPTH, 3, 512])
        self.natab = di("natab", [DEPTH, 2, 128, 8, NJ * 64])
        self.qkgain = di("qkgain", [DEPTH, 640])
        self.ropec = di("ropec", [TL, 640])
        self.ropes = di("ropes", [TL, 640])
        self.w_br = di("w_br", [DEPTH, 3, 512, D])
        self.w_out = di("w_out", [DEPTH, D, D])
        self.router_w = di("router_w", [DEPTH, D, NE])
        self.router_b = di("router_b", [DEPTH, NE])
        self.w_gu = di("w_gu", [DEPTH, NE, D, 2 * D])
        self.b_gu = di("b_gu", [DEPTH, NE, 2 * D])
        self.w_down = di("w_down", [DEPTH, NE, D, D])
        self.b_down = di("b_down", [DEPTH, NE, D])
        self.final_g = di("final_g", [1, D])
        self.out = nc.dram_tensor("out", [TL, D], F32, kind="ExternalOutput").ap()

        def scr(n, s, dt):
            kind = "ExternalOutput" if n in self.dbg else "Internal"
            return nc.dram_tensor(n, list(s), dt, kind=kind).ap()
        self.X = scr("X", [T, D], F32)
        self.MOD = scr("MOD", [DEPTH, 2, 6 * D], F32)
        self.FT = scr("FT", [INC, T], BF16)
        self.TM = scr("TM", [T, 1280], BF16)
        self.YT = scr("YT", [1536, T], BF16)
        self.XE = scr("XE", [NE * CAP, D], BF16)
        self.YE = scr("YE", [NE * CAP, D], F32)
        self.rX = [Res() for _ in range(NT)]
        self.rMOD = Res()
        self.rFT = Res()
        self.rTM = Res()
        self.rYT = Res()
        self.rXE = Res()
        self.rYE = Res()
        self.rOUT = Res()

    def build(self):
        nc = self.nc
        self.declare()
        with ExitStack() as gst:
            S = self.S = Sched(nc, gst)
            self.pA = Rot([(gst.enter_context(nc.psum_tensor("pA%d" % i, [128, 512], F32)), Res()) for i in range(4)])
            self.pB = Rot([(gst.enter_context(nc.psum_tensor("pB%d" % i, [128, 512], F32)), Res()) for i in range(2)])
            self.pT = Rot([(gst.enter_context(nc.psum_tensor("pT%d" % i, [128, 1024], BF16)), Res()) for i in range(2)])
            self.identf = self.sb(gst, "identf", [128, 128], F32)
            self.identb = self.sb(gst, "identb", [128, 128], BF16)
            self.r_id = Res()
            idf, idb = self.identf, self.identb
            S.op("pool", lambda e: e.memset(idf[:], 0.0), writes=[self.r_id])
            S.op("pool", lambda e: e.affine_select(out=idf[:], in_=idf[:], pattern=[[-1, 128]],
                                                   compare_op=ALU.not_equal, fill=1.0, base=0, channel_multiplier=1),
                 reads=[self.r_id], writes=[self.r_id])
            S.op("dve", lambda e: e.tensor_copy(out=idb[:], in_=idf[:]), reads=[self.r_id], writes=[self.r_id])
            self.DEST = self.sb(gst, "DEST", [128, NT, 4], I32)
            self.GATES = self.sb(gst, "GATES", [128, NT, 4], F32)
            self.rROUTE = [Res() for _ in range(NT)]
            self.CNTI = self.sb(gst, "CNTI", [128, NE], I32)
            self.end_phase()

            self.phase_mods()
            if self.stop_after == "mods":
                return self.finish()
            for l in self.layers:
                last = (l == DEPTH - 1)
                self.Xsrc = self.xin if l == 0 else self.X
                self.nt_act = 32 if last else NT
                self.phase_AB(l)
                if self.stop_after == "AB%d" % l:
                    return self.finish()
                self.phase_conv(l)
                self.phase_gqa(l, last)
                self.phase_na(l, last)
                if self.stop_after == "attn%d" % l:
                    return self.finish()
                self.phase_merge(l)
                if self.stop_after == "merge%d" % l:
                    return self.finish()
                self.phase_route(l)
                self.phase_experts(l)
                if self.stop_after == "exp%d" % l:
                    return self.finish()
                self.phase_combine(l, last)
                if self.stop_after == "comb%d" % l:
                    return self.finish()
            return self.finish()

    def finish(self):
        S = self.S
        S.barrier()
        S.flush()

    def phase_mods(self):
        nc, S = self.nc, self.S
        with ExitStack() as st:
            cs = self.sb(st, "cs", [128, 8, 2], F32)
            ca = self.sb(st, "ca", [128, 8, 2], F32)
            r_cs = Res()
            for j in range(2):
                S.dma("sp", lambda e, j=j: e.dma_start(out=cs[:, :, j], in_=self.cvec[j, :].rearrange("(p k) -> p k", k=8), allow_slow_non_contiguous=True),
                      writes=[r_cs])
            S.op("act", lambda e: e.activation(out=ca[:], in_=cs[:], func=AF.Silu), reads=[r_cs], writes=[r_cs])
            wrot = self.rot_sb(st, "adaw", [128, 8, 512], F32, 2)
            bias = self.sb(st, "adab", [2, 6 * D], F32)
            modsb = self.sb(st, "modsb", [2, 6 * D], F32)
            r_b = Res()
            r_m = Res()
            for l in range(DEPTH):
                S.dma("sp", lambda e, l=l: e.dma_start(out=bias[:], in_=self.ada_b[l:l + 1, :].to_broadcast([2, 6 * D])),
                      writes=[r_b])
                wv = self.ada_w[l].rearrange("(p k) f -> p k f", k=8)
                for fb in range(12):
                    wt, r_w = wrot.next()
                    S.dma("sp" if fb % 2 == 0 else "act",
                          lambda e, wt=wt, fb=fb, wv=wv: e.dma_start(out=wt[:], in_=wv[:, :, fb * 512:(fb + 1) * 512]),
                          writes=[r_w])
                    pt, r_p = self.pA.next()
                    for k in range(8):
                        S.op("pe", lambda e, pt=pt, wt=wt, k=k: e.matmul(pt[0:2, :], lhsT=ca[:, k, :], rhs=wt[:, k, :],
                                                                        start=(k == 0), stop=(k == 7)),
                             reads=[r_cs, r_w], writes=[r_p], signal=(k == 7))
                    S.op("dve", lambda e, pt=pt, fb=fb: e.tensor_tensor(out=modsb[:, fb * 512:(fb + 1) * 512], in0=pt[0:2, :],
                                                                        in1=bias[:, fb * 512:(fb + 1) * 512], op=ALU.add),
                         reads=[r_p, r_b], writes=[r_m])
                S.dma("sp", lambda e, l=l: e.dma_start(out=self.MOD[l], in_=modsb[:]), reads=[r_m], writes=[self.rMOD])
            self.end_phase()

    def load_feat(self, queue, tile, res, src_row):
        self.S.dma(queue, lambda e: e.dma_start(out=tile[:], in_=src_row.rearrange("(k p) -> p k", p=128),
                                                allow_slow_non_contiguous=True),
                   reads=[self.rMOD], writes=[res])

    def load_bc(self, queue, tile_ap, res, src_row2d, n=128):
        F = src_row2d.shape[-1]
        self.S.dma(queue, lambda e: e.dma_start(out=tile_ap, in_=src_row2d.to_broadcast([n, F])),
                   reads=[self.rMOD], writes=[res])

    def rms_rstd(self, st_tiles, xt, r_x, width):
        S = self.S
        junk, ss, ms, rstd, r_s = st_tiles
        S.op("act", lambda e: e.activation(out=junk[:, 0:width], in_=xt[:, 0:width], func=AF.Square, accum_out=ss[:, 0:1]),
             reads=[r_x], writes=[r_s])
        S.op("dve", lambda e: e.tensor_scalar(out=ms[:], in0=ss[:], scalar1=1.0 / width, scalar2=EPS, op0=ALU.mult, op1=ALU.add),
             reads=[r_s], writes=[r_s])
        S.op("act", lambda e: e.activation(out=ms[:], in_=ms[:], func=AF.Sqrt), reads=[r_s], writes=[r_s])
        S.op("dve", lambda e: e.reciprocal(out=rstd[:], in_=ms[:]), reads=[r_s], writes=[r_s])
        return rstd, r_s

    def stat_tiles(self, st, name, n=2):
        items = []
        for i in range(n):
            items.append((self.sb(st, "%sj%d" % (name, i), [128, 1024], BF16), self.sb(st, "%ss%d" % (name, i), [128, 1], F32),
                          self.sb(st, "%sm%d" % (name, i), [128, 1], F32), self.sb(st, "%sr%d" % (name, i), [128, 1], F32), Res()))
        return Rot(items)

    def phase_AB(self, l):
        nc, S = self.nc, self.S
        with ExitStack() as st:
            hT = self.sb(st, "hT", [128, 8, T], BF16)
            r_h = [Res() for _ in range(NT)]
            G1 = self.sb(st, "G1", [128, 2, 8], F32)
            SH1 = self.sb(st, "SH1", [128, 2, 8], F32)
            ng = self.sb(st, "ng", [128, 8], F32)
            r_g = Res()
            self.load_feat("sp", ng, r_g, self.norm1_g[l, :])
            for j in range(2):
                S.dma("sp", lambda e, j=j: e.dma_start(out=SH1[:, j, :], in_=self.MOD[l, j, 0:D].rearrange("(k p) -> p k", p=128),
                                                       allow_slow_non_contiguous=True), reads=[self.rMOD], writes=[r_g])
                S.dma("sp", lambda e, j=j: e.dma_start(out=G1[:, j, :], in_=self.MOD[l, j, D:2 * D].rearrange("(k p) -> p k", p=128),
                                                       allow_slow_non_contiguous=True), reads=[self.rMOD], writes=[r_g])
            for j in range(2):
                S.op("dve", lambda e, j=j: e.scalar_tensor_tensor(out=G1[:, j, :], in0=G1[:, j, :], scalar=1.0, in1=ng[:],
                                                                  op0=ALU.add, op1=ALU.mult), reads=[r_g], writes=[r_g])
            xrot = self.rot_sb(st, "xt", [128, D], F32, 2)
            xsrot = self.rot_sb(st, "xs", [128, D], BF16, 2)
            strot = self.stat_tiles(st, "st")
            for i in range(NT):
                xt, r_x = xrot.next()
                S.dma("sp", lambda e, xt=xt, i=i: e.dma_start(out=xt[:], in_=self.Xsrc[i * 128:(i + 1) * 128, :]),
                      reads=[self.rX[i]], writes=[r_x])
                stt = strot.next()
                rstd, r_s = self.rms_rstd(stt, xt, r_x, D)
                xs, r_xs = xsrot.next()
                S.op("act", lambda e, xs=xs, xt=xt, rstd=rstd: e.activation(out=xs[:], in_=xt[:], func=AF.Copy, scale=rstd[:, 0:1]),
                     reads=[r_x, r_s], writes=[r_xs])
                pt, r_p = self.pT.next()
                for k in range(8):
                    S.op("pe", lambda e, pt=pt, xs=xs, k=k: e.transpose(out=pt[:, k * 128:(k + 1) * 128], in_=xs[:, k * 128:(k + 1) * 128],
                                                                        identity=self.identb[:]),
                         reads=[r_xs, self.r_id], writes=[r_p], signal=(k == 7))
                j = 0 if i < 32 else 1
                for k in range(8):
                    S.op("act", lambda e, pt=pt, k=k, i=i, j=j: e.activation(out=hT[:, k, i * 128:(i + 1) * 128], in_=pt[:, k * 128:(k + 1) * 128],
                                                                             func=AF.Identity, scale=G1[:, j, k:k + 1], bias=SH1[:, j, k:k + 1]),
                         reads=[r_p, r_g], writes=[r_h[i]])
            wv = self.w_in[l].rearrange("(k p) c -> p k c", p=128)
            wrot = self.rot_sb(st, "wblk", [128, 8, 512], BF16, 2)
            stg = self.rot_sb(st, "stg", [128, T], BF16, 2)
            tmst = self.rot_sb(st, "tmst", [128, 512], BF16, 3)
            nts = ntiles512(T)
            ev_i = 0
            for cb in range(14):
                c0 = cb * 512
                cw = min(512, INC - c0)
                wt, r_w = wrot.next()
                S.dma("pool", lambda e, wt=wt, c0=c0, cw=cw: e.dma_start(out=wt[:, :, 0:cw], in_=wv[:, :, c0:c0 + cw]), writes=[r_w])
                tm_lo, tm_hi = max(c0, 2560), min(c0 + cw, 3840)
                for cc in range(cw // 128):
                    col = c0 + cc * 128
                    if 2560 <= col < 3840:
                        continue
                    is_gate = col >= 3840
                    sg, r_sg = stg.next()
                    for (t0, tw) in nts:
                        pt, r_p = self.pA.next()
                        rh = r_h[t0 // 128:(t0 + tw) // 128]
                        for k in range(8):
                            S.op("pe", lambda e, pt=pt, wt=wt, k=k, cc=cc, t0=t0, tw=tw: e.matmul(
                                pt[:, 0:tw], lhsT=wt[:, k, cc * 128:(cc + 1) * 128], rhs=hT[:, k, t0:t0 + tw], start=(k == 0), stop=(k == 7)),
                                reads=[r_w] + rh, writes=[r_p], signal=(k == 7))
                        if is_gate:
                            S.op("act", lambda e, pt=pt, sg=sg, t0=t0, tw=tw: e.activation(out=sg[:, t0:t0 + tw], in_=pt[:, 0:tw], func=AF.Sigmoid),
                                 reads=[r_p], writes=[r_sg])
                        elif ev_i % 2 == 0:
                            S.op("act", lambda e, pt=pt, sg=sg, t0=t0, tw=tw: e.activation(out=sg[:, t0:t0 + tw], in_=pt[:, 0:tw], func=AF.Copy),
                                 reads=[r_p], writes=[r_sg])
                        else:
                            S.op("dve", lambda e, pt=pt, sg=sg, t0=t0, tw=tw: e.tensor_copy(out=sg[:, t0:t0 + tw], in_=pt[:, 0:tw]),
                                 reads=[r_p], writes=[r_sg])
                        ev_i += 1
                    S.dma("sp", lambda e, sg=sg, col=col: e.dma_start(out=self.FT[col:col + 128, :], in_=sg[:]), reads=[r_sg], writes=[self.rFT])
                if tm_lo < tm_hi:
                    w0, wn = tm_lo - c0, tm_hi - tm_lo
                    for i in range(NT):
                        pt, r_p = self.pA.next()
                        for k in range(8):
                            S.op("pe", lambda e, pt=pt, wt=wt, k=k, i=i, w0=w0, wn=wn: e.matmul(
                                pt[:, 0:wn], lhsT=hT[:, k, i * 128:(i + 1) * 128], rhs=wt[:, k, w0:w0 + wn], start=(k == 0), stop=(k == 7)),
                                reads=[r_w, r_h[i]], writes=[r_p], signal=(k == 7))
                        ts, r_ts = tmst.next()
                        if i % 2 == 0:
                            S.op("act", lambda e, pt=pt, ts=ts, wn=wn: e.activation(out=ts[:, 0:wn], in_=pt[:, 0:wn], func=AF.Copy),
                                 reads=[r_p], writes=[r_ts])
                        else:
                            S.op("dve", lambda e, pt=pt, ts=ts, wn=wn: e.tensor_copy(out=ts[:, 0:wn], in_=pt[:, 0:wn]),
                                 reads=[r_p], writes=[r_ts])
                        S.dma("sp", lambda e, ts=ts, i=i, wn=wn, tm_lo=tm_lo: e.dma_start(
                            out=self.TM[i * 128:(i + 1) * 128, tm_lo - 2560:tm_lo - 2560 + wn], in_=ts[:, 0:wn]), reads=[r_ts], writes=[self.rTM])
            self.end_phase()

    def phase_conv(self, l):
        nc, S = self.nc, self.S
        with ExitStack() as st:
            cw = self.sb(st, "cw", [128, 4, 3], F32)
            r_cw = Res()
            for kk in range(3):
                S.dma("sp", lambda e, kk=kk: e.dma_start(out=cw[:, :, kk], in_=self.conv_w[l, kk, :].rearrange("(j p) -> p j", p=128),
                                                         allow_slow_non_contiguous=True), writes=[r_cw])
            inrot = self.rot_sb(st, "cin", [128, 3, T], BF16, 2)
            u = self.sb(st, "cu", [128, T], F32)
            acc = self.sb(st, "cacc", [128, T], F32)
            yrot = self.rot_sb(st, "cy", [128, T], BF16, 2)
            r_u, r_a = Res(), Res()
            for j in range(4):
                ci, r_ci = inrot.next()
                for b in range(3):
                    S.dma("sp" if b != 1 else "act", lambda e, ci=ci, b=b, j=j: e.dma_start(
                        out=ci[:, b, :], in_=self.FT[b * 512 + j * 128:b * 512 + (j + 1) * 128, :]), reads=[self.rFT], writes=[r_ci])
                S.op("pool", lambda e, ci=ci: e.tensor_tensor(out=u[:], in0=ci[:, 1, :], in1=ci[:, 2, :], op=ALU.mult),
                     reads=[r_ci], writes=[r_u])
                S.op("dve", lambda e, j=j: e.tensor_scalar(out=acc[:], in0=u[:], scalar1=cw[:, j, 1:2], scalar2=None, op0=ALU.mult),
                     reads=[r_u, r_cw], writes=[r_a])
                for (a, b) in ((0, TL), (TL, T)):
                    S.op("dve", lambda e, j=j, a=a, b=b: e.scalar_tensor_tensor(out=acc[:, a + 1:b], in0=u[:, a:b - 1], scalar=cw[:, j, 0:1],
                                                                              in1=acc[:, a + 1:b], op0=ALU.mult, op1=ALU.add),
                         reads=[r_u, r_cw, r_a], writes=[r_a])
                    S.op("dve", lambda e, j=j, a=a, b=b: e.scalar_tensor_tensor(out=acc[:, a:b - 1], in0=u[:, a + 1:b], scalar=cw[:, j, 2:3],
                                                                              in1=acc[:, a:b - 1], op0=ALU.mult, op1=ALU.add),
                         reads=[r_u, r_cw, r_a], writes=[r_a])
                y, r_y = yrot.next()
                S.op("pool", lambda e, y=y, ci=ci: e.tensor_tensor(out=y[:], in0=ci[:, 0, :], in1=acc[:], op=ALU.mult),
                     reads=[r_ci, r_a], writes=[r_y])
                S.dma("sp", lambda e, y=y, j=j: e.dma_start(out=self.YT[j * 128:(j + 1) * 128, :], in_=y[:]), reads=[r_y], writes=[self.rYT])
            self.end_phase()

    def attn_block(self, kt_ap_fn, q_ap, va_ap_fn, chunks, N, acc, r_acc, prot, reads, tab_fn=None, addrot=None, scale=0.125):
        S = self.S
        n = len(chunks)
        pend = []

        def qk(si):
            s = chunks[si]
            ps, r_ps = self.pA.next()
            kt = kt_ap_fn(s)
            S.op("pe", lambda e: e.matmul(ps[:, 0:N], lhsT=kt, rhs=q_ap, start=True, stop=True), reads=reads, writes=[r_ps])
            pe_t, r_pe = prot.next()
            tb = tab_fn(s) if tab_fn is not None else None
            if tb is not None:
                ad, r_ad = addrot.next()
                S.op("dve", lambda e: e.scalar_tensor_tensor(out=ad[:, 0:N], in0=ps[:, 0:N], scalar=scale, in1=tb, op0=ALU.mult, op1=ALU.add),
                     reads=[r_ps] + reads, writes=[r_ad])
                S.op("act", lambda e: e.activation(out=pe_t[:, 0:N], in_=ad[:, 0:N], func=AF.Exp), reads=[r_ad], writes=[r_pe])
            else:
                S.op("act", lambda e: e.activation(out=pe_t[:, 0:N], in_=ps[:, 0:N], func=AF.Exp, scale=scale), reads=[r_ps], writes=[r_pe])
            return (s, pe_t, r_pe)

        LA = 2
        for si in range(min(LA, n)):
            pend.append(qk(si))
        for si in range(n):
            if si + LA < n:
                pend.append(qk(si + LA))
            s, pe_t, r_pe = pend.pop(0)
            va_ = va_ap_fn(s)
            S.op("pe", lambda e, va_=va_, pe_t=pe_t, si=si: e.matmul(acc[:, 0:N], lhsT=va_, rhs=pe_t[:, 0:N], start=(si == 0), stop=(si == n - 1)),
                 reads=[r_pe] + reads, writes=[r_acc], signal=(si == n - 1))

    def attn_finish(self, acc, r_acc, N, out_ap, r_out, recrot):
        S = self.S
        rec, r_rec = recrot.next()
        S.op("act", lambda e: e.activation(out=rec[0:64, 0:N], in_=acc[64:128, 0:N], func=AF.Copy), reads=[r_acc], writes=[r_rec])
        S.op("dve", lambda e: e.reciprocal(out=rec[0:64, 0:N], in_=rec[0:64, 0:N]), reads=[r_rec], writes=[r_rec])
        S.op("dve", lambda e: e.tensor_tensor(out=out_ap, in0=acc[0:64, 0:N], in1=rec[0:64, 0:N], op=ALU.mult),
             reads=[r_acc, r_rec], writes=[r_out])

    def phase_gqa(self, l, last):
        nc, S = self.nc, self.S
        with ExitStack() as st:
            QT = self.sb(st, "QT", [128, 8, T], BF16)
            KT2 = self.sb(st, "KT2", [128, 2, T], BF16)
            VA = self.sb(st, "VA", [128, NT, 2, 128], BF16)
            r_q = Res()
            r_vat = [Res() for _ in range(NT)]
            S.op("pool", lambda e: e.memset(VA[:, :, :, 64:128], 1.0), writes=r_vat)
            S.op("pool", lambda e: e.memset(QT[:], 0.0), writes=[r_q])
            for i in range(NT):
                S.dma("sp" if i % 2 == 0 else "act", lambda e, i=i: e.dma_start(
                    out=VA[:, i, :, 0:64], in_=self.TM[i * 128:(i + 1) * 128, 1152:1280].rearrange("p (g d) -> p g d", g=2)),
                    reads=[self.rTM], writes=[r_vat[i]])
            gain = self.sb(st, "gain", [128, 640], F32)
            r_gn = Res()
            S.dma("sp", lambda e: e.dma_start(out=gain[:], in_=self.qkgain[l:l + 1, :].to_broadcast([128, 640])), writes=[r_gn])
            with ExitStack() as st2:
                inrot = self.rot_sb(st2, "gin", [128, 640], BF16, 2)
                sqrot = self.rot_sb(st2, "gsq", [128, 640], F32, 2)
                xnrot = self.rot_sb(st2, "gxn", [128, 640], F32, 2)
                swrot = self.rot_sb(st2, "gsw", [128, 640], F32, 2)
                cosrot = self.rot_sb(st2, "gcos", [128, 640], F32, 2)
                sinrot = self.rot_sb(st2, "gsin", [128, 640], F32, 2)
                qbrot = self.rot_sb(st2, "gqb", [128, 640], BF16, 2)
                ssrot = Rot([(self.sb(st2, "gss%d" % i, [128, 10], F32), Res()) for i in range(2)])
                for i in range(NT):
                    xi, r_xi = inrot.next()
                    S.dma("sp", lambda e, xi=xi, i=i: e.dma_start(out=xi[:], in_=self.TM[i * 128:(i + 1) * 128, 512:1152]), reads=[self.rTM], writes=[r_xi])
                    sq, r_sq = sqrot.next()
                    S.op("pool", lambda e, sq=sq, xi=xi: e.tensor_tensor(out=sq[:], in0=xi[:], in1=xi[:], op=ALU.mult), reads=[r_xi], writes=[r_sq])
                    ss, r_ss = ssrot.next()
                    S.op("dve", lambda e, ss=ss, sq=sq: e.tensor_reduce(out=ss[:], in_=sq[:].rearrange("p (h d) -> p h d", d=64), axis=AX.X, op=ALU.add),
                         reads=[r_sq], writes=[r_ss])
                    S.op("dve", lambda e, ss=ss: e.tensor_scalar(out=ss[:], in0=ss[:], scalar1=1.0 / 64, scalar2=EPS, op0=ALU.mult, op1=ALU.add),
                         reads=[r_ss], writes=[r_ss])
                    S.op("act", lambda e, ss=ss: e.activation(out=ss[:], in_=ss[:], func=AF.Sqrt), reads=[r_ss], writes=[r_ss])
                    S.op("dve", lambda e, ss=ss: e.reciprocal(out=ss[:], in_=ss[:]), reads=[r_ss], writes=[r_ss])
                    xn, r_xn = xnrot.next()
                    r_xh = [Res() for _ in range(10)]
                    for h in range(10):
                        S.op("dve", lambda e, xn=xn, xi=xi, ss=ss, h=h: e.tensor_scalar(
                            out=xn[:, h * 64:(h + 1) * 64], in0=xi[:, h * 64:(h + 1) * 64], scalar1=ss[:, h:h + 1], scalar2=None, op0=ALU.mult),
                            reads=[r_xi, r_ss, r_xn], writes=[r_xh[h]])
                    S.op("dve", lambda e, xn=xn: e.tensor_tensor(out=xn[:], in0=xn[:], in1=gain[:], op=ALU.mult), reads=r_xh + [r_gn], writes=[r_xn])
                    qb, r_qb = qbrot.next()
                    if i < 32:
                        co, r_co = cosrot.next()
                        si_, r_si = sinrot.next()
                        S.dma("act", lambda e, co=co, i=i: e.dma_start(out=co[:], in_=self.ropec[i * 128:(i + 1) * 128, :]), writes=[r_co])
                        S.dma("act", lambda e, si_=si_, i=i: e.dma_start(out=si_[:], in_=self.ropes[i * 128:(i + 1) * 128, :]), writes=[r_si])
                        sw, r_sw = swrot.next()
                        xv = xn[:].rearrange("p (g two d) -> p g two d", two=2, d=16)
                        swv = sw[:].rearrange("p (g two d) -> p g two d", two=2, d=16)
                        S.op("pool", lambda e, swv=swv, xv=xv: e.tensor_copy(out=swv[:, :, 0, :], in_=xv[:, :, 1, :]), reads=[r_xn], writes=[r_sw])
                        S.op("pool", lambda e, swv=swv, xv=xv: e.tensor_copy(out=swv[:, :, 1, :], in_=xv[:, :, 0, :]), reads=[r_xn, r_sw], writes=[r_sw])
                        S.op("pool", lambda e, sw=sw, si_=si_: e.tensor_tensor(out=sw[:], in0=sw[:], in1=si_[:], op=ALU.mult), reads=[r_sw, r_si], writes=[r_sw])
                        S.op("dve", lambda e, xn=xn, co=co: e.tensor_tensor(out=xn[:], in0=xn[:], in1=co[:], op=ALU.mult), reads=[r_xn, r_co], writes=[r_xn])
                        S.op("dve", lambda e, qb=qb, xn=xn, sw=sw: e.tensor_tensor(out=qb[:], in0=xn[:], in1=sw[:], op=ALU.add), reads=[r_xn, r_sw], writes=[r_qb])
                    else:
                        S.op("dve", lambda e, qb=qb, xn=xn: e.tensor_copy(out=qb[:], in_=xn[:]), reads=[r_xn], writes=[r_qb])
                    pt, r_p = self.pT.next()
                    for c in range(5):
                        S.op("pe", lambda e, pt=pt, qb=qb, c=c: e.transpose(out=pt[:, c * 128:(c + 1) * 128], in_=qb[:, c * 128:(c + 1) * 128], identity=self.identb[:]),
                             reads=[r_qb, self.r_id], writes=[r_p], signal=(c == 4))
                    tsl = slice(i * 128, (i + 1) * 128)
                    QTv = QT[:].rearrange("p (c two) t -> p c two t", two=2)
                    S.op("act", lambda e, pt=pt, tsl=tsl, QTv=QTv: e.activation(out=QTv[0:64, :, 0, tsl], in_=pt[0:64, 0:512].rearrange("p (c t) -> p c t", c=4), func=AF.Copy),
                         reads=[r_p, r_q], writes=[r_q])
                    S.op("act", lambda e, pt=pt, tsl=tsl, QTv=QTv: e.activation(out=QTv[64:128, :, 1, tsl], in_=pt[64:128, 0:512].rearrange("p (c t) -> p c t", c=4), func=AF.Copy),
                         reads=[r_p, r_q], writes=[r_q])
                    S.op("dve", lambda e, pt=pt, tsl=tsl: e.tensor_copy(out=KT2[0:64, 0, tsl], in_=pt[0:64, 512:640]), reads=[r_p, r_q], writes=[r_q])
                    S.op("dve", lambda e, pt=pt, tsl=tsl: e.tensor_copy(out=KT2[64:128, 1, tsl], in_=pt[64:128, 512:640]), reads=[r_p, r_q], writes=[r_q])
                    S.op("act", lambda e, pt=pt, tsl=tsl: e.activation(out=KT2[64:128, 0, tsl], in_=pt[0:64, 512:640], func=AF.Copy), reads=[r_p, r_q], writes=[r_q])
                    S.op("act", lambda e, pt=pt, tsl=tsl: e.activation(out=KT2[0:64, 1, tsl], in_=pt[64:128, 512:640], func=AF.Copy), reads=[r_p, r_q], writes=[r_q])
                self.S.barrier()
            prot = self.rot_sb(st, "gpe", [128, 512], BF16, 5)
            recrot = self.rot_sb(st, "grec", [64, 512], F32, 2)
            ysrot = self.rot_sb(st, "gys", [64, T], BF16, 2)
            for h in range(8):
                g, c, hh = h // 4, h // 2, h % 2
                ps_ = slice(hh * 64, (hh + 1) * 64)
                ys, r_ys = ysrot.next()
                blocks = [(n * 512, 512, list(range(NT))) for n in range(8)]
                if not last:
                    blocks.append((TL, TC, [32, 33]))
                for (q0, N, chunks) in blocks:
                    acc, r_acc = self.pB.next()
                    self.attn_block(lambda s: KT2[:, g, s * 128:(s + 1) * 128], QT[:, h, q0:q0 + N],
                                    lambda s: VA[:, s, g, :], chunks, N, acc, r_acc, prot, [r_q] + r_vat)
                    self.attn_finish(acc, r_acc, N, ys[:, q0:q0 + N], r_ys, recrot)
                ncol = T if not last else TL
                S.dma("sp", lambda e, ys=ys, h=h, ncol=ncol: e.dma_start(out=self.YT[1024 + h * 64:1024 + (h + 1) * 64, 0:ncol], in_=ys[:, 0:ncol]),
                      reads=[r_ys], writes=[self.rYT])
            self.end_phase()

    def phase_na(self, l, last):
        nc, S = self.nc, self.S
        qblocks = [(0, 4, 1, [0, 2, 4, 6])]
        for r0 in range(4, 60, 8):
            qblocks.append((r0, 8, 0, list(range(r0 - 4, r0 + 12, 2))))
        qblocks.append((60, 1, 0, [56, 58, 60, 62]))
        qblocks.append((61, 3, 1, [56, 58, 60, 62]))
        with ExitStack() as st:
            qkrot = self.rot_sb(st, "nqk", [128, 2, T], BF16, 2)
            qzrot = self.rot_sb(st, "nqz", [128, 2, T], BF16, 2)
            varot = self.rot_sb(st, "nva", [128, NT, 2, 128], BF16, 2)
            tabrot = self.rot_sb(st, "ntab", [128, 2, 2, NJ * 64], BF16, 2)
            prot = self.rot_sb(st, "npe", [128, 512], BF16, 5)
            addrot = self.rot_sb(st, "nad", [128, 512], F32, 4)
            recrot = self.rot_sb(st, "nrec", [64, 512], F32, 2)
            ysrot = self.rot_sb(st, "nys", [64, T], BF16, 2)
            for c in range(4):
                qk, r_qk = qkrot.next()
                va, r_va = varot.next()
                tab, r_tab = tabrot.next()
                S.dma("sp", lambda e, qk=qk, c=c: e.dma_start(out=qk[:, 0, :], in_=self.FT[1536 + c * 128:1536 + (c + 1) * 128, :]), reads=[self.rFT], writes=[r_qk])
                S.dma("act", lambda e, qk=qk, c=c: e.dma_start(out=qk[:, 1, :], in_=self.FT[2048 + c * 128:2048 + (c + 1) * 128, :]), reads=[self.rFT], writes=[r_qk])
                S.op("pool", lambda e, va=va: e.memset(va[:, :, :, 64:128], 1.0), writes=[r_va])
                qz, r_qz = qzrot.next()
                S.op("pool", lambda e, qz=qz: e.memset(qz[:], 0.0), writes=[r_qz])
                S.op("dve", lambda e, qz=qz, qk=qk: e.tensor_copy(out=qz[0:64, 0, :], in_=qk[0:64, 0, :]), reads=[r_qk, r_qz], writes=[r_qz])
                S.op("pool", lambda e, qz=qz, qk=qk: e.tensor_copy(out=qz[64:128, 1, :], in_=qk[64:128, 0, :]), reads=[r_qk, r_qz], writes=[r_qz])
                r_vt = [Res() for _ in range(NT)]
                for i in range(NT):
                    S.dma("sp" if i % 2 == 0 else "act", lambda e, va=va, i=i, c=c: e.dma_start(
                        out=va[:, i, :, 0:64], in_=self.TM[i * 128:(i + 1) * 128, c * 128:(c + 1) * 128].rearrange("p (g d) -> p g d", g=2)),
                        reads=[self.rTM, r_va], writes=[r_vt[i]])
                for v in range(2):
                    S.dma("pool", lambda e, tab=tab, v=v, c=c: e.dma_start(out=tab[:, v, :, :], in_=self.natab[l, v, :, 2 * c:2 * c + 2, :]), writes=[r_tab])
                for hh in range(2):
                    h = 2 * c + hh
                    ps_ = slice(hh * 64, (hh + 1) * 64)
                    ys, r_ys = ysrot.next()
                    for (r0, R, v, krows) in qblocks:
                        N = 64 * R
                        q0 = r0 * 64
                        chunks = [kr // 2 for kr in krows] + [32, 33]

                        def tab_fn(s, r0=r0, v=v, N=N, tab=tab, hh=hh):
                            if s >= 32:
                                return None
                            j0 = r0 - 2 * s + JOFF
                            return tab[:, v, hh, j0 * 64:j0 * 64 + N]
                        acc, r_acc = self.pB.next()
                        self.attn_block(lambda s, qk=qk: qk[:, 1, s * 128:(s + 1) * 128], qz[:, hh, q0:q0 + N],
                                        lambda s, va=va, hh=hh: va[:, s, hh, :], chunks, N, acc, r_acc, prot, [r_qk, r_qz, r_va, r_tab] + r_vt,
                                        tab_fn=tab_fn, addrot=addrot)
                        self.attn_finish(acc, r_acc, N, ys[:, q0:q0 + N], r_ys, recrot)
                    if not last:
                        acc, r_acc = self.pB.next()
                        self.attn_block(lambda s, qk=qk: qk[:, 1, s * 128:(s + 1) * 128], qz[:, hh, TL:T],
                                        lambda s, va=va, hh=hh: va[:, s, hh, :], [32, 33], TC, acc, r_acc, prot, [r_qk, r_qz, r_va] + r_vt)
                        self.attn_finish(acc, r_acc, TC, ys[:, TL:T], r_ys, recrot)
                    ncol = T if not last else TL
                    S.dma("sp", lambda e, ys=ys, h=h, ncol=ncol: e.dma_start(out=self.YT[512 + h * 64:512 + (h + 1) * 64, 0:ncol], in_=ys[:, 0:ncol]),
                          reads=[r_ys], writes=[self.rYT])
            self.end_phase()

    def phase_merge(self, l):
        nc, S = self.nc, self.S
        ntok = self.nt_act * 128
        with ExitStack() as st:
            WBR = self.sb(st, "WBR", [128, 12, D], BF16)
            WO = self.sb(st, "WO", [128, 8, D], BF16)
            r_w = Res()
            for b in range(3):
                S.dma("pool", lambda e, b=b: e.dma_start(out=WBR[:, b * 4:(b + 1) * 4, :], in_=self.w_br[l, b].rearrange("(k p) f -> p k f", p=128)), writes=[r_w])
            S.dma("pool", lambda e: e.dma_start(out=WO[:], in_=self.w_out[l].rearrange("(k p) f -> p k f", p=128)), writes=[r_w])
            GT = self.sb(st, "GT1", [128, 2, D], F32)
            r_gt = Res()
            for j in range(2):
                self.load_bc("sp", GT[:, j, :], r_gt, self.MOD[l, j:j + 1, 2 * D:3 * D])
            ytrot = self.rot_sb(st, "mYT", [128, 12, 512], BF16, 2)
            sgrot = self.rot_sb(st, "mSG", [128, 24, 512], BF16, 2)
            mrot = self.rot_sb(st, "mT", [128, 8, 512], BF16, 2)
            m0rot = self.rot_sb(st, "m0", [128, 512], F32, 2)
            m1rot = self.rot_sb(st, "m1", [128, 512], F32, 2)
            m2rot = self.rot_sb(st, "m2", [128, 512], F32, 2)
            xrot = self.rot_sb(st, "mx", [128, D], F32, 2)
            xorot = self.rot_sb(st, "mxo", [128, D], F32, 2)
            ytv = self.YT.rearrange("(j p) t -> p j t", p=128)
            sgv = self.FT[3840:INC, :].rearrange("(j p) t -> p j t", p=128)
            for (t0, tw) in ntiles512(ntok):
                yt, r_yt = ytrot.next()
                sg, r_sg = sgrot.next()
                S.dma("sp", lambda e, yt=yt, t0=t0, tw=tw: e.dma_start(out=yt[:, :, 0:tw], in_=ytv[:, :, t0:t0 + tw]), reads=[self.rYT], writes=[r_yt])
                S.dma("act", lambda e, sg=sg, t0=t0, tw=tw: e.dma_start(out=sg[:, :, 0:tw], in_=sgv[:, :, t0:t0 + tw]), reads=[self.rFT], writes=[r_sg])
                mT, r_m = mrot.next()
                for f in range(8):
                    pb = []
                    for b in range(3):
                        pt, r_p = self.pA.next()
                        for kc in range(4):
                            S.op("pe", lambda e, pt=pt, b=b, kc=kc, f=f, yt=yt, tw=tw: e.matmul(
                                pt[:, 0:tw], lhsT=WBR[:, b * 4 + kc, f * 128:(f + 1) * 128], rhs=yt[:, b * 4 + kc, 0:tw], start=(kc == 0), stop=(kc == 3)),
                                reads=[r_w, r_yt], writes=[r_p], signal=(kc == 3))
                        pb.append((pt, r_p))
                    a0, r_a0 = m0rot.next()
                    a1, r_a1 = m1rot.next()
                    a2, r_a2 = m2rot.next()
                    for b, (a, r_a) in enumerate(((a0, r_a0), (a1, r_a1), (a2, r_a2))):
                        pt, r_p = pb[b]
                        S.op("dve", lambda e, a=a, pt=pt, sg=sg, b=b, f=f, tw=tw: e.tensor_tensor(out=a[:, 0:tw], in0=pt[:, 0:tw], in1=sg[:, b * 8 + f, 0:tw], op=ALU.mult),
                             reads=[r_p, r_sg], writes=[r_a])
                    S.op("pool", lambda e, a0=a0, a1=a1, tw=tw: e.tensor_tensor(out=a0[:, 0:tw], in0=a0[:, 0:tw], in1=a1[:, 0:tw], op=ALU.add),
                         reads=[r_a0, r_a1], writes=[r_a0])
                    S.op("pool", lambda e, a0=a0, a2=a2, mT=mT, f=f, tw=tw: e.tensor_tensor(out=mT[:, f, 0:tw], in0=a0[:, 0:tw], in1=a2[:, 0:tw], op=ALU.add),
                         reads=[r_a0, r_a2], writes=[r_m])
                for ts in range(tw // 128):
                    i = t0 // 128 + ts
                    j = 0 if i < 32 else 1
                    xt, r_x = xrot.next()
                    S.dma("sp", lambda e, xt=xt, i=i: e.dma_start(out=xt[:], in_=self.Xsrc[i * 128:(i + 1) * 128, :]), reads=[self.rX[i]], writes=[r_x])
                    xo, r_xo = xorot.next()
                    for half in range(2):
                        pt, r_p = self.pA.next()
                        for f in range(8):
                            S.op("pe", lambda e, pt=pt, f=f, mT=mT, ts=ts, half=half: e.matmul(
                                pt[:, :], lhsT=mT[:, f, ts * 128:(ts + 1) * 128], rhs=WO[:, f, half * 512:(half + 1) * 512], start=(f == 0), stop=(f == 7)),
                                reads=[r_w, r_m], writes=[r_p], signal=(f == 7))
                        hs = slice(half * 512, (half + 1) * 512)
                        S.op("dve", lambda e, pt=pt, xo=xo, hs=hs, j=j: e.tensor_tensor(out=xo[:, hs], in0=pt[:, :], in1=GT[:, j, hs], op=ALU.mult),
                             reads=[r_p, r_gt], writes=[r_xo])
                    S.op("pool", lambda e, xo=xo, xt=xt: e.tensor_tensor(out=xo[:], in0=xo[:], in1=xt[:], op=ALU.add), reads=[r_xo, r_x], writes=[r_xo])
                    S.dma("sp", lambda e, xo=xo, i=i: e.dma_start(out=self.X[i * 128:(i + 1) * 128, :], in_=xo[:]), reads=[r_xo], writes=[self.rX[i]])
            self.end_phase()
            self.Xsrc = self.X

    def phase_route(self, l):
        nc, S = self.nc, self.S
        with ExitStack() as st:
            G2 = self.sb(st, "G2", [128, 2, D], F32)
            SH2 = self.sb(st, "SH2", [128, 2, D], F32)
            NG = self.sb(st, "NG2", [128, D], F32)
            r_g = Res()
            self.load_bc("sp", NG[:], r_g, self.norm2_g[l:l + 1, :])
            for j in range(2):
                self.load_bc("sp", SH2[:, j, :], r_g, self.MOD[l, j:j + 1, 3 * D:4 * D])
                self.load_bc("act", G2[:, j, :], r_g, self.MOD[l, j:j + 1, 4 * D:5 * D])
            for j in range(2):
                S.op("dve", lambda e, j=j: e.scalar_tensor_tensor(out=G2[:, j, :], in0=G2[:, j, :], scalar=1.0, in1=NG[:], op0=ALU.add, op1=ALU.mult),
                     reads=[r_g], writes=[r_g])
            RW = self.sb(st, "RW", [128, 8, NE], F32)
            RB = self.sb(st, "RB", [128, NE], F32)
            S.dma("sp", lambda e: e.dma_start(out=RW[:], in_=self.router_w[l].rearrange("(k p) e -> p k e", p=128)), writes=[r_g])
            self.load_bc("sp", RB[:], r_g, self.router_b[l:l + 1, :])
            UTf = self.sb(st, "UTf", [128, 128], F32)
            UT = self.sb(st, "UT", [128, 128], BF16)
            ONES = self.sb(st, "ONES", [128, 128], BF16)
            EB = self.sb(st, "EB", [128, NE], F32)
            CNT = self.sb(st, "CNT", [128, NE], F32)
            r_c = Res()
            r_cnt = Res()
            S.op("pool", lambda e: e.memset(UTf[:], 1.0), writes=[r_c])
            S.op("pool", lambda e: e.affine_select(out=UTf[:], in_=UTf[:], pattern=[[1, 128]], compare_op=ALU.is_gt, fill=0.0, base=0, channel_multiplier=-1),
                 reads=[r_c], writes=[r_c])
            S.op("dve", lambda e: e.tensor_copy(out=UT[:], in_=UTf[:]), reads=[r_c], writes=[r_c])
            S.op("pool", lambda e: e.memset(ONES[:], 1.0), writes=[r_c])
            S.op("pool", lambda e: e.iota(EB[:], pattern=[[CAP, NE]], base=0, channel_multiplier=0, allow_small_or_imprecise_dtypes=True), writes=[r_c])
            S.op("pool", lambda e: e.memset(CNT[:], 0.0), writes=[r_cnt])
            xrot = self.rot_sb(st, "rx", [128, D], F32, 3)
            hrot = self.rot_sb(st, "rh", [128, D], F32, 3)
            hbrot = self.rot_sb(st, "rhb", [128, D], BF16, 6)
            htrot = self.rot_sb(st, "rht", [128, 8, 128], F32, 3)
            strot = self.stat_tiles(st, "rst", 3)
            smrot = Rot([({n: self.sb(st, "rs%s%d" % (n, i), [128, w], dt) for n, w, dt in (
                ("lg", NE, F32), ("t8", 8, F32), ("nm", 1, F32), ("e4", 4, F32), ("sm", 1, F32), ("mk", NE, BF16),
                ("pos", NE, F32), ("oh", NE, F32), ("df", 4, F32))}, Res()) for i in range(4)])
            for i in range(self.nt_act):
                j = 0 if i < 32 else 1
                xt, r_x = xrot.next()
                S.dma("sp", lambda e, xt=xt, i=i: e.dma_start(out=xt[:], in_=self.X[i * 128:(i + 1) * 128, :]), reads=[self.rX[i]], writes=[r_x])
                stt = strot.next()
                rstd, r_s = self.rms_rstd(stt, xt, r_x, D)
                h2, r_h = hrot.next()
                S.op("dve", lambda e, h2=h2, xt=xt, rstd=rstd, j=j: e.scalar_tensor_tensor(out=h2[:], in0=xt[:], scalar=rstd[:, 0:1], in1=G2[:, j, :], op0=ALU.mult, op1=ALU.mult),
                     reads=[r_x, r_s, r_g], writes=[r_h])
                S.op("pool", lambda e, h2=h2, j=j: e.tensor_tensor(out=h2[:], in0=h2[:], in1=SH2[:, j, :], op=ALU.add), reads=[r_h, r_g], writes=[r_h])
                hb, r_hb = hbrot.next()
                S.op("act", lambda e, hb=hb, h2=h2: e.activation(out=hb[:], in_=h2[:], func=AF.Copy), reads=[r_h], writes=[r_hb])
                ht, r_ht = htrot.next()
                for half in range(2):
                    pt, r_p = self.pA.next()
                    for kk in range(4):
                        k = half * 4 + kk
                        S.op("pe", lambda e, pt=pt, h2=h2, k=k, kk=kk: e.transpose(out=pt[:, kk * 128:(kk + 1) * 128], in_=h2[:, k * 128:(k + 1) * 128], identity=self.identf[:]),
                             reads=[r_h, self.r_id], writes=[r_p], signal=(kk == 3))
                    if half == 0:
                        S.op("act", lambda e, pt=pt, ht=ht: e.activation(out=ht[:, 0:4, :], in_=pt[:, :].rearrange("p (k t) -> p k t", k=4), func=AF.Copy), reads=[r_p], writes=[r_ht])
                    else:
                        S.op("dve", lambda e, pt=pt, ht=ht: e.tensor_copy(out=ht[:, 4:8, :], in_=pt[:, :].rearrange("p (k t) -> p k t", k=4)), reads=[r_p, r_ht], writes=[r_ht])
                pl, r_pl = self.pB.next()
                for k in range(8):
                    S.op("pe", lambda e, pl=pl, ht=ht, k=k: e.matmul(pl[:, 0:NE], lhsT=ht[:, k, :], rhs=RW[:, k, :], start=(k == 0), stop=(k == 7)),
                         reads=[r_ht, r_g], writes=[r_pl], signal=(k == 7))
                sm, r_sm = smrot.next()
                S.op("dve", lambda e, sm=sm, pl=pl: e.tensor_tensor(out=sm["lg"][:], in0=pl[:, 0:NE], in1=RB[:], op=ALU.add), reads=[r_pl, r_g], writes=[r_sm])
                S.op("dve", lambda e, sm=sm: e.max(out=sm["t8"][:], in_=sm["lg"][:]), reads=[r_sm], writes=[r_sm])
                S.op("dve", lambda e, sm=sm: e.tensor_scalar(out=sm["nm"][:], in0=sm["t8"][:, 0:1], scalar1=-1.0, scalar2=None, op0=ALU.mult), reads=[r_sm], writes=[r_sm])
                S.op("act", lambda e, sm=sm: e.activation(out=sm["e4"][:], in_=sm["t8"][:, 0:4], func=AF.Exp, bias=sm["nm"][:, 0:1], accum_out=sm["sm"][:, 0:1]), reads=[r_sm], writes=[r_sm])
                S.op("dve", lambda e, sm=sm: e.reciprocal(out=sm["sm"][:], in_=sm["sm"][:]), reads=[r_sm], writes=[r_sm])
                S.op("dve", lambda e, sm=sm, i=i: e.tensor_scalar(out=self.GATES[:, i, :], in0=sm["e4"][:], scalar1=sm["sm"][:, 0:1], scalar2=None, op0=ALU.mult),
                     reads=[r_sm], writes=[self.rROUTE[i]])
                S.op("dve", lambda e, sm=sm: e.tensor_scalar(out=sm["mk"][:], in0=sm["lg"][:], scalar1=sm["t8"][:, 3:4], scalar2=None, op0=ALU.is_ge), reads=[r_sm], writes=[r_sm])
                pp, r_pp = self.pB.next()
                S.op("pe", lambda e, pp=pp, sm=sm: e.matmul(pp[:, 0:NE], lhsT=UT[:], rhs=sm["mk"][:], start=True, stop=True), reads=[r_sm, r_c], writes=[r_pp], signal=False)
                S.op("pe", lambda e, pp=pp, sm=sm: e.matmul(pp[:, 64:64 + NE], lhsT=ONES[:], rhs=sm["mk"][:], start=True, stop=True), reads=[r_sm, r_c], writes=[r_pp])
                S.op("dve", lambda e, sm=sm, pp=pp: e.tensor_tensor(out=sm["pos"][:], in0=pp[:, 0:NE], in1=CNT[:], op=ALU.add), reads=[r_pp, r_cnt, r_sm], writes=[r_sm])
                S.op("dve", lambda e, pp=pp: e.tensor_tensor(out=CNT[:], in0=pp[:, 64:64 + NE], in1=CNT[:], op=ALU.add), reads=[r_pp, r_cnt], writes=[r_cnt])
                S.op("dve", lambda e, sm=sm: e.scalar_tensor_tensor(out=sm["pos"][:], in0=sm["pos"][:], scalar=float(CAP - 1), in1=EB[:], op0=ALU.min, op1=ALU.add),
                     reads=[r_sm, r_c], writes=[r_sm])
                for k in range(4):
                    S.op("dve", lambda e, sm=sm, k=k: e.scalar_tensor_tensor(out=sm["oh"][:], in0=sm["lg"][:], scalar=sm["t8"][:, k:k + 1], in1=sm["pos"][:], op0=ALU.is_equal, op1=ALU.mult),
                         reads=[r_sm], writes=[r_sm])
                    S.op("dve", lambda e, sm=sm, k=k: e.reduce_sum(out=sm["df"][:, k:k + 1], in_=sm["oh"][:], axis=AX.X), reads=[r_sm], writes=[r_sm])
                S.op("dve", lambda e, sm=sm, i=i: e.tensor_copy(out=self.DEST[:, i, :], in_=sm["df"][:]), reads=[r_sm, self.rROUTE[i]], writes=[self.rROUTE[i]])
                for k in range(4):
                    S.dma("pool", lambda e, hb=hb, i=i, k=k: e.indirect_dma_start(
                        out=self.XE[:, :], out_offset=bass.IndirectOffsetOnAxis(ap=self.DEST[:, i, k:k + 1], axis=0), in_=hb[:], in_offset=None),
                        reads=[r_hb, self.rROUTE[i]], writes=[self.rXE])
            S.op("dve", lambda e: e.tensor_copy(out=self.CNTI[:], in_=CNT[:]), reads=[r_cnt], writes=[r_cnt])
            self.end_phase()

    def phase_experts(self, l):
        nc, S = self.nc, self.S
        NTE = CAP // 512
        with ExitStack() as st:
            xe0rot = self.rot_sb(st, "xe0", [128, 4, D], BF16, 2)
            xeCrot = self.rot_sb(st, "xeC", [128, 2, D], BF16, 2)
            xtrot = self.rot_sb(st, "xeT", [128, 8, 512], BF16, 2)
            wgrot = self.rot_sb(st, "wg", [128, 8, D], BF16, 2)
            wurot = self.rot_sb(st, "wu", [128, 8, D], BF16, 2)
            wdrot = self.rot_sb(st, "wd", [128, 8, D], BF16, 1)
            stgrot = self.rot_sb(st, "wstg", [128, 2, 1024], F32, 3)
            bgrot = self.rot_sb(st, "bgu", [128, 8, 2], F32, 2)
            bdrot = self.rot_sb(st, "bd", [128, D], F32, 2)
            atrot = self.rot_sb(st, "aT", [128, 8, 512], BF16, 2)
            gsrot = self.rot_sb(st, "gs", [128, 512], F32, 3)
            sgrot = self.rot_sb(st, "sg", [128, 512], F32, 3)
            usrot = self.rot_sb(st, "us", [128, 512], F32, 3)
            yrot = self.rot_sb(st, "ye", [128, D], F32, 2)
            def load_xe(buf, row0, nsub):
                t_, r_ = buf
                S.dma("sp", lambda e: e.dma_start(out=t_[:, 0:nsub, :], in_=self.XE[row0:row0 + nsub * 128, :].rearrange("(j p) d -> p j d", p=128)),
                      reads=[self.rXE], writes=[r_])

            x0_cur = xe0rot.next()
            load_xe(x0_cur, 0, 4)
            for e_ in range(NE):
                wg, r_wg = wgrot.next()
                wu, r_wu = wurot.next()
                wd, r_wd = wdrot.next()
                wguv = self.w_gu[l, e_].rearrange("(k p) c -> p k c", p=128)
                for kh in range(4):
                    for q in range(2):
                        sgt, r_st = stgrot.next()
                        S.dma("sp" if (kh * 2 + q) % 2 == 0 else "act", lambda e, sgt=sgt, q=q, kh=kh, wguv=wguv: e.dma_start(
                            out=sgt[:], in_=wguv[:, kh * 2:(kh + 1) * 2, q * 1024:(q + 1) * 1024]), writes=[r_st])
                        sv = sgt[:].rearrange("p k (c two) -> p k c two", two=2)
                        S.op("pool", lambda e, wg=wg, sv=sv, q=q, kh=kh: e.tensor_copy(out=wg[:, kh * 2:(kh + 1) * 2, q * 512:(q + 1) * 512], in_=sv[:, :, :, 0]),
                             reads=[r_st], writes=[r_wg])
                        S.op("act", lambda e, wu=wu, sv=sv, q=q, kh=kh: e.activation(out=wu[:, kh * 2:(kh + 1) * 2, q * 512:(q + 1) * 512], in_=sv[:, :, :, 1], func=AF.Copy),
                             reads=[r_st], writes=[r_wu])
                S.dma("pool", lambda e, wd=wd, e_=e_: e.dma_start(out=wd[:], in_=self.w_down[l, e_].rearrange("(k p) c -> p k c", p=128)), writes=[r_wd])
                bg, r_bg = bgrot.next()
                S.dma("sp", lambda e, bg=bg, e_=e_: e.dma_start(out=bg[:], in_=self.b_gu[l, e_, :].rearrange("(k p two) -> p k two", p=128, two=2),
                                                               allow_slow_non_contiguous=True), writes=[r_bg])
                bd, r_bd = bdrot.next()
                S.dma("act", lambda e, bd=bd, e_=e_: e.dma_start(out=bd[:], in_=self.b_down[l, e_:e_ + 1, :].to_broadcast([128, D])), writes=[r_bd])
                S.load_count(self.CNTI[0:1, e_:e_ + 1])
                args = ((wg, r_wg), (wu, r_wu), (wd, r_wd), (bg, r_bg), (bd, r_bd), xtrot, atrot, gsrot, sgrot, usrot, yrot)
                x0_next = None
                if e_ + 1 < NE:
                    x0_next = xe0rot.next()
                    load_xe(x0_next, (e_ + 1) * CAP, 4)
                xc_cur = xeCrot.next()
                load_xe(xc_cur, e_ * CAP + 512, 2)
                self.expert_ntile(e_ * CAP, 4, x0_cur, *args)
                regs = list(range(512, CAP, 256))
                for ri, r in enumerate(regs):
                    S.cond_begin(r)
                    xc_next = None
                    if ri + 1 < len(regs):
                        xc_next = xeCrot.next()
                        load_xe(xc_next, e_ * CAP + regs[ri + 1], 2)
                    self.expert_ntile(e_ * CAP + r, 2, xc_cur, *args)
                    S.cond_end()
                    if xc_next is not None:
                        xc_cur = xc_next
                x0_cur = x0_next
            self.end_phase()

    def expert_ntile(self, row0, nsub, xe_, wg_, wu_, wd_, bg_, bd_, xtrot, atrot, gsrot, sgrot, usrot, yrot):
        S = self.S
        W = nsub * 128
        wg, r_wg = wg_
        wu, r_wu = wu_
        wd, r_wd = wd_
        bg, r_bg = bg_
        bd, r_bd = bd_
        xe, r_xe = xe_
        xT, r_xT = xtrot.next()
        for jt in range(nsub):
            pt, r_p = self.pT.next()
            for k in range(8):
                S.op("pe", lambda e, pt=pt, jt=jt, k=k: e.transpose(out=pt[:, k * 128:(k + 1) * 128], in_=xe[:, jt, k * 128:(k + 1) * 128], identity=self.identb[:]),
                     reads=[r_xe, self.r_id], writes=[r_p], signal=(k == 7))
            S.op("dve", lambda e, pt=pt, jt=jt: e.tensor_copy(out=xT[:, :, jt * 128:(jt + 1) * 128], in_=pt[:, :].rearrange("p (k t) -> p k t", k=8)),
                 reads=[r_p], writes=[r_xT])
        aT, r_aT = atrot.next()
        pending = None
        for fk in range(8):
            pg, r_pg = self.pA.next()
            pu, r_pu = self.pA.next()
            for k in range(8):
                S.op("pe", lambda e, pg=pg, k=k, fk=fk: e.matmul(pg[:, 0:W], lhsT=wg[:, k, fk * 128:(fk + 1) * 128], rhs=xT[:, k, 0:W], start=(k == 0), stop=(k == 7)),
                     reads=[r_wg, r_xT], writes=[r_pg], signal=(k == 7))
            for k in range(8):
                S.op("pe", lambda e, pu=pu, k=k, fk=fk: e.matmul(pu[:, 0:W], lhsT=wu[:, k, fk * 128:(fk + 1) * 128], rhs=xT[:, k, 0:W], start=(k == 0), stop=(k == 7)),
                     reads=[r_wu, r_xT], writes=[r_pu], signal=(k == 7))
            gs, r_gs = gsrot.next()
            sg, r_sg = sgrot.next()
            us, r_us = usrot.next()
            S.op("dve", lambda e, gs=gs, pg=pg, fk=fk: e.tensor_scalar(out=gs[:, 0:W], in0=pg[:, 0:W], scalar1=bg[:, fk, 0:1], scalar2=7.0, op0=ALU.add, op1=ALU.min),
                 reads=[r_pg, r_bg], writes=[r_gs])
            S.op("act", lambda e, sg=sg, gs=gs: e.activation(out=sg[:, 0:W], in_=gs[:, 0:W], func=AF.Sigmoid, scale=1.702), reads=[r_gs], writes=[r_sg])
            S.op("dve", lambda e, us=us, pu=pu, fk=fk: e.tensor_scalar(out=us[:, 0:W], in0=pu[:, 0:W], scalar1=bg[:, fk, 1:2], scalar2=7.0, op0=ALU.add, op1=ALU.min),
                 reads=[r_pu, r_bg], writes=[r_us])
            S.op("dve", lambda e, us=us: e.tensor_scalar(out=us[:, 0:W], in0=us[:, 0:W], scalar1=-7.0, scalar2=1.0, op0=ALU.max, op1=ALU.add),
                 reads=[r_us], writes=[r_us])
            S.op("pool", lambda e, gs=gs, sg=sg: e.tensor_tensor(out=gs[:, 0:W], in0=gs[:, 0:W], in1=sg[:, 0:W], op=ALU.mult), reads=[r_gs, r_sg], writes=[r_gs])
            if pending is not None:
                pending()

            def fin(us=us, gs=gs, fk=fk, r_us=r_us, r_gs=r_gs):
                S.op("dve", lambda e: e.tensor_tensor(out=aT[:, fk, 0:W], in0=us[:, 0:W], in1=gs[:, 0:W], op=ALU.mult),
                     reads=[r_us, r_gs], writes=[r_aT])
            pending = fin
        pending()
        for jt in range(nsub):
            ye, r_ye = yrot.next()
            for half in range(2):
                pt, r_p = self.pA.next()
                for fk in range(8):
                    S.op("pe", lambda e, pt=pt, fk=fk, jt=jt, half=half: e.matmul(
                        pt[:, :], lhsT=aT[:, fk, jt * 128:(jt + 1) * 128], rhs=wd[:, fk, half * 512:(half + 1) * 512], start=(fk == 0), stop=(fk == 7)),
                        reads=[r_aT, r_wd], writes=[r_p], signal=(fk == 7))
                hs = slice(half * 512, (half + 1) * 512)
                S.op("dve", lambda e, ye=ye, pt=pt, hs=hs: e.tensor_tensor(out=ye[:, hs], in0=pt[:, :], in1=bd[:, hs], op=ALU.add),
                     reads=[r_p, r_bd], writes=[r_ye])
            S.dma("sp", lambda e, ye=ye, jt=jt: e.dma_start(out=self.YE[row0 + jt * 128:row0 + (jt + 1) * 128, :], in_=ye[:]),
                  reads=[r_ye], writes=[self.rYE])

    def phase_combine(self, l, last):
        nc, S = self.nc, self.S
        with ExitStack() as st:
            GT = self.sb(st, "GT2", [128, 2, D], F32)
            r_gt = Res()
            for j in range(2):
                self.load_bc("sp", GT[:, j, :], r_gt, self.MOD[l, j:j + 1, 5 * D:6 * D])
            if last:
                FG = self.sb(st, "FG", [128, D], F32)
                self.load_bc("sp", FG[:], r_gt, self.final_g[0:1, :])
            ykrot = self.rot_sb(st, "yk", [128, 4, D], F32, 4)
            accrot = self.rot_sb(st, "kacc", [128, D], F32, 3)
            xrot = self.rot_sb(st, "kx", [128, D], F32, 3)
            orot = self.rot_sb(st, "ko", [128, D], F32, 3)
            strot = self.stat_tiles(st, "kst")
            for i in range(self.nt_act):
                j = 0 if i < 32 else 1
                yk, r_yk = ykrot.next()
                for k in range(4):
                    S.dma("pool", lambda e, yk=yk, i=i, k=k: e.indirect_dma_start(
                        out=yk[:, k, :], out_offset=None, in_=self.YE[:, :], in_offset=bass.IndirectOffsetOnAxis(ap=self.DEST[:, i, k:k + 1], axis=0)),
                        reads=[self.rYE, self.rROUTE[i]], writes=[r_yk])
                xt, r_x = xrot.next()
                S.dma("sp", lambda e, xt=xt, i=i: e.dma_start(out=xt[:], in_=self.X[i * 128:(i + 1) * 128, :]), reads=[self.rX[i]], writes=[r_x])
                acc, r_a = accrot.next()
                S.op("dve", lambda e, acc=acc, yk=yk, i=i: e.tensor_scalar(out=acc[:], in0=yk[:, 0, :], scalar1=self.GATES[:, i, 0:1], scalar2=None, op0=ALU.mult),
                     reads=[r_yk, self.rROUTE[i]], writes=[r_a])
                for k in range(1, 4):
                    S.op("dve", lambda e, acc=acc, yk=yk, i=i, k=k: e.scalar_tensor_tensor(out=acc[:], in0=yk[:, k, :], scalar=self.GATES[:, i, k:k + 1], in1=acc[:], op0=ALU.mult, op1=ALU.add),
                         reads=[r_yk, self.rROUTE[i], r_a], writes=[r_a])
                S.op("pool", lambda e, acc=acc, j=j: e.tensor_tensor(out=acc[:], in0=acc[:], in1=GT[:, j, :], op=ALU.mult), reads=[r_a, r_gt], writes=[r_a])
                xo, r_xo = orot.next()
                S.op("pool", lambda e, xo=xo, acc=acc, xt=xt: e.tensor_tensor(out=xo[:], in0=acc[:], in1=xt[:], op=ALU.add), reads=[r_a, r_x], writes=[r_xo])
                if not last:
                    S.dma("sp", lambda e, xo=xo, i=i: e.dma_start(out=self.X[i * 128:(i + 1) * 128, :], in_=xo[:]), reads=[r_xo], writes=[self.rX[i]])
                else:
                    stt = strot.next()
                    rstd, r_s = self.rms_rstd(stt, xo, r_xo, D)
                    S.op("dve", lambda e, xo=xo, rstd=rstd: e.scalar_tensor_tensor(out=xo[:], in0=xo[:], scalar=rstd[:, 0:1], in1=FG[:], op0=ALU.mult, op1=ALU.mult),
                         reads=[r_xo, r_s, r_gt], writes=[r_xo])
                    S.dma("sp", lambda e, xo=xo, i=i: e.dma_start(out=self.out[i * 128:(i + 1) * 128, :], in_=xo[:]), reads=[r_xo], writes=[self.rOUT])
            self.end_phase()


def _na_table(rpb):
    a = np.arange(2)[:, None, None, None]
    kc = np.arange(64)[None, :, None, None]
    j = np.arange(NJ)[None, None, :, None]
    qc = np.arange(64)[None, None, None, :]
    dr = a - (j - JOFF) + 0 * kc + 0 * qc
    cs = np.clip(qc - 8, 0, 48)
    colv = (kc >= cs) & (kc < cs + 16)
    dc = kc - qc + 0 * a + 0 * j
    out = np.empty((2, 128, 8, NJ * 64), np.float32)
    for v in range(2):
        rowv = ((dr >= -4) & (dr <= 3)) if v == 0 else ((dr >= -7) & (dr <= 7))
        valid = np.broadcast_to(rowv & colv, (2, 64, NJ, 64))
        ri = np.clip(dr + 7, 0, 14)
        ci = np.clip(dc + 15, 0, 30)
        ri = np.broadcast_to(ri, (2, 64, NJ, 64))
        ci = np.broadcast_to(ci, (2, 64, NJ, 64))
        for h in range(8):
            vals = rpb[h][ri, ci]
            tbl = np.where(valid, vals, np.float32(NEG)).astype(np.float32)
            out[v, :, h, :] = tbl.reshape(128, NJ * 64)
    return out


def _rope_tables():
    half = 32
    inv = (10000.0 ** (-np.arange(0, half, 2, dtype=np.float32) / half)).astype(np.float32)
    t = np.arange(TL)
    ang_r = (t // 64).astype(np.float32)[:, None] * inv
    ang_c = (t % 64).astype(np.float32)[:, None] * inv
    cos = np.zeros((TL, 64), np.float32)
    sin = np.zeros((TL, 64), np.float32)
    for base, ang in ((0, ang_r), (32, ang_c)):
        c, s = np.cos(ang).astype(np.float32), np.sin(ang).astype(np.float32)
        cos[:, base:base + 16] = c
        cos[:, base + 16:base + 32] = c
        sin[:, base:base + 16] = -s
        sin[:, base + 16:base + 32] = s
    return np.tile(cos, (1, 10)), np.tile(sin, (1, 10))


def make_in_maps(inputs, n_cores=8):
    f = lambda a: np.ascontiguousarray(np.asarray(a, dtype=np.float32))
    x, c, ctx, c_ctx = f(inputs["x"]), f(inputs["c"]), f(inputs["ctx"]), f(inputs["c_ctx"])
    natab = np.stack([_na_table(f(inputs["na_rpb"])[l]) for l in range(DEPTH)])
    qg, kg = f(inputs["q_norm_g"]), f(inputs["k_norm_g"])
    qkgain = np.concatenate([np.tile(qg, (1, 8)), np.tile(kg, (1, 2))], axis=1)
    ropec, ropes = _rope_tables()
    w_br = np.stack([f(inputs["w_br_conv"]), f(inputs["w_br_na"]), f(inputs["w_br_gqa"])], axis=1)
    shared = dict(
        ada_w=f(inputs["ada_w"]), ada_b=f(inputs["ada_b"]), norm1_g=f(inputs["norm1_g"]), norm2_g=f(inputs["norm2_g"]),
        w_in=f(inputs["w_in"]), conv_w=f(inputs["conv_w"]), natab=natab, qkgain=np.ascontiguousarray(qkgain),
        ropec=ropec, ropes=ropes, w_br=np.ascontiguousarray(w_br), w_out=f(inputs["w_out"]),
        router_w=f(inputs["router_w"]), router_b=f(inputs["router_b"]), w_gu=f(inputs["w_gu"]), b_gu=f(inputs["b_gu"]),
        w_down=f(inputs["w_down"]), b_down=f(inputs["b_down"]), final_g=f(inputs["final_g"]).reshape(1, D))
    maps = []
    for b in range(n_cores):
        m = dict(shared)
        m["xin"] = np.ascontiguousarray(np.concatenate([x[b], ctx[b]], axis=0))
        m["cvec"] = np.ascontiguousarray(np.stack([c[b], c_ctx], axis=0))
        maps.append(m)
    return maps


def kernel(**inputs):
    nc = bass.Bass("TRN2", target_bir_lowering=False)
    Builder(nc).build()
    maps = make_in_maps(inputs)
    res = run_bass_kernel_spmd(nc, maps, core_ids=list(range(8)))
    return np.stack([np.asarray(r["out"], dtype=np.float32) for r in res.results], axis=0)
```
